# Optimizing a Trainium2 kernel written in Bass

```python
import math
import jax, jax.numpy as jnp
from jax import lax
import numpy as np

D_MODEL = 2048
BATCH = 1
SEQ = 8192
DEPTH = 2

CHUNK = 64
HEAD_DIM = 128
D_FF = ((8 * D_MODEL // 3 + 127) // 128) * 128
N_SUB = 3
EPS = 1e-6
D_A = D_MODEL // 2
S5_GROUP = 16
S5_GROUPS = D_A // S5_GROUP
S5_STATE = 64
DT_MIN = 1e-3
DT_MAX = 1e-1
D_B = D_MODEL // 2
B_HEADS = D_B // HEAD_DIM
KV_RANK = D_MODEL // 8
IDX_HEADS = 16
IDX_DIM = 64
TOPK_MAX = 256
Q_BLOCK = 128
AB_SPLITS = (D_A, D_A + D_B, D_A + D_B + KV_RANK, D_A + D_B + KV_RANK + IDX_HEADS * IDX_DIM, D_A + D_B + KV_RANK + IDX_HEADS * IDX_DIM + IDX_DIM)
D_IN_AB = AB_SPLITS[-1] + IDX_HEADS
C_HEADS = D_MODEL // HEAD_DIM
C_LEFT_CHUNKS = 8
BAND = (C_LEFT_CHUNKS + 1) * CHUNK
MAX_REL = 256
N_EVEN = (DEPTH + 1) // 2
N_ODD = DEPTH // 2

kernel_name = 'hybrid_s5_dsa_chunkattn_macaron'


def rmsnorm(x, g):
    xf = x.astype(jnp.float32)
    y = xf * lax.rsqrt(jnp.mean(xf * xf, axis=-1, keepdims=True) + EPS)
    return (y * g.astype(jnp.float32)).astype(x.dtype)


def adaln(x, g, shift, scale):
    return rmsnorm(x, g) * (1.0 + scale[:, None, :]) + shift[:, None, :]


def swiglu(h, w_gate, w_up, w_down):
    return (jax.nn.silu(h @ w_gate) * (h @ w_up)) @ w_down


def alibi_slopes(n_heads):
    return jnp.asarray(2.0 ** (-8.0 * (np.arange(n_heads) + 1) / n_heads), dtype=jnp.float32)


def s5_mixer(u, lam_re, lam_im, log_dt, b_re, b_im, c_re, c_im, d_skip, w_glu, b_glu):
    f32 = jnp.float32
    bsz, L, _ = u.shape
    uf = u.astype(f32)
    ug = uf.reshape(bsz, L, S5_GROUPS, S5_GROUP)
    lr = lam_re.astype(f32)
    li = lam_im.astype(f32)
    dt = jnp.exp(log_dt.astype(f32))[:, None]
    mag = jnp.exp(lr * dt)
    ab_re = mag * jnp.cos(li * dt)
    ab_im = mag * jnp.sin(li * dt)
    den = lr * lr + li * li
    nr = ab_re - 1.0
    f_re = (nr * lr + ab_im * li) / den
    f_im = (ab_im * lr - nr * li) / den
    br = b_re.astype(f32)
    bi = b_im.astype(f32)
    bb_re = f_re[..., None] * br - f_im[..., None] * bi
    bb_im = f_re[..., None] * bi + f_im[..., None] * br
    bu_re = jnp.einsum('blgc,gpc->blgp', ug, bb_re)
    bu_im = jnp.einsum('blgc,gpc->blgp', ug, bb_im)
    a_re = jnp.broadcast_to(ab_re, bu_re.shape)
    a_im = jnp.broadcast_to(ab_im, bu_im.shape)

    def combine(e1, e2):
        a1r, a1i, b1r, b1i = e1
        a2r, a2i, b2r, b2i = e2
        return (a2r * a1r - a2i * a1i, a2r * a1i + a2i * a1r,
                a2r * b1r - a2i * b1i + b2r, a2r * b1i + a2i * b1r + b2i)

    _, _, xr, xi = lax.associative_scan(combine, (a_re, a_im, bu_re, bu_im), axis=1)
    y = jnp.einsum('blgp,gcp->blgc', xr, c_re.astype(f32)) - jnp.einsum('blgp,gcp->blgc', xi, c_im.astype(f32))
    y = y.reshape(bsz, L, D_A) + d_skip.astype(f32) * uf
    y = jax.nn.gelu(y.astype(u.dtype))
    return y * jax.nn.sigmoid(y @ w_glu + b_glu)


def dsa_mixer(q, kv_lat, q_idx, k_idx, w_idx, kv_norm_g, w_kv_up):
    f32 = jnp.float32
    bsz, L, _ = q.shape
    topk = min(TOPK_MAX, L // 4)
    kv = rmsnorm(kv_lat, kv_norm_g) @ w_kv_up
    k, v = jnp.split(kv, 2, axis=-1)
    k = k.reshape(bsz, L, B_HEADS, HEAD_DIM)
    v = v.reshape(bsz, L, B_HEADS, HEAD_DIM)
    q = q.reshape(bsz, L, B_HEADS, HEAD_DIM)
    q_idx = q_idx.reshape(bsz, L, IDX_HEADS, IDX_DIM)
    n_blk = L // Q_BLOCK
    key_chunk = jnp.arange(L) // CHUNK
    slopes = alibi_slopes(B_HEADS)
    k_idx32 = k_idx.astype(f32)

    def to_blocks(a):
        return jnp.moveaxis(a.reshape(bsz, n_blk, Q_BLOCK, *a.shape[2:]), 1, 0)

    def block(args):
        blk, qb, qib, wb = args
        t = blk * Q_BLOCK + jnp.arange(Q_BLOCK)
        s_h = jnp.einsum('bqhd,bsd->bqhs', qib.astype(f32), k_idx32) * (IDX_DIM ** -0.5)
        score = jnp.einsum('bqhs,bqh->bqs', jax.nn.relu(s_h), wb.astype(f32)) * (IDX_HEADS ** -0.5)
        admissible = key_chunk[None, :] <= (t // CHUNK)[:, None]
        score = jnp.where(admissible[None], score, -jnp.inf)
        _, sel = lax.top_k(score, topk)
        k_sel = jax.vmap(lambda kk, ii: kk[ii])(k, sel)
        v_sel = jax.vmap(lambda vv, ii: vv[ii])(v, sel)
        logits = jnp.einsum('bqhd,bqkhd->bhqk', qb, k_sel).astype(f32) * (HEAD_DIM ** -0.5)
        dist = jnp.abs(t[None, :, None] - sel).astype(f32)
        logits = logits - slopes[None, :, None, None] * dist[:, None]
        valid = (sel // CHUNK) <= (t // CHUNK)[None, :, None]
        logits = jnp.where(valid[:, None], logits, -jnp.inf)
        p = jax.nn.softmax(logits, axis=-1).astype(v.dtype)
        return jnp.einsum('bhqk,bqkhd->bqhd', p, v_sel)

    out = lax.map(block, (jnp.arange(n_blk), to_blocks(q), to_blocks(q_idx), to_blocks(w_idx)))
    return jnp.moveaxis(out, 0, 1).reshape(bsz, L, D_B)


def ab_mixer(h, w_in, lam_re, lam_im, log_dt, b_re, b_im, c_re, c_im, d_skip, w_glu, b_glu, kv_norm_g, w_kv_up, w_out):
    proj = h @ w_in
    u, q, kv_lat, q_idx, k_idx, w_idx = jnp.split(proj, list(AB_SPLITS), axis=-1)
    y_a = s5_mixer(u, lam_re, lam_im, log_dt, b_re, b_im, c_re, c_im, d_skip, w_glu, b_glu)
    y_b = dsa_mixer(q, kv_lat, q_idx, k_idx, w_idx, kv_norm_g, w_kv_up)
    return jnp.concatenate([y_a, y_b], axis=-1) @ w_out


def chunked_relpos_attention(h, w_qkv, rel_bias, w_out):
    f32 = jnp.float32
    bsz, L, _ = h.shape
    n_chunks = L // CHUNK
    pad = C_LEFT_CHUNKS * CHUNK
    q, k, v = jnp.split(h @ w_qkv, 3, axis=-1)
    q = q.reshape(bsz, L, C_HEADS, HEAD_DIM)
    k = jnp.pad(k.reshape(bsz, L, C_HEADS, HEAD_DIM), ((0, 0), (pad, 0), (0, 0), (0, 0)))
    v = jnp.pad(v.reshape(bsz, L, C_HEADS, HEAD_DIM), ((0, 0), (pad, 0), (0, 0), (0, 0)))
    i = jnp.arange(CHUNK)
    j = jnp.arange(BAND)
    rel = i[:, None] - j[None, :] + pad
    bias = rel_bias.astype(f32)[:, jnp.clip(rel, -MAX_REL, MAX_REL) + MAX_REL]
    q_chunks = jnp.moveaxis(q.reshape(bsz, n_chunks, CHUNK, C_HEADS, HEAD_DIM), 1, 0)

    def chunk_fn(args):
        cidx, qc = args
        kb = lax.dynamic_slice_in_dim(k, cidx * CHUNK, BAND, axis=1)
        vb = lax.dynamic_slice_in_dim(v, cidx * CHUNK, BAND, axis=1)
        logits = jnp.einsum('bqhd,bkhd->bhqk', qc, kb).astype(f32) * (HEAD_DIM ** -0.5) + bias[None]
        valid = (cidx * CHUNK - pad + j) >= 0
        logits = jnp.where(valid[None, None, None, :], logits, -jnp.inf)
        p = jax.nn.softmax(logits, axis=-1).astype(vb.dtype)
        return jnp.einsum('bhqk,bkhd->bqhd', p, vb)

    out = lax.map(chunk_fn, (jnp.arange(n_chunks), q_chunks))
    return jnp.moveaxis(out, 0, 1).reshape(bsz, L, D_MODEL) @ w_out


def setup_inputs(seed: int = 0) -> dict:
    key = jax.random.key(seed)
    ks = jax.random.split(key, 32)
    f32 = jnp.float32
    D = D_MODEL
    G, P, CG = S5_GROUPS, S5_STATE, S5_GROUP

    def nrm(k, shape, scale):
        return jax.random.normal(k, shape, f32) * scale

    return {
        'x': nrm(ks[0], (BATCH, SEQ, D), 1.0),
        'c': nrm(ks[1], (BATCH, D), 1.0),
        'ada_w': nrm(ks[2], (DEPTH, D, N_SUB * 3 * D), 0.5 * D ** -0.5),
        'ada_b': nrm(ks[3], (DEPTH, N_SUB * 3 * D), 0.02),
        'norm_g': 1.0 + nrm(ks[4], (DEPTH, N_SUB, D), 0.02),
        'ffn_w_gate': nrm(ks[5], (DEPTH, 2, D, D_FF), D ** -0.5),
        'ffn_w_up': nrm(ks[6], (DEPTH, 2, D, D_FF), D ** -0.5),
        'ffn_w_down': nrm(ks[7], (DEPTH, 2, D_FF, D), D_FF ** -0.5),
        'ab_w_in': nrm(ks[8], (N_EVEN, D, D_IN_AB), D ** -0.5),
        's5_lam_re': -0.5 + nrm(ks[9], (N_EVEN, G, P), 0.01),
        's5_lam_im': jnp.pi * jnp.arange(P, dtype=f32) + nrm(ks[10], (N_EVEN, G, P), 0.01),
        's5_log_dt': jax.random.uniform(ks[11], (N_EVEN, G), f32, minval=math.log(DT_MIN), maxval=math.log(DT_MAX)),
        's5_b_re': nrm(ks[12], (N_EVEN, G, P, CG), (2 * CG) ** -0.5),
        's5_b_im': nrm(ks[13], (N_EVEN, G, P, CG), (2 * CG) ** -0.5),
        's5_c_re': nrm(ks[14], (N_EVEN, G, CG, P), (2 * P) ** -0.5),
        's5_c_im': nrm(ks[15], (N_EVEN, G, CG, P), (2 * P) ** -0.5),
        's5_d': nrm(ks[16], (N_EVEN, D_A), 0.5),
        's5_w_glu': nrm(ks[17], (N_EVEN, D_A, D_A), D_A ** -0.5),
        's5_b_glu': nrm(ks[18], (N_EVEN, D_A), 0.02),
        'dsa_kv_norm_g': 1.0 + nrm(ks[19], (N_EVEN, KV_RANK), 0.02),
        'dsa_w_kv_up': nrm(ks[20], (N_EVEN, KV_RANK, 2 * D_B), KV_RANK ** -0.5),
        'ab_w_out': nrm(ks[21], (N_EVEN, D_A + D_B, D), (D_A + D_B) ** -0.5),
        'c_w_qkv': nrm(ks[22], (N_ODD, D, 3 * D), D ** -0.5),
        'c_rel_bias': nrm(ks[23], (N_ODD, C_HEADS, 2 * MAX_REL + 1), 0.5),
        'c_w_out': nrm(ks[24], (N_ODD, D, D), D ** -0.5),
        'final_norm_g': 1.0 + nrm(ks[25], (D,), 0.02),
    }


def reference(x, c, ada_w, ada_b, norm_g, ffn_w_gate, ffn_w_up, ffn_w_down,
              ab_w_in, s5_lam_re, s5_lam_im, s5_log_dt, s5_b_re, s5_b_im, s5_c_re, s5_c_im,
              s5_d, s5_w_glu, s5_b_glu, dsa_kv_norm_g, dsa_w_kv_up, ab_w_out,
              c_w_qkv, c_rel_bias, c_w_out, final_norm_g):
    bsz = x.shape[0]
    cond = jax.nn.silu(c)
    h = x
    for layer in range(DEPTH):
        mod = (cond @ ada_w[layer] + ada_b[layer]).reshape(bsz, N_SUB, 3, D_MODEL)
        shift, scale, gate = mod[:, :, 0], mod[:, :, 1], mod[:, :, 2]
        y = swiglu(adaln(h, norm_g[layer, 0], shift[:, 0], scale[:, 0]),
                   ffn_w_gate[layer, 0], ffn_w_up[layer, 0], ffn_w_down[layer, 0])
        h = h + 0.5 * gate[:, 0, None, :] * y
        hn = adaln(h, norm_g[layer, 1], shift[:, 1], scale[:, 1])
        if layer % 2 == 0:
            e = layer // 2
            y = ab_mixer(hn, ab_w_in[e], s5_lam_re[e], s5_lam_im[e], s5_log_dt[e], s5_b_re[e], s5_b_im[e],
                         s5_c_re[e], s5_c_im[e], s5_d[e], s5_w_glu[e], s5_b_glu[e],
                         dsa_kv_norm_g[e], dsa_w_kv_up[e], ab_w_out[e])
        else:
            o = layer // 2
            y = chunked_relpos_attention(hn, c_w_qkv[o], c_rel_bias[o], c_w_out[o])
        h = h + gate[:, 1, None, :] * y
        y = swiglu(adaln(h, norm_g[layer, 2], shift[:, 2], scale[:, 2]),
                   ffn_w_gate[layer, 1], ffn_w_up[layer, 1], ffn_w_down[layer, 1])
        h = h + 0.5 * gate[:, 2, None, :] * y
    return rmsnorm(h, final_norm_g)
```

```python
import numpy as np
import concourse.bass as bass
import concourse.mybir as mybir
from concourse.bass_utils import run_bass_kernel_spmd

F32 = mybir.dt.float32
BF16 = mybir.dt.bfloat16
AF = mybir.ActivationFunctionType
ALU = mybir.AluOpType
AX = mybir.AxisListType

NCORES = 8


class Buf:
    __slots__ = ("name", "lw", "rd")

    def __init__(self, name):
        self.name = name
        self.lw = None
        self.rd = {}


class Prog:
    ENG = ("pe", "act", "dve", "pool", "sp")

    def __init__(self, nc, n_dma_sems=12):
        self.nc = nc
        self.ops = {e: [] for e in self.ENG}
        self.cnt = {e: 0 for e in self.ENG}
        self.seen = {e: {} for e in self.ENG}
        self.n_dma_sems = n_dma_sems
        self.dma_cnt = [0] * n_dma_sems
        self.dma_rr = 0
        self.sems = {}
        self._stack = None

    def _deps(self, eng, reads, writes):
        need = {}

        def add(kc):
            if kc is None:
                return
            k, c = kc
            if need.get(k, 0) < c:
                need[k] = c

        for b in reads:
            add(b.lw)
        for b in writes:
            add(b.lw)
            for k, c in b.rd.items():
                add((k, c))
        out = []
        for k, c in need.items():
            if k == "pe" and eng == "pe":
                continue
            if self.seen[eng].get(k, 0) >= c:
                continue
            self.seen[eng][k] = c
            out.append((k, c))
        return out

    def op(self, eng, fn, reads=(), writes=()):
        waits = self._deps(eng, reads, writes)
        self.cnt[eng] += 1
        me = (eng, self.cnt[eng])
        for b in reads:
            b.rd[eng] = me[1]
        for b in writes:
            b.lw = me
            b.rd = {}
        self.ops[eng].append((waits, fn, (eng, 1)))

    def dma(self, eng, fn, reads=(), writes=()):
        s = self.dma_rr
        self.dma_rr = (self.dma_rr + 1) % self.n_dma_sems
        key = "dma%d" % s
        waits = self._deps(eng, reads, writes)
        prev = self.dma_cnt[s]
        if prev and self.seen[eng].get(key, 0) < prev:
            self.seen[eng][key] = prev
            waits.append((key, prev))
        self.dma_cnt[s] += 1
        me = (key, self.dma_cnt[s])
        for b in reads:
            b.rd[key] = me[1]
        for b in writes:
            b.lw = me
            b.rd = {}
        self.ops[eng].append((waits, fn, (key, 16)))

    def final_wait(self, eng, bufs):
        waits = self._deps(eng, bufs, ())
        self.ops[eng].append((waits, None, None))

    def emit(self):
        nc = self.nc
        import contextlib
        with contextlib.ExitStack() as st:
            keys = list(self.ENG) + ["dma%d" % i for i in range(self.n_dma_sems)]
            sem = {k: st.enter_context(nc.semaphore("s_" + k)) for k in keys}
            block = st.enter_context(nc.Block())
            mult = {k: (16 if k.startswith("dma") else 1) for k in keys}

            def run(engname):
                def body(e):
                    for waits, fn, inc in self.ops[engname]:
                        for k, c in waits:
                            e.wait_ge(sem[k], c * mult[k])
                        if fn is not None:
                            ins = fn(e)
                            ins.then_inc(sem[inc[0]], inc[1])
                return body

            block.tensor(run("pe"))
            block.scalar(run("act"))
            block.vector(run("dve"))
            block.gpsimd(run("pool"))
            block.sync(run("sp"))


D = 2048
KC = D // 128
DFF = 5504
FC = DFF // 128
SEQ = 8192
TOKC = SEQ // NCORES
TT = 512
EPS = 1e-6
D_IN_AB = 3408


class Ctx:
    def __init__(self, nc, st):
        self.nc = nc
        self.st = st
        self.P = Prog(nc)
        self._n = 0
        self.outs = []

    def sb(self, shape, dt, name=None):
        self._n += 1
        return self.st.enter_context(self.nc.sbuf_tensor("sb_" + (name or str(self._n)), list(shape), dt))

    def ps(self, shape=(128, 512), dt=F32, name=None):
        self._n += 1
        return self.st.enter_context(self.nc.psum_tensor("ps_" + (name or str(self._n)), list(shape), dt))

    def din(self, name, shape, dt=F32):
        return self.nc.dram_tensor(name, list(shape), dt, kind="ExternalInput").ap()

    def dout(self, name, shape, dt=F32):
        return self.nc.dram_tensor(name, list(shape), dt, kind="ExternalOutput").ap()

    def finish(self):
        P = self.P
        waits = [("dma%d" % i, P.dma_cnt[i]) for i in range(P.n_dma_sems) if P.dma_cnt[i]]
        waits += [(e, P.cnt[e]) for e in ("pe", "act", "dve", "pool") if P.cnt[e]]
        P.ops["sp"].append((waits, None, None))
        P.emit()


class Stream:
    def __init__(self, cx, nslots=3, elems=8192):
        self.cx = cx
        self.n = nslots
        self.elems = elems
        self.t = cx.sb([128, nslots, elems], BF16, "wbuf")
        self.bufs = [Buf("w%d" % i) for i in range(nslots)]
        self.i = 0

    def load(self, src_ap, k, n, rows=128):
        s = self.i
        self.i = (self.i + 1) % self.n
        view = self.t[0:rows, s, 0:k * n].rearrange("p (k n) -> p k n", k=k)
        b = self.bufs[s]
        self.cx.P.dma("pool", lambda e, v=view, a=src_ap: e.dma_start(out=v, in_=a), writes=[b])
        return view, b


class Core:
    def __init__(self, cx, ntok=TOKC):
        self.cx = cx
        P = cx.P
        self.ntok = ntok
        self.hT = cx.sb([128, KC, ntok], F32, "hT")
        self.hT_b = [Buf("hT%d" % k) for k in range(KC)]
        self.hn = cx.sb([128, KC, TT], BF16, "hn")
        self.hn_b = Buf("hn")
        self.A = cx.sb([128, FC, TT], BF16, "A")
        self.A_b = [Buf("A%d" % f) for f in range(FC)]
        self.ws = Stream(cx)
        self.ones = cx.sb([128, 128], BF16, "ones")
        self.ones_b = Buf("ones")
        P.op("pool", lambda e: e.memset(self.ones[:], 1.0), writes=[self.ones_b])
        self.sq = [cx.sb([128, TT], BF16, "sq%d" % i) for i in range(2)]
        self.sq_b = [Buf("sq%d" % i) for i in range(2)]
        self.tmp = [cx.sb([128, TT], F32, "tmp%d" % i) for i in range(3)]
        self.tmp_b = [Buf("tmp%d" % i) for i in range(3)]
        self.tmp_i = 0
        self.rstd = cx.sb([128, TT], F32, "rstd")
        self.rstd_b = Buf("rstd")
        self.rtmp = cx.sb([128, TT], F32, "rtmp")
        self.rtmp_b = Buf("rtmp")
        self.pgu = [cx.ps(name="pgu%d" % i) for i in range(4)]
        self.pgu_b = [Buf("pgu%d" % i) for i in range(4)]
        self.pacc = [cx.ps(name="pacc%d" % i) for i in range(2)]
        self.pacc_b = [Buf("pacc%d" % i) for i in range(2)]
        self.pacc_i = 0
        self.pst = cx.ps(name="pst")
        self.pst_b = Buf("pst")
        self.modT = cx.sb([128, 144], F32, "modT")
        self.mod_b = Buf("mod")
        self.vec = cx.sb([128, 3, 3, KC], F32, "vec")
        self.vec_b = Buf("vec")

    def next_tmp(self):
        i = self.tmp_i
        self.tmp_i = (i + 1) % len(self.tmp)
        return self.tmp[i], self.tmp_b[i]

    def next_acc(self):
        i = self.pacc_i
        self.pacc_i = (i + 1) % len(self.pacc)
        return self.pacc[i], self.pacc_b[i]

    def load_h(self, hT_dram):
        P = self.cx.P
        src = hT_dram.rearrange("(k p) t -> p k t", p=128)
        for k in range(KC):
            P.dma("sp", lambda e, k=k: e.dma_start(out=self.hT[:, k, :], in_=src[:, k, :]), writes=[self.hT_b[k]])

    def store_h(self, out_dram):
        P = self.cx.P
        dst = out_dram.rearrange("(k p) t -> p k t", p=128)
        for k in range(KC):
            P.dma("sp", lambda e, k=k: e.dma_start(out=dst[:, k, :], in_=self.hT[:, k, :]), reads=[self.hT_b[k]])

    def compute_mod(self, condT_dram, ada_w, ada_bT_dram, gT_dram):
        cx, P = self.cx, self.cx.P
        self._modn = getattr(self, "_modn", 0) + 1
        sfx = str(self._modn)
        c32 = cx.sb([128, KC], F32, "c32" + sfx)
        cb = cx.sb([128, KC], BF16, "cb" + sfx)
        abT = cx.sb([128, 144], F32, "abT" + sfx)
        gT = cx.sb([128, 3, KC], F32, "gT" + sfx)
        b_c32, b_cb, b_ab, b_g = Buf("c32"), Buf("cb"), Buf("abT"), Buf("gT")
        P.dma("sp", lambda e: e.dma_start(out=c32[:], in_=condT_dram), writes=[b_c32])
        P.dma("sp", lambda e: e.dma_start(out=abT[:], in_=ada_bT_dram), writes=[b_ab])
        P.dma("sp", lambda e: e.dma_start(out=gT[:], in_=gT_dram), writes=[b_g])
        P.op("act", lambda e: e.activation(out=cb[:], in_=c32[:], func=AF.Silu), reads=[b_c32], writes=[b_cb])
        wsrc = ada_w.rearrange("(k p) n -> p k n", p=128)
        pm = self.pacc[0]
        pm_b = self.pacc_b[0]
        for nb in range(36):
            view, wb = self.ws.load(wsrc[:, :, nb * 512:(nb + 1) * 512], KC, 512)
            for j in range(4):
                col = nb * 4 + j
                for k in range(KC):
                    P.op("pe", lambda e, v=view, j=j, k=k, col=col: e.matmul(
                        pm[:, col:col + 1], v[:, k, j * 128:(j + 1) * 128], cb[:, k:k + 1],
                        start=(k == 0), stop=(k == KC - 1)), reads=[wb, b_cb], writes=[pm_b])
        P.op("dve", lambda e: e.tensor_tensor(out=self.modT[:], in0=pm[:, 0:144], in1=abT[:], op=ALU.add),
             reads=[pm_b, b_ab], writes=[self.mod_b])
        self.derive_vec(gT, b_g)

    def load_mod(self, mod_dram, gT_dram):
        cx, P = self.cx, self.cx.P
        self._modn = getattr(self, "_modn", 0) + 1
        gT = cx.sb([128, 3, KC], F32, "gT" + str(self._modn))
        b_g = Buf("gT")
        P.dma("sp", lambda e: e.dma_start(out=gT[:], in_=gT_dram), writes=[b_g])
        P.dma("sp", lambda e: e.dma_start(out=self.modT[:], in_=mod_dram), writes=[self.mod_b])
        self.derive_vec(gT, b_g)

    def derive_vec(self, gT, b_g):
        P = self.cx.P
        for s in range(3):
            sh = self.modT[:, (s * 3 + 0) * KC:(s * 3 + 1) * KC]
            sc = self.modT[:, (s * 3 + 1) * KC:(s * 3 + 2) * KC]
            ga = self.modT[:, (s * 3 + 2) * KC:(s * 3 + 3) * KC]
            P.op("dve", lambda e, s=s, sc=sc: e.scalar_tensor_tensor(
                out=self.vec[:, s, 0, :], in0=sc, scalar=1.0, in1=gT[:, s, :], op0=ALU.add, op1=ALU.mult),
                reads=[self.mod_b, b_g], writes=[self.vec_b])
            P.op("dve", lambda e, s=s, sh=sh: e.tensor_copy(self.vec[:, s, 1, :], sh),
                 reads=[self.mod_b], writes=[self.vec_b])
            cgate = 1.0 if s == 1 else 0.5
            P.op("dve", lambda e, s=s, ga=ga, cgate=cgate: e.tensor_scalar(
                out=self.vec[:, s, 2, :], in0=ga, scalar1=cgate, scalar2=None, op0=ALU.mult),
                reads=[self.mod_b], writes=[self.vec_b])

    def adaln(self, sub, t0, plain_g=None):
        cx, P = self.cx, self.cx.P
        for k in range(KC):
            i = k % 2
            P.op("act", lambda e, k=k, i=i: e.activation(out=self.sq[i][:], in_=self.hT[:, k, t0:t0 + TT], func=AF.Square),
                 reads=[self.hT_b[k]], writes=[self.sq_b[i]])
            P.op("pe", lambda e, k=k, i=i: e.matmul(self.pst[:], self.ones[:], self.sq[i][:], start=(k == 0), stop=(k == KC - 1)),
                 reads=[self.sq_b[i], self.ones_b], writes=[self.pst_b])
        P.op("dve", lambda e: e.tensor_scalar(out=self.rtmp[:], in0=self.pst[:], scalar1=1.0 / D, scalar2=EPS,
                                              op0=ALU.mult, op1=ALU.add), reads=[self.pst_b], writes=[self.rtmp_b])
        P.op("act", lambda e: e.activation(out=self.rtmp[:], in_=self.rtmp[:], func=AF.Sqrt),
             reads=[self.rtmp_b], writes=[self.rtmp_b])
        P.op("dve", lambda e: e.reciprocal(self.rstd[:], self.rtmp[:]), reads=[self.rtmp_b], writes=[self.rstd_b])

    def adaln_apply(self, sub, t0, k, out_ap, out_bufs, gs_ap=None, shift_ap=None):
        P = self.cx.P
        tmp, tb = self.next_tmp()
        gs = gs_ap if gs_ap is not None else self.vec[:, sub, 0, k:k + 1]
        P.op("dve", lambda e: e.scalar_tensor_tensor(out=tmp[:], in0=self.hT[:, k, t0:t0 + TT], scalar=gs,
                                                     in1=self.rstd[:], op0=ALU.mult, op1=ALU.mult),
             reads=[self.hT_b[k], self.rstd_b, self.vec_b], writes=[tb])
        if shift_ap is None and gs_ap is None:
            shift_ap = self.vec[:, sub, 1, k:k + 1]
        if shift_ap is not None:
            P.op("act", lambda e: e.activation(out=out_ap, in_=tmp[:], func=AF.Identity, bias=shift_ap, scale=1.0),
                 reads=[tb, self.vec_b], writes=out_bufs)
        else:
            P.op("act", lambda e: e.activation(out=out_ap, in_=tmp[:], func=AF.Copy), reads=[tb], writes=out_bufs)

    def adaln_full(self, sub, t0):
        self.adaln(sub, t0)
        for k in range(KC):
            self.adaln_apply(sub, t0, k, self.hn[:, k, :], [self.hn_b])

    def ffn(self, sub, w_gate, w_up, w_down):
        cx, P = self.cx, self.cx.P
        wg_src = w_gate.rearrange("(k p) n -> p k n", p=128)
        wu_src = w_up.rearrange("(k p) n -> p k n", p=128)
        wd_src = w_down.rearrange("(f p) n -> p f n", p=128)
        for t0 in range(0, self.ntok, TT):
            self.adaln_full(sub, t0)
            gi = 0
            for nb in range((FC + 1) // 2):
                ncol = min(256, DFF - nb * 256)
                nj = ncol // 128
                vg, bg = self.ws.load(wg_src[:, :, nb * 256:nb * 256 + ncol], KC, ncol)
                vu, bu = self.ws.load(wu_src[:, :, nb * 256:nb * 256 + ncol], KC, ncol)
                for j in range(nj):
                    f = nb * 2 + j
                    pg, pg_b = self.pgu[gi], self.pgu_b[gi]
                    pu, pu_b = self.pgu[gi + 1], self.pgu_b[gi + 1]
                    gi = (gi + 2) % 4
                    for k in range(KC):
                        P.op("pe", lambda e, k=k, j=j, vg=vg, pg=pg: e.matmul(
                            pg[:], vg[:, k, j * 128:(j + 1) * 128], self.hn[:, k, :], start=(k == 0), stop=(k == KC - 1)),
                            reads=[bg, self.hn_b], writes=[pg_b])
                    for k in range(KC):
                        P.op("pe", lambda e, k=k, j=j, vu=vu, pu=pu: e.matmul(
                            pu[:], vu[:, k, j * 128:(j + 1) * 128], self.hn[:, k, :], start=(k == 0), stop=(k == KC - 1)),
                            reads=[bu, self.hn_b], writes=[pu_b])
                    tmp, tb = self.next_tmp()
                    P.op("act", lambda e, tmp=tmp, pg=pg: e.activation(out=tmp[:], in_=pg[:], func=AF.Silu),
                         reads=[pg_b], writes=[tb])
                    P.op("dve", lambda e, tmp=tmp, pu=pu, f=f: e.tensor_tensor(out=self.A[:, f, :], in0=tmp[:], in1=pu[:], op=ALU.mult),
                         reads=[tb, pu_b], writes=[self.A_b[f]])
            for dc in range(KC):
                vd, bd = self.ws.load(wd_src[:, :, dc * 128:(dc + 1) * 128], FC, 128)
                py, py_b = self.next_acc()
                for f in range(FC):
                    P.op("pe", lambda e, f=f, vd=vd, py=py: e.matmul(
                        py[:], vd[:, f, :], self.A[:, f, :], start=(f == 0), stop=(f == FC - 1)),
                        reads=[bd, self.A_b[f]], writes=[py_b])
                P.op("dve", lambda e, dc=dc, py=py, t0=t0: e.scalar_tensor_tensor(
                    out=self.hT[:, dc, t0:t0 + TT], in0=py[:], scalar=self.vec[:, sub, 2, dc:dc + 1],
                    in1=self.hT[:, dc, t0:t0 + TT], op0=ALU.mult, op1=ALU.add),
                    reads=[py_b, self.vec_b, self.hT_b[dc]], writes=[self.hT_b[dc]])

    def proj(self, w, ncols, t0, epilogue, xsrc=None, xbufs=None, nk=KC):
        cx, P = self.cx, self.cx.P
        xsrc = self.hn if xsrc is None else xsrc
        xbufs = [self.hn_b] * nk if xbufs is None else xbufs
        src = w.rearrange("(k p) n -> p k n", p=128)
        for nb in range((ncols + 511) // 512):
            nc_ = min(512, ncols - nb * 512)
            v, b = self.ws.load(src[:, :, nb * 512:nb * 512 + nc_], nk, nc_)
            for j in range((nc_ + 127) // 128):
                rows = min(128, nc_ - j * 128)
                pa, pa_b = self.next_acc()
                for k in range(nk):
                    P.op("pe", lambda e, k=k, j=j, v=v, pa=pa, rows=rows: e.matmul(
                        pa[0:rows, :], v[:, k, j * 128:j * 128 + rows], xsrc[:, k, :], start=(k == 0), stop=(k == nk - 1)),
                        reads=[b, xbufs[k]], writes=[pa_b])
                epilogue(nb * 4 + j, rows, pa, pa_b)


    def resid_epilogue(self, sub, t0):
        P = self.cx.P

        def epi(c, rows, pa, pa_b):
            P.op("dve", lambda e: e.scalar_tensor_tensor(
                out=self.hT[:, c, t0:t0 + TT], in0=pa[:], scalar=self.vec[:, sub, 2, c:c + 1],
                in1=self.hT[:, c, t0:t0 + TT], op0=ALU.mult, op1=ALU.add),
                reads=[pa_b, self.vec_b, self.hT_b[c]], writes=[self.hT_b[c]])
        return epi

    def out_epilogue(self, dst, t0):
        cx, P = self.cx, self.cx.P
        if not hasattr(self, "_stg"):
            self._stg = [cx.sb([128, TT], F32, "ostg%d" % i) for i in range(2)]
            self._stg_b = [Buf("ostg%d" % i) for i in range(2)]
            self._stg_i = 0

        def epi(c, rows, pa, pa_b):
            i = self._stg_i
            self._stg_i = (i + 1) % 2
            stg, sb_ = self._stg[i], self._stg_b[i]
            P.op("act", lambda e: e.activation(out=stg[0:rows, :], in_=pa[0:rows, :], func=AF.Copy), reads=[pa_b], writes=[sb_])
            P.dma("sp", lambda e: e.dma_start(out=dst[c * 128:c * 128 + rows, t0:t0 + TT], in_=stg[0:rows, :]), reads=[sb_])
        return epi

    def mixer0_out(self, yaT, ybT, w_glu, bgT, w_out):
        cx, P = self.cx, self.cx.P
        bg = cx.sb([128, 8], F32, "bglu")
        bg_b = Buf("bglu")
        P.dma("sp", lambda e: e.dma_start(out=bg[:], in_=bgT), writes=[bg_b])
        ya_src = yaT.rearrange("(k p) t -> p k t", p=128)
        yb_src = ybT.rearrange("(k p) t -> p k t", p=128)
        for t0 in range(0, self.ntok, TT):
            P.dma("pool", lambda e, t0=t0: e.dma_start(out=self.hn[:, 0:8, :], in_=ya_src[:, :, t0:t0 + TT]), writes=[self.hn_b])
            for k in range(8):
                P.dma("pool", lambda e, t0=t0, k=k: e.dma_start(out=self.A[:, 8 + k, :], in_=yb_src[:, k, t0:t0 + TT]), writes=[self.A_b[8 + k]])

            def epi(c, rows, pa, pa_b):
                tmp, tb = self.next_tmp()
                P.op("act", lambda e: e.activation(out=tmp[:], in_=pa[:], func=AF.Sigmoid, bias=bg[:, c:c + 1], scale=1.0),
                     reads=[pa_b, bg_b], writes=[tb])
                P.op("dve", lambda e: e.tensor_tensor(out=self.A[:, c, :], in0=tmp[:], in1=self.hn[:, c, :], op=ALU.mult),
                     reads=[tb, self.hn_b], writes=[self.A_b[c]])
            self.proj(w_glu, 1024, t0, epi, nk=8)
            self.proj(w_out, D, t0, self.resid_epilogue(1, t0), xsrc=self.A, xbufs=self.A_b[0:KC], nk=KC)

    def mixer1_out(self, oT, w_out):
        P = self.cx.P
        o_src = oT.rearrange("(k p) t -> p k t", p=128)
        for t0 in range(0, self.ntok, TT):
            P.dma("pool", lambda e, t0=t0: e.dma_start(out=self.hn[:], in_=o_src[:, :, t0:t0 + TT]), writes=[self.hn_b])
            self.proj(w_out, D, t0, self.resid_epilogue(1, t0))

    def final_norm(self, gfT, outT):
        cx, P = self.cx, self.cx.P
        gf = cx.sb([128, KC], F32, "gfin")
        gf_b = Buf("gfin")
        P.dma("sp", lambda e: e.dma_start(out=gf[:], in_=gfT), writes=[gf_b])
        dst = outT.rearrange("(k p) t -> p k t", p=128)
        for t0 in range(0, self.ntok, TT):
            self.adaln(0, t0)
            for k in range(KC):
                tmp, tb = self.next_tmp()
                P.op("dve", lambda e, k=k, tmp=tmp, t0=t0: e.scalar_tensor_tensor(
                    out=tmp[:], in0=self.hT[:, k, t0:t0 + TT], scalar=gf[:, k:k + 1], in1=self.rstd[:], op0=ALU.mult, op1=ALU.mult),
                    reads=[self.hT_b[k], self.rstd_b, gf_b], writes=[tb])
                P.dma("sp", lambda e, k=k, tmp=tmp, t0=t0: e.dma_start(out=dst[:, k, t0:t0 + TT], in_=tmp[:]), reads=[tb])


def build_A0():
    import contextlib
    nc = bass.Bass("TRN2", target_bir_lowering=False)
    with contextlib.ExitStack() as st:
        cx = Ctx(nc, st)
        P = cx.P
        xT = cx.din("xT", [D, TOKC])
        mod0 = cx.din("mod0", [128, 144])
        gT = cx.din("gT", [128, 3, KC])
        wg = cx.din("wg", [D, DFF])
        wu = cx.din("wu", [D, DFF])
        wd = cx.din("wd", [DFF, D])
        w_in = cx.din("w_in", [D, D_IN_AB])
        hT_out = cx.dout("hT_out", [D, TOKC])
        pT_out = cx.dout("pT_out", [D_IN_AB, TOKC])
        co = Core(cx)
        co.load_h(xT)
        co.load_mod(mod0, gT)
        co.ffn(0, wg, wu, wd)
        co.store_h(hT_out)
        stg = [cx.sb([128, TT], F32, "stg%d" % i) for i in range(2)]
        stg_b = [Buf("stg%d" % i) for i in range(2)]
        cnt = [0]
        for t0 in range(0, TOKC, TT):
            co.adaln_full(1, t0)

            def epi(c, rows, pa, pa_b, t0=t0):
                i = cnt[0] % 2
                cnt[0] += 1
                P.op("act", lambda e: e.activation(out=stg[i][0:rows, :], in_=pa[0:rows, :], func=AF.Copy),
                     reads=[pa_b], writes=[stg_b[i]])
                P.dma("sp", lambda e: e.dma_start(out=pT_out[c * 128:c * 128 + rows, t0:t0 + TT], in_=stg[i][0:rows, :]),
                      reads=[stg_b[i]])
            co.proj(w_in, D_IN_AB, t0, epi)
        cx.finish()
    return nc


HD = 128
C_HEADS = 16
HALO = 512


def att_bias_table(rel_bias):
    s_l = np.arange(128)[:, None, None]
    i = np.arange(5)[None, :, None]
    q_l = np.arange(128)[None, None, :]
    rel = 512 + q_l - 128 * i - s_l
    dchunk = q_l // 64 + 8 - 2 * i - s_l // 64
    ok = (dchunk >= 0) & (dchunk <= 8)
    idx = np.clip(rel, -256, 256) + 256
    tab = rel_bias[:, idx]
    tab = np.where(ok[None], tab, np.float32(-30000.0)).astype(np.float32)
    return np.ascontiguousarray(tab.transpose(1, 0, 2, 3).reshape(128, 16, 640))


def emit_attention(cx, qT, kT, v, biasT, vones, oT, ntok=TOKC):
    P = cx.P
    NQT = ntok // 128
    NKT = NQT + 4
    qb = cx.sb([128, C_HEADS, ntok], BF16, "qb")
    kb = cx.sb([128, C_HEADS, ntok + HALO], BF16, "kb")
    vb = cx.sb([128, NKT, D], BF16, "vb")
    bias = cx.sb([128, C_HEADS, 640], F32, "bias")
    ones = cx.sb([128, 128], BF16, "aones")
    von = cx.sb([128, 128], BF16, "vones")
    qb_b = [Buf("qb%d" % h) for h in range(C_HEADS)]
    kb_b = [Buf("kb%d" % h) for h in range(C_HEADS)]
    vb_b = [Buf("vb%d" % t) for t in range(NKT)]
    bias_b, ones_b, von_b = Buf("bias"), Buf("ones"), Buf("von")
    P.op("dve", lambda e: e.memset(ones[:], 1.0), writes=[ones_b])
    P.dma("pool", lambda e: e.dma_start(out=von[:], in_=vones), writes=[von_b])
    P.dma("sp", lambda e: e.dma_start(out=bias[:], in_=biasT), writes=[bias_b])
    qsrc = qT.rearrange("(h p) t -> p h t", p=128)
    ksrc = kT.rearrange("(h p) t -> p h t", p=128)
    vsrc = v.rearrange("(n p) d -> p n d", p=128)
    for h in range(C_HEADS):
        P.dma("pool", lambda e, h=h: e.dma_start(out=qb[:, h, :], in_=qsrc[:, h, :]), writes=[qb_b[h]])
        P.dma("pool", lambda e, h=h: e.dma_start(out=kb[:, h, :], in_=ksrc[:, h, :]), writes=[kb_b[h]])
    for t in range(NKT):
        P.dma("pool", lambda e, t=t: e.dma_start(out=vb[:, t, :], in_=vsrc[:, t, :]), writes=[vb_b[t]])
    psA = [cx.ps(name="attA%d" % i) for i in range(2)]
    psB = [cx.ps(name="attB%d" % i) for i in range(2)]
    psO = [cx.ps(name="attO%d" % i) for i in range(2)]
    psA_b = [Buf("psA%d" % i) for i in range(2)]
    psB_b = [Buf("psB%d" % i) for i in range(2)]
    psO_b = [Buf("psO%d" % i) for i in range(2)]
    tmp = [cx.sb([128, 640], F32, "atmp%d" % i) for i in range(2)]
    tmp_b = [Buf("atmp%d" % i) for i in range(2)]
    pT = [cx.sb([128, 640], BF16, "apT%d" % i) for i in range(2)]
    pT_b = [Buf("apT%d" % i) for i in range(2)]
    rec = [cx.sb([128, 128], F32, "arec%d" % i) for i in range(2)]
    rec_b = [Buf("arec%d" % i) for i in range(2)]
    ost = [cx.sb([128, ntok], F32, "aost%d" % i) for i in range(2)]
    ost_b = [Buf("aost%d" % i) for i in range(2)]
    odst = oT.rearrange("(h p) t -> p h t", p=128)
    scale = float(HD) ** -0.5
    u = 0
    for h in range(C_HEADS):
        oi = h % 2
        for qt in range(NQT):
            a = u % 2
            u += 1
            qs = slice(qt * 128, (qt + 1) * 128)
            for i in range(5):
                kt = qt + i
                dst = psA[a][:, i * 128:(i + 1) * 128] if i < 4 else psB[a][:, 0:128]
                dst_b = psA_b[a] if i < 4 else psB_b[a]
                P.op("pe", lambda e, dst=dst, h=h, kt=kt, qs=qs: e.matmul(
                    dst, kb[:, h, kt * 128:(kt + 1) * 128], qb[:, h, qs], start=True, stop=True),
                    reads=[kb_b[h], qb_b[h]], writes=[dst_b])
            P.op("dve", lambda e, a=a, h=h: e.scalar_tensor_tensor(
                out=tmp[a][:, 0:512], in0=psA[a][:, 0:512], scalar=scale, in1=bias[:, h, 0:512], op0=ALU.mult, op1=ALU.add),
                reads=[psA_b[a], bias_b], writes=[tmp_b[a]])
            P.op("dve", lambda e, a=a, h=h: e.scalar_tensor_tensor(
                out=tmp[a][:, 512:640], in0=psB[a][:, 0:128], scalar=scale, in1=bias[:, h, 512:640], op0=ALU.mult, op1=ALU.add),
                reads=[psB_b[a], bias_b], writes=[tmp_b[a]])
            P.op("act", lambda e, a=a: e.activation(out=pT[a][:], in_=tmp[a][:], func=AF.Exp),
                 reads=[tmp_b[a]], writes=[pT_b[a]])
            for i in range(5):
                kt = qt + i
                P.op("pe", lambda e, a=a, h=h, kt=kt, i=i: e.matmul(
                    psO[a][:, 0:128], vb[:, kt, h * 128:(h + 1) * 128], pT[a][:, i * 128:(i + 1) * 128],
                    start=(i == 0), stop=(i == 4)), reads=[vb_b[kt], pT_b[a]], writes=[psO_b[a]])
            for i in range(5):
                kt = qt + i
                on = von if kt < 4 else ones
                on_b = von_b if kt < 4 else ones_b
                P.op("pe", lambda e, a=a, on=on, i=i: e.matmul(
                    psO[a][:, 128:256], on[:], pT[a][:, i * 128:(i + 1) * 128],
                    start=(i == 0), stop=(i == 4)), reads=[on_b, pT_b[a]], writes=[psO_b[a]])
            P.op("dve", lambda e, a=a: e.reciprocal(rec[a][:], psO[a][:, 128:256]), reads=[psO_b[a]], writes=[rec_b[a]])
            P.op("dve", lambda e, a=a, oi=oi, qs=qs: e.tensor_tensor(
                out=ost[oi][:, qs], in0=psO[a][:, 0:128], in1=rec[a][:], op=ALU.mult),
                reads=[psO_b[a], rec_b[a]], writes=[ost_b[oi]])
        P.dma("sp", lambda e, h=h, oi=oi: e.dma_start(out=odst[:, h, :], in_=ost[oi][:]), reads=[ost_b[oi]])


def build_ATT():
    import contextlib
    nc = bass.Bass("TRN2", target_bir_lowering=False)
    with contextlib.ExitStack() as st:
        cx = Ctx(nc, st)
        qT = cx.din("qT", [D, TOKC])
        kT = cx.din("kT", [D, TOKC + HALO])
        v = cx.din("v", [TOKC + HALO, D])
        biasT = cx.din("biasT", [128, C_HEADS, 640])
        vones = cx.din("vones", [128, 128])
        oT = cx.dout("oT", [D, TOKC])
        emit_attention(cx, qT, kT, v, biasT, vones, oT)
        cx.finish()
    return nc


class TB:
    def __init__(self, cx, shape, dt, name):
        self.t = cx.sb(shape, dt, name)
        self.b = Buf(name)


S5_LC = 512
S5_NJ = 4


def s5_host_layout(inp, core):
    g0 = core * 8
    lam = np.zeros((128, S5_NJ, 3), np.float32)
    Bre = np.zeros((128, S5_NJ, 128), np.float32)
    Bim = np.zeros((128, S5_NJ, 128), np.float32)
    Cre = np.zeros((128, S5_NJ, 128), np.float32)
    Cim = np.zeros((128, S5_NJ, 128), np.float32)
    for j in range(S5_NJ):
        for gl in range(2):
            g8 = 2 * j + gl
            g = g0 + g8
            sl = slice(gl * 64, gl * 64 + 64)
            lam[sl, j, 0] = inp['s5_lam_re'][0, g]
            lam[sl, j, 1] = inp['s5_lam_im'][0, g]
            lam[sl, j, 2] = inp['s5_log_dt'][0, g]
            Bre[16 * g8:16 * g8 + 16, j, sl] = inp['s5_b_re'][0, g].T
            Bim[16 * g8:16 * g8 + 16, j, sl] = inp['s5_b_im'][0, g].T
            Cre[sl, j, 16 * g8:16 * g8 + 16] = inp['s5_c_re'][0, g].T
            Cim[sl, j, 16 * g8:16 * g8 + 16] = inp['s5_c_im'][0, g].T
    dsk = np.ascontiguousarray(inp['s5_d'][0, core * 128:(core + 1) * 128].reshape(128, 1))
    return {"lam": lam, "Bre": Bre, "Bim": Bim, "Cre": Cre, "Cim": Cim, "dsk": dsk}


def emit_s5(cx, uT, lam, Bre, Bim, Cre, Cim, dsk, yT, L=SEQ):
    P = cx.P
    LC, NJ = S5_LC, S5_NJ
    NCH = L // LC
    u32 = TB(cx, [128, L], F32, "u32")
    ub = TB(cx, [128, L], BF16, "ub")
    for c4 in range(4):
        sl = slice(c4 * (L // 4), (c4 + 1) * (L // 4))
        P.dma("sp", lambda e, sl=sl: e.dma_start(out=u32.t[:, sl], in_=uT[:, sl]), writes=[u32.b])
    for c4 in range(4):
        sl = slice(c4 * (L // 4), (c4 + 1) * (L // 4))
        P.op("act", lambda e, sl=sl: e.activation(out=ub.t[:, sl], in_=u32.t[:, sl], func=AF.Copy), reads=[u32.b], writes=[ub.b])
    lm = TB(cx, [128, NJ, 3], F32, "lam")
    P.dma("sp", lambda e: e.dma_start(out=lm.t[:], in_=lam), writes=[lm.b])
    dk = TB(cx, [128, 1], F32, "dsk")
    P.dma("sp", lambda e: e.dma_start(out=dk.t[:], in_=dsk), writes=[dk.b])
    mats = {}
    for nm, src in (("Bre", Bre), ("Bim", Bim), ("Cre", Cre), ("Cim", Cim)):
        m = TB(cx, [128, NJ, 128], BF16, "m" + nm)
        P.dma("pool", lambda e, m=m, src=src: e.dma_start(out=m.t[:], in_=src), writes=[m.b])
        mats[nm] = m
    P.op("dve", lambda e: e.tensor_scalar(out=mats["Cim"].t[:], in0=mats["Cim"].t[:], scalar1=-1.0, scalar2=None, op0=ALU.mult),
         reads=[mats["Cim"].b], writes=[mats["Cim"].b])
    def sc(name):
        return TB(cx, [128, NJ], F32, name)
    dt, lrdt, th, mag, den, rden = sc("dt"), sc("lrdt"), sc("th"), sc("mag"), sc("den"), sc("rden")
    abre, abim, nr, fre, fim, t1, t2 = sc("abre"), sc("abim"), sc("nr"), sc("fre"), sc("fim"), sc("t1"), sc("t2")
    lr, li, ldt = lm.t[:, :, 0], lm.t[:, :, 1], lm.t[:, :, 2]
    P.op("act", lambda e: e.activation(out=dt.t[:], in_=ldt, func=AF.Exp), reads=[lm.b], writes=[dt.b])
    P.op("dve", lambda e: e.tensor_tensor(out=lrdt.t[:], in0=lr, in1=dt.t[:], op=ALU.mult), reads=[lm.b, dt.b], writes=[lrdt.b])
    P.op("dve", lambda e: e.tensor_tensor(out=th.t[:], in0=li, in1=dt.t[:], op=ALU.mult), reads=[lm.b, dt.b], writes=[th.b])
    P.op("act", lambda e: e.activation(out=mag.t[:], in_=lrdt.t[:], func=AF.Exp), reads=[lrdt.b], writes=[mag.b])
    NLV = 16
    Wre = TB(cx, [128, NLV, NJ], F32, "Wre")
    Wim = TB(cx, [128, NLV, NJ], F32, "Wim")
    hpi = TB(cx, [128, 1], F32, "hpi")
    P.op("dve", lambda e: e.memset(hpi.t[:], float(np.pi / 2)), writes=[hpi.b])
    P.op("act", lambda e: e.activation(out=Wim.t[:, 0, :], in_=th.t[:], func=AF.Sin, scale=1.0 / 64), reads=[th.b], writes=[Wim.b])
    P.op("act", lambda e: e.activation(out=Wre.t[:, 0, :], in_=th.t[:], func=AF.Sin, scale=1.0 / 64, bias=hpi.t[:, 0:1]),
         reads=[th.b, hpi.b], writes=[Wre.b])
    for lv in range(1, NLV):
        a, b_ = Wre.t[:, lv - 1, :], Wim.t[:, lv - 1, :]
        P.op("dve", lambda e, a=a: e.tensor_tensor(out=t1.t[:], in0=a, in1=a, op=ALU.mult), reads=[Wre.b], writes=[t1.b])
        P.op("dve", lambda e, b_=b_: e.tensor_tensor(out=t2.t[:], in0=b_, in1=b_, op=ALU.mult), reads=[Wim.b], writes=[t2.b])
        P.op("dve", lambda e, lv=lv: e.tensor_tensor(out=Wre.t[:, lv, :], in0=t1.t[:], in1=t2.t[:], op=ALU.subtract),
             reads=[t1.b, t2.b], writes=[Wre.b])
        P.op("dve", lambda e, lv=lv, a=a, b_=b_: e.scalar_tensor_tensor(out=Wim.t[:, lv, :], in0=a, scalar=2.0, in1=b_, op0=ALU.mult, op1=ALU.mult),
             reads=[Wre.b, Wim.b], writes=[Wim.b])
    cth, sth = Wre.t[:, 6, :], Wim.t[:, 6, :]
    P.op("dve", lambda e: e.tensor_tensor(out=abre.t[:], in0=mag.t[:], in1=cth, op=ALU.mult), reads=[mag.b, Wre.b], writes=[abre.b])
    P.op("dve", lambda e: e.tensor_tensor(out=abim.t[:], in0=mag.t[:], in1=sth, op=ALU.mult), reads=[mag.b, Wim.b], writes=[abim.b])
    P.op("dve", lambda e: e.tensor_scalar(out=nr.t[:], in0=abre.t[:], scalar1=-1.0, scalar2=None, op0=ALU.add), reads=[abre.b], writes=[nr.b])
    P.op("dve", lambda e: e.tensor_tensor(out=t1.t[:], in0=lr, in1=lr, op=ALU.mult), reads=[lm.b], writes=[t1.b])
    P.op("dve", lambda e: e.tensor_tensor(out=t2.t[:], in0=li, in1=li, op=ALU.mult), reads=[lm.b], writes=[t2.b])
    P.op("dve", lambda e: e.tensor_tensor(out=den.t[:], in0=t1.t[:], in1=t2.t[:], op=ALU.add), reads=[t1.b, t2.b], writes=[den.b])
    P.op("dve", lambda e: e.reciprocal(rden.t[:], den.t[:]), reads=[den.b], writes=[rden.b])
    P.op("dve", lambda e: e.tensor_tensor(out=t1.t[:], in0=nr.t[:], in1=lr, op=ALU.mult), reads=[nr.b, lm.b], writes=[t1.b])
    P.op("dve", lambda e: e.tensor_tensor(out=t2.t[:], in0=abim.t[:], in1=li, op=ALU.mult), reads=[abim.b, lm.b], writes=[t2.b])
    P.op("dve", lambda e: e.tensor_tensor(out=t1.t[:], in0=t1.t[:], in1=t2.t[:], op=ALU.add), reads=[t1.b, t2.b], writes=[t1.b])
    P.op("dve", lambda e: e.tensor_tensor(out=fre.t[:], in0=t1.t[:], in1=rden.t[:], op=ALU.mult), reads=[t1.b, rden.b], writes=[fre.b])
    P.op("dve", lambda e: e.tensor_tensor(out=t1.t[:], in0=abim.t[:], in1=lr, op=ALU.mult), reads=[abim.b, lm.b], writes=[t1.b])
    P.op("dve", lambda e: e.tensor_tensor(out=t2.t[:], in0=nr.t[:], in1=li, op=ALU.mult), reads=[nr.b, lm.b], writes=[t2.b])
    P.op("dve", lambda e: e.tensor_tensor(out=t1.t[:], in0=t1.t[:], in1=t2.t[:], op=ALU.subtract), reads=[t1.b, t2.b], writes=[t1.b])
    P.op("dve", lambda e: e.tensor_tensor(out=fim.t[:], in0=t1.t[:], in1=rden.t[:], op=ALU.mult), reads=[t1.b, rden.b], writes=[fim.b])
    cosT = TB(cx, [128, NJ, LC], F32, "cosT")
    sinT = TB(cx, [128, NJ, LC], F32, "sinT")
    Gre = TB(cx, [128, NJ, LC], F32, "Gre")
    Gim = TB(cx, [128, NJ, LC], F32, "Gim")
    rho = TB(cx, [128, NJ, LC], F32, "rho")
    tw = TB(cx, [128, LC], F32, "tw")
    P.op("pool", lambda e: e.memset(cosT.t[:, :, 0:1], 1.0), writes=[cosT.b])
    P.op("pool", lambda e: e.memset(sinT.t[:, :, 0:1], 0.0), writes=[sinT.b])
    P.op("pool", lambda e: e.memset(rho.t[:], 1.0), writes=[rho.b])
    for j in range(NJ):
        P.op("dve", lambda e, j=j: e.tensor_scalar(out=rho.t[:, j, :], in0=rho.t[:, j, :], scalar1=mag.t[:, j:j + 1], scalar2=None, op0=ALU.mult),
             reads=[rho.b, mag.b], writes=[rho.b])
        m = 1
        lv = 6
        while m < LC:
            wre, wim = Wre.t[:, lv, j:j + 1], Wim.t[:, lv, j:j + 1]
            src_re, src_im = cosT.t[:, j, 0:m], sinT.t[:, j, 0:m]
            P.op("dve", lambda e, m=m, wim=wim, src_im=src_im: e.tensor_scalar(out=tw.t[:, 0:m], in0=src_im, scalar1=wim, scalar2=None, op0=ALU.mult),
                 reads=[sinT.b, Wim.b], writes=[tw.b])
            P.op("dve", lambda e, m=m, j=j, wre=wre, src_re=src_re: e.scalar_tensor_tensor(
                out=cosT.t[:, j, m:2 * m], in0=src_re, scalar=wre, in1=tw.t[:, 0:m], op0=ALU.mult, op1=ALU.subtract),
                reads=[cosT.b, Wre.b, tw.b], writes=[cosT.b])
            P.op("dve", lambda e, m=m, wre=wre, src_im=src_im: e.tensor_scalar(out=tw.t[:, 0:m], in0=src_im, scalar1=wre, scalar2=None, op0=ALU.mult),
                 reads=[sinT.b, Wre.b], writes=[tw.b])
            P.op("dve", lambda e, m=m, j=j, wim=wim, src_re=src_re: e.scalar_tensor_tensor(
                out=sinT.t[:, j, m:2 * m], in0=src_re, scalar=wim, in1=tw.t[:, 0:m], op0=ALU.mult, op1=ALU.add),
                reads=[cosT.b, Wim.b, tw.b], writes=[sinT.b])
            m *= 2
            lv += 1
        P.op("dve", lambda e, j=j: e.tensor_scalar(out=tw.t[:], in0=sinT.t[:, j, :], scalar1=fim.t[:, j:j + 1], scalar2=None, op0=ALU.mult),
             reads=[sinT.b, fim.b], writes=[tw.b])
        P.op("dve", lambda e, j=j: e.scalar_tensor_tensor(out=Gre.t[:, j, :], in0=cosT.t[:, j, :], scalar=fre.t[:, j:j + 1], in1=tw.t[:],
                                                          op0=ALU.mult, op1=ALU.add), reads=[cosT.b, fre.b, tw.b], writes=[Gre.b])
        P.op("dve", lambda e, j=j: e.tensor_scalar(out=tw.t[:], in0=sinT.t[:, j, :], scalar1=fre.t[:, j:j + 1], scalar2=None, op0=ALU.mult),
             reads=[sinT.b, fre.b], writes=[tw.b])
        P.op("dve", lambda e, j=j: e.scalar_tensor_tensor(out=Gim.t[:, j, :], in0=cosT.t[:, j, :], scalar=fim.t[:, j:j + 1], in1=tw.t[:],
                                                          op0=ALU.mult, op1=ALU.subtract), reads=[cosT.b, fim.b, tw.b], writes=[Gim.b])
    Ere, Eim = Wre.t[:, 15, :], Wim.t[:, 15, :]
    NW = 2
    W = []
    for i in range(NW):
        d = {}
        for nm in ("sre", "sim", "a", "b", "a2", "b2", "cre", "cim", "zre", "zim"):
            d[nm] = TB(cx, [128, LC], F32, "%s%d" % (nm, i))
        for nm in ("xre", "xim"):
            d[nm] = TB(cx, [128, LC], BF16, "%s%d" % (nm, i))
        d["pre"] = cx.ps(name="s5pre%d" % i)
        d["pim"] = cx.ps(name="s5pim%d" % i)
        d["pre_b"], d["pim_b"] = Buf("pre"), Buf("pim")
        W.append(d)
    psY = [cx.ps(name="s5y%d" % i) for i in range(2)]
    psY_b = [Buf("psY%d" % i) for i in range(2)]
    init = [[TB(cx, [128, 2], F32, "init%d_%d" % (j, k)) for k in range(2)] for j in range(NJ)]
    for j in range(NJ):
        P.op("pool", lambda e, j=j: e.memset(init[j][0].t[:], 0.0), writes=[init[j][0].b])
    ct = TB(cx, [128, 2], F32, "ct")
    yw = [{nm: TB(cx, [128, LC], F32, "y%s%d" % (nm, i)) for nm in ("y", "y2", "v", "s", "o")} for i in range(2)]
    un = 0
    for ch in range(NCH):
        ts_ = slice(ch * LC, (ch + 1) * LC)
        py, py_b = psY[ch % 2], psY_b[ch % 2]
        for j in range(NJ):
            w = W[un % NW]
            un += 1
            ini, nini = init[j][ch % 2], init[j][(ch + 1) % 2]
            P.op("pe", lambda e, w=w, j=j, ts_=ts_: e.matmul(w["pre"][:], mats["Bre"].t[:, j, :], ub.t[:, ts_], start=True, stop=True),
                 reads=[mats["Bre"].b, ub.b], writes=[w["pre_b"]])
            P.op("pe", lambda e, w=w, j=j, ts_=ts_: e.matmul(w["pim"][:], mats["Bim"].t[:, j, :], ub.t[:, ts_], start=True, stop=True),
                 reads=[mats["Bim"].b, ub.b], writes=[w["pim_b"]])
            P.op("act", lambda e, w=w: e.activation(out=w["sre"].t[:], in_=w["pre"][:], func=AF.Copy), reads=[w["pre_b"]], writes=[w["sre"].b])
            P.op("act", lambda e, w=w: e.activation(out=w["sim"].t[:], in_=w["pim"][:], func=AF.Copy), reads=[w["pim_b"]], writes=[w["sim"].b])
            def tt(eng, out, in0, in1, op):
                P.op(eng, lambda e: e.tensor_tensor(out=out.t[:], in0=in0[0], in1=in1[0], op=op),
                     reads=[in0[1], in1[1]], writes=[out.b])
            gre, gim = (Gre.t[:, j, :], Gre.b), (Gim.t[:, j, :], Gim.b)
            cs, sn = (cosT.t[:, j, :], cosT.b), (sinT.t[:, j, :], sinT.b)
            S = lambda x: (x.t[:], x.b)
            tt("pool", w["a2"], gre, S(w["sre"]), ALU.mult)
            tt("pool", w["b2"], gim, S(w["sim"]), ALU.mult)
            tt("pool", w["cre"], S(w["a2"]), S(w["b2"]), ALU.subtract)
            tt("pool", w["a2"], gre, S(w["sim"]), ALU.mult)
            tt("pool", w["b2"], gim, S(w["sre"]), ALU.mult)
            tt("pool", w["cim"], S(w["a2"]), S(w["b2"]), ALU.add)
            P.op("dve", lambda e, w=w, j=j, ini=ini: e.tensor_tensor_scan(w["zre"].t[:], rho.t[:, j, :], w["cre"].t[:], ini.t[:, 0:1], ALU.mult, ALU.add),
                 reads=[rho.b, w["cre"].b, ini.b], writes=[w["zre"].b])
            P.op("dve", lambda e, w=w, j=j, ini=ini: e.tensor_tensor_scan(w["zim"].t[:], rho.t[:, j, :], w["cim"].t[:], ini.t[:, 1:2], ALU.mult, ALU.add),
                 reads=[rho.b, w["cim"].b, ini.b], writes=[w["zim"].b])
            zre_e, zim_e = w["zre"].t[:, LC - 1:LC], w["zim"].t[:, LC - 1:LC]
            ere, eim = Ere[:, j:j + 1], Eim[:, j:j + 1]
            P.op("dve", lambda e, zim_e=zim_e, eim=eim: e.tensor_scalar(out=ct.t[:, 0:1], in0=zim_e, scalar1=eim, scalar2=None, op0=ALU.mult),
                 reads=[w["zim"].b, Wim.b], writes=[ct.b])
            P.op("dve", lambda e, zre_e=zre_e, ere=ere, nini=nini: e.scalar_tensor_tensor(
                out=nini.t[:, 0:1], in0=zre_e, scalar=ere, in1=ct.t[:, 0:1], op0=ALU.mult, op1=ALU.subtract),
                reads=[w["zre"].b, Wre.b, ct.b], writes=[nini.b])
            P.op("dve", lambda e, zre_e=zre_e, eim=eim: e.tensor_scalar(out=ct.t[:, 1:2], in0=zre_e, scalar1=eim, scalar2=None, op0=ALU.mult),
                 reads=[w["zre"].b, Wim.b], writes=[ct.b])
            P.op("dve", lambda e, zim_e=zim_e, ere=ere, nini=nini: e.scalar_tensor_tensor(
                out=nini.t[:, 1:2], in0=zim_e, scalar=ere, in1=ct.t[:, 1:2], op0=ALU.mult, op1=ALU.add),
                reads=[w["zim"].b, Wre.b, ct.b], writes=[nini.b])
            tt("dve", w["a"], cs, S(w["zre"]), ALU.mult)
            tt("dve", w["b"], sn, S(w["zim"]), ALU.mult)
            tt("dve", w["xre"], S(w["a"]), S(w["b"]), ALU.subtract)
            tt("dve", w["a"], sn, S(w["zre"]), ALU.mult)
            tt("dve", w["b"], cs, S(w["zim"]), ALU.mult)
            tt("dve", w["xim"], S(w["a"]), S(w["b"]), ALU.add)
            P.op("pe", lambda e, w=w, j=j, py=py: e.matmul(py[:], mats["Cre"].t[:, j, :], w["xre"].t[:], start=(j == 0), stop=False),
                 reads=[mats["Cre"].b, w["xre"].b], writes=[py_b])
            P.op("pe", lambda e, w=w, j=j, py=py: e.matmul(py[:], mats["Cim"].t[:, j, :], w["xim"].t[:], start=False, stop=(j == NJ - 1)),
                 reads=[mats["Cim"].b, w["xim"].b], writes=[py_b])
        Y = yw[ch % 2]
        P.op("dve", lambda e, Y=Y, py=py, ts_=ts_: e.scalar_tensor_tensor(out=Y["y"].t[:], in0=u32.t[:, ts_], scalar=dk.t[:, 0:1], in1=py[:],
                                                                         op0=ALU.mult, op1=ALU.add), reads=[u32.b, dk.b, py_b], writes=[Y["y"].b])
        P.op("pool", lambda e, Y=Y: e.tensor_tensor(out=Y["y2"].t[:], in0=Y["y"].t[:], in1=Y["y"].t[:], op=ALU.mult), reads=[Y["y"].b], writes=[Y["y2"].b])
        P.op("pool", lambda e, Y=Y: e.tensor_scalar(out=Y["y2"].t[:], in0=Y["y2"].t[:], scalar1=0.044715, scalar2=1.0, op0=ALU.mult, op1=ALU.add),
             reads=[Y["y2"].b], writes=[Y["y2"].b])
        P.op("pool", lambda e, Y=Y: e.tensor_tensor(out=Y["v"].t[:], in0=Y["y2"].t[:], in1=Y["y"].t[:], op=ALU.mult), reads=[Y["y2"].b, Y["y"].b], writes=[Y["v"].b])
        P.op("act", lambda e, Y=Y: e.activation(out=Y["s"].t[:], in_=Y["v"].t[:], func=AF.Sigmoid, scale=1.5957691216057308),
             reads=[Y["v"].b], writes=[Y["s"].b])
        P.op("pool", lambda e, Y=Y: e.tensor_tensor(out=Y["o"].t[:], in0=Y["s"].t[:], in1=Y["y"].t[:], op=ALU.mult), reads=[Y["s"].b, Y["y"].b], writes=[Y["o"].b])
        P.dma("sp", lambda e, Y=Y, ts_=ts_: e.dma_start(out=yT[:, ts_], in_=Y["o"].t[:]), reads=[Y["o"].b])


def build_S5():
    import contextlib
    nc = bass.Bass("TRN2", target_bir_lowering=False)
    with contextlib.ExitStack() as st:
        cx = Ctx(nc, st)
        uT = cx.din("uT", [128, SEQ])
        lam = cx.din("lam", [128, S5_NJ, 3])
        Bre = cx.din("Bre", [128, S5_NJ, 128])
        Bim = cx.din("Bim", [128, S5_NJ, 128])
        Cre = cx.din("Cre", [128, S5_NJ, 128])
        Cim = cx.din("Cim", [128, S5_NJ, 128])
        dsk = cx.din("dsk", [128, 1])
        yT = cx.dout("yT", [128, SEQ])
        emit_s5(cx, uT, lam, Bre, Bim, Cre, Cim, dsk, yT)
        cx.finish()
    return nc


NSLOT = 8
TOPK = 256
NBIS = 20


def slot_tiles(i):
    return 8 * (i + 1)


def slot_off(i):
    return 4 * i * (i + 1)


NT_TOTAL = slot_off(NSLOT)


def dsa_negpos(core):
    return (np.arange(SEQ)[None, :] - 128 * core - np.arange(128)[:, None]).astype(np.float32)


def dsa_adm_mask(core):
    r = np.arange(1024)[None, :]
    ql = np.arange(128)[:, None]
    kch = r // 64
    qch = (128 * core + ql) // 64
    return np.where(kch <= qch, 0.0, -1e30).astype(np.float32)


DBG = {"nslot": 8, "bis": NBIS, "tr": True, "idx": True}


def emit_dsa_index(cx, kidxT, qidxT, widx, adm, pow2, ident, selT_out, negpos=None, dmin_out=None):
    P = cx.P
    S = SEQ
    kb = TB(cx, [64, S], BF16, "ikb")
    qb = TB(cx, [64, 16, NSLOT * 128], BF16, "iqb")
    for c4 in range(4):
        sl = slice(c4 * 2048, (c4 + 1) * 2048)
        P.dma("pool", lambda e, sl=sl: e.dma_start(out=kb.t[:, sl], in_=kidxT[:, sl]), writes=[kb.b])
    qsrc = qidxT.rearrange("(h d) t -> d h t", d=64)
    for h4 in range(4):
        P.dma("pool", lambda e, h4=h4: e.dma_start(out=qb.t[:, h4 * 4:(h4 + 1) * 4, :], in_=qsrc[:, h4 * 4:(h4 + 1) * 4, :]), writes=[qb.b])
    w = TB(cx, [128, NSLOT, 16], F32, "iw")
    wa = TB(cx, [128, NSLOT, 16], F32, "iwa")
    wsg = TB(cx, [128, NSLOT, 16], F32, "iws")
    am = TB(cx, [128, 1024], F32, "iadm")
    p2 = TB(cx, [128, NBIS + 1], F32, "ip2")
    idf = TB(cx, [128, 128], F32, "iidf")
    idb = TB(cx, [128, 128], BF16, "iidb")
    P.dma("sp", lambda e: e.dma_start(out=w.t[:], in_=widx), writes=[w.b])
    P.dma("sp", lambda e: e.dma_start(out=am.t[:], in_=adm), writes=[am.b])
    P.dma("sp", lambda e: e.dma_start(out=p2.t[:], in_=pow2), writes=[p2.b])
    P.dma("sp", lambda e: e.dma_start(out=idf.t[:], in_=ident), writes=[idf.b])
    P.op("dve", lambda e: e.tensor_copy(idb.t[:], idf.t[:]), reads=[idf.b], writes=[idb.b])
    P.op("act", lambda e: e.activation(out=wa.t[:], in_=w.t[:], func=AF.Abs), reads=[w.b], writes=[wa.b])
    P.op("dve", lambda e: e.tensor_scalar(out=wsg.t[:], in0=w.t[:], scalar1=0.0, scalar2=2.0, op0=ALU.is_ge, op1=ALU.mult),
         reads=[w.b], writes=[wsg.b])
    P.op("dve", lambda e: e.tensor_scalar(out=wsg.t[:], in0=wsg.t[:], scalar1=-1.0, scalar2=None, op0=ALU.add), reads=[wsg.b], writes=[wsg.b])
    sc = TB(cx, [128, S], F32, "isc")
    jk = TB(cx, [128, S], BF16, "ijk")
    rr = [TB(cx, [128, 512], F32, "irr%d" % i) for i in range(3)]
    ps = [cx.ps(name="ips%d" % i) for i in range(4)]
    ps_b = [Buf("ips%d" % i) for i in range(4)]
    pst = [cx.ps([128, 512], BF16, name="ipt%d" % i) for i in range(2)]
    pst_b = [Buf("ipt%d" % i) for i in range(2)]
    stg = [TB(cx, [128, 4, 128], BF16, "istg%d" % i) for i in range(2)]
    M = TB(cx, [128, 1], F32, "iM")
    steps = TB(cx, [128, NBIS + 1], F32, "isteps")
    nsteps = TB(cx, [128, NBIS + 1], F32, "insteps")
    mid = [TB(cx, [128, 1], F32, "imid%d" % i) for i in range(2)]
    cnt = TB(cx, [128, 1], F32, "icnt")
    dd = TB(cx, [128, 1], F32, "idd")
    n = 0
    nt = 0
    for i in range(DBG["nslot"]):
        Si = 1024 * (i + 1)
        qs = slice(i * 128, (i + 1) * 128)
        for kbk in range(Si // 512 if DBG["idx"] else 0):
            ks = slice(kbk * 512, (kbk + 1) * 512)
            for h in range(16):
                pp, pp_b = ps[n % 4], ps_b[n % 4]
                r = rr[n % 3]
                n += 1
                P.op("pe", lambda e, pp=pp, h=h, qs=qs, ks=ks: e.matmul(pp[:], qb.t[:, h, qs], kb.t[:, ks], start=True, stop=True),
                     reads=[qb.b, kb.b], writes=[pp_b])
                P.op("act", lambda e, pp=pp, r=r, i=i, h=h: e.activation(out=r.t[:], in_=pp[:], func=AF.Relu, scale=wa.t[:, i, h:h + 1]),
                     reads=[pp_b, wa.b], writes=[r.b])
                if h == 0:
                    P.op("dve", lambda e, r=r, i=i, h=h, ks=ks: e.tensor_scalar(out=sc.t[:, ks], in0=r.t[:], scalar1=wsg.t[:, i, h:h + 1], scalar2=None, op0=ALU.mult),
                         reads=[r.b, wsg.b], writes=[sc.b])
                else:
                    P.op("dve", lambda e, r=r, i=i, h=h, ks=ks: e.scalar_tensor_tensor(out=sc.t[:, ks], in0=r.t[:], scalar=wsg.t[:, i, h:h + 1], in1=sc.t[:, ks],
                                                                                       op0=ALU.mult, op1=ALU.add), reads=[r.b, wsg.b, sc.b], writes=[sc.b])
        P.op("dve", lambda e, Si=Si: e.tensor_reduce(out=M.t[:], in_=sc.t[:, 0:Si], axis=AX.X, op=ALU.max, apply_absolute_value=True), reads=[sc.b], writes=[M.b])
        P.op("dve", lambda e: e.tensor_scalar(out=M.t[:], in0=M.t[:], scalar1=1.001, scalar2=1e-20, op0=ALU.mult, op1=ALU.add), reads=[M.b], writes=[M.b])
        P.op("dve", lambda e, Si=Si: e.tensor_tensor(out=sc.t[:, Si - 1024:Si], in0=sc.t[:, Si - 1024:Si], in1=am.t[:], op=ALU.add),
             reads=[sc.b, am.b], writes=[sc.b])
        P.op("dve", lambda e: e.tensor_scalar(out=steps.t[:], in0=p2.t[:], scalar1=M.t[:, 0:1], scalar2=None, op0=ALU.mult), reads=[p2.b, M.b], writes=[steps.b])
        P.op("dve", lambda e: e.tensor_scalar(out=nsteps.t[:], in0=steps.t[:], scalar1=-1.0, scalar2=None, op0=ALU.mult), reads=[steps.b], writes=[nsteps.b])
        P.op("dve", lambda e: e.memset(mid[0].t[:], 0.0), writes=[mid[0].b])
        for k in range(DBG["bis"]):
            m0, m1 = mid[k % 2], mid[(k + 1) % 2]
            P.op("dve", lambda e, Si=Si, m0=m0: e.tensor_scalar(out=jk.t[:, 0:Si], in0=sc.t[:, 0:Si], scalar1=m0.t[:, 0:1], scalar2=0.0,
                                                              op0=ALU.is_gt, op1=ALU.add, accum_out=cnt.t[:, 0:1]),
                 reads=[sc.b, m0.b], writes=[jk.b, cnt.b])
            P.op("dve", lambda e, k=k: e.tensor_scalar(out=dd.t[:], in0=cnt.t[:], scalar1=float(TOPK), scalar2=steps.t[:, k:k + 1],
                                                       op0=ALU.is_ge, op1=ALU.mult), reads=[cnt.b, steps.b], writes=[dd.b])
            P.op("dve", lambda e, k=k, m0=m0, m1=m1: e.scalar_tensor_tensor(out=m1.t[:], in0=dd.t[:], scalar=nsteps.t[:, k + 1:k + 2], in1=m0.t[:],
                                                                          op0=ALU.add, op1=ALU.add), reads=[dd.b, nsteps.b, m0.b], writes=[m1.b])
        mf = mid[DBG["bis"] % 2]
        P.op("dve", lambda e, Si=Si, mf=mf: e.tensor_scalar(out=jk.t[:, 0:Si], in0=sc.t[:, 0:Si], scalar1=mf.t[:, 0:1], scalar2=-30000.0,
                                                          op0=ALU.is_le, op1=ALU.mult), reads=[sc.b, mf.b], writes=[jk.b])
        if negpos is not None:
            if i == 0:
                npos = TB(cx, [128, S], F32, "inpos")
                P.dma("sp", lambda e: e.dma_start(out=npos.t[:], in_=negpos), writes=[npos.b])
                offs = TB(cx, [128, NSLOT], F32, "ioffs")
                for ii in range(NSLOT):
                    P.op("pool", lambda e, ii=ii: e.memset(offs.t[:, ii:ii + 1], -1024.0 * ii), writes=[offs.b])
                dm = TB(cx, [128, NSLOT], F32, "idmin")
                emit_dsa_index._st = (npos, offs, dm)
            npos, offs, dm = emit_dsa_index._st
            P.op("act", lambda e, Si=Si, i=i: e.activation(out=sc.t[:, 0:Si], in_=npos.t[:, 0:Si], func=AF.Abs, bias=offs.t[:, i:i + 1], scale=1.0),
                 reads=[npos.b, offs.b, sc.b], writes=[sc.b])
            P.op("pool", lambda e, Si=Si: e.tensor_tensor(out=sc.t[:, 0:Si], in0=jk.t[:, 0:Si], in1=sc.t[:, 0:Si], op=ALU.subtract),
                 reads=[jk.b, sc.b], writes=[sc.b])
            P.op("dve", lambda e, Si=Si, i=i: e.tensor_reduce(out=dm.t[:, i:i + 1], in_=sc.t[:, 0:Si], axis=AX.X, op=ALU.max),
                 reads=[sc.b], writes=[dm.b])
            if i == DBG["nslot"] - 1:
                P.dma("sp", lambda e: e.dma_start(out=dmin_out, in_=dm.t[:]), reads=[dm.b])
        for g in range(Si // 512 if DBG["tr"] else 0):
            pt, pt_b = pst[nt % 2], pst_b[nt % 2]
            sg = stg[nt % 2]
            nt += 1
            for j in range(4):
                tix = g * 4 + j
                P.op("pe", lambda e, pt=pt, j=j, tix=tix: e.transpose(pt[:, j * 128:(j + 1) * 128], jk.t[:, tix * 128:(tix + 1) * 128], idb.t[:]),
                     reads=[jk.b, idb.b], writes=[pt_b])
            P.op("act", lambda e, pt=pt, sg=sg: e.activation(out=sg.t[:].rearrange("p a b -> p (a b)"), in_=pt[:], func=AF.Copy),
                 reads=[pt_b], writes=[sg.b])
            t0 = slot_off(i) + g * 4
            P.dma("sp", lambda e, sg=sg, t0=t0: e.dma_start(out=selT_out[:, t0:t0 + 4, :], in_=sg.t[:]), reads=[sg.b])


def build_C1():
    import contextlib
    nc = bass.Bass("TRN2", target_bir_lowering=False)
    with contextlib.ExitStack() as st:
        cx = Ctx(nc, st)
        kidxT = cx.din("kidxT", [64, SEQ])
        qidxT = cx.din("qidxT", [1024, NSLOT * 128])
        widx = cx.din("widx", [128, NSLOT, 16])
        adm = cx.din("adm", [128, 1024])
        pow2 = cx.din("pow2", [128, NBIS + 1])
        ident = cx.din("ident", [128, 128])
        selT = cx.dout("selT", [128, NT_TOTAL, 128], BF16)
        negpos = cx.din("negpos", [128, SEQ])
        dmin = cx.dout("ndmin", [128, NSLOT])
        emit_dsa_index(cx, kidxT, qidxT, widx, adm, pow2, ident, selT, negpos, dmin)
        cx.finish()
    return nc


B_HEADS = 8
KV_RANK = 256


def dsa_alibi_tables(core):
    slopes = (2.0 ** (-8.0 * (np.arange(8) + 1) / 8)).astype(np.float32)
    s_l = np.arange(128)[:, None, None]
    idx = np.arange(64)[None, None, :]
    rel = s_l + 128 * (idx - 56 - core) - 64
    biascol = (slopes[None, :, None] * np.minimum(rel, 63)).astype(np.float32)
    corr = np.zeros((128, 8, 8, 128), np.float32)
    sl = np.arange(128)[:, None]
    ql = np.arange(128)[None, :]
    corr[:, core, :, :] = -2.0 * slopes[None, :, None] * np.maximum(sl - ql, 0)[:, None, :]
    return biascol, corr


def emit_dsa_attn(cx, kvT, kvg, wkv, qT, selT, biascol, corr, ident, ybT, ndminT=None, qoff=None):
    P = cx.P
    slopes = [2.0 ** (-8.0 * (h + 1) / 8) for h in range(B_HEADS)]
    nd = TB(cx, [1, NSLOT * 128], F32, "dnd")
    qo = TB(cx, [1, 128], F32, "dqo")
    rrow = TB(cx, [1, B_HEADS, NSLOT * 128], BF16, "drrow")
    ones1 = TB(cx, [1, 128], BF16, "dones1")
    P.dma("sp", lambda e: e.dma_start(out=nd.t[:], in_=ndminT), writes=[nd.b])
    P.dma("sp", lambda e: e.dma_start(out=qo.t[:], in_=qoff), writes=[qo.b])
    P.op("pool", lambda e: e.memset(ones1.t[:], 1.0), writes=[ones1.b])
    for i in range(NSLOT):
        P.op("pool", lambda e, i=i: e.tensor_tensor(out=nd.t[:, i * 128:(i + 1) * 128], in0=qo.t[:], in1=nd.t[:, i * 128:(i + 1) * 128], op=ALU.subtract),
             reads=[qo.b, nd.b], writes=[nd.b])
    for h in range(B_HEADS):
        P.op("pool", lambda e, h=h: e.tensor_scalar(out=rrow.t[:, h, :], in0=nd.t[:], scalar1=float(slopes[h]), scalar2=None, op0=ALU.mult),
             reads=[nd.b], writes=[rrow.b])
    S = SEQ
    NKB = S // 512
    kvn = TB(cx, [128, 2, S], BF16, "dkvn")
    Wb = TB(cx, [128, 2, 2048], BF16, "dW")
    P.dma("pool", lambda e: e.dma_start(out=Wb.t[:], in_=wkv.rearrange("(k p) n -> p k n", p=128)), writes=[Wb.b])
    g = TB(cx, [128, 2], F32, "dg")
    P.dma("sp", lambda e: e.dma_start(out=g.t[:], in_=kvg), writes=[g.b])
    bc = TB(cx, [128, B_HEADS, 64], F32, "dbc")
    P.dma("sp", lambda e: e.dma_start(out=bc.t[:], in_=biascol), writes=[bc.b])
    cr = TB(cx, [128, 8, B_HEADS, 128], BF16, "dcorr")
    P.dma("pool", lambda e: e.dma_start(out=cr.t[:], in_=corr), writes=[cr.b])
    idb = TB(cx, [128, 128], BF16, "didb")
    P.dma("pool", lambda e: e.dma_start(out=idb.t[:], in_=ident), writes=[idb.b])
    ones = TB(cx, [128, 128], BF16, "dones")
    P.op("dve", lambda e: e.memset(ones.t[:], 1.0), writes=[ones.b])
    qb = TB(cx, [128, B_HEADS, NSLOT * 128], BF16, "dqb")
    P.dma("pool", lambda e: e.dma_start(out=qb.t[:], in_=qT.rearrange("(h p) t -> p h t", p=128)), writes=[qb.b])
    P.op("act", lambda e: e.activation(out=qb.t[:], in_=qb.t[:], func=AF.Identity, scale=float(HD) ** -0.5), reads=[qb.b], writes=[qb.b])
    kv32 = [TB(cx, [128, 2, 512], F32, "dkv32_%d" % i) for i in range(2)]
    sq = [TB(cx, [128, 512], BF16, "dsq%d" % i) for i in range(2)]
    rt = [TB(cx, [128, 512], F32, "drt%d" % i) for i in range(2)]
    pkv = [cx.ps(name="dpkv%d" % i) for i in range(2)]
    pkv_b = [Buf("dpkv%d" % i) for i in range(2)]
    pst, pst_b = pkv[0], pkv_b[0]
    kvsrc = kvT.rearrange("(k p) t -> p k t", p=128)
    for blk in range(NKB):
        bs = slice(blk * 512, (blk + 1) * 512)
        kv = kv32[blk % 2]
        r = rt[blk % 2]
        P.dma("sp", lambda e, kv=kv, bs=bs: e.dma_start(out=kv.t[:], in_=kvsrc[:, :, bs]), writes=[kv.b])
        for kc in range(2):
            s_ = sq[kc]
            P.op("act", lambda e, kv=kv, kc=kc, s_=s_: e.activation(out=s_.t[:], in_=kv.t[:, kc, :], func=AF.Square), reads=[kv.b], writes=[s_.b])
            P.op("pe", lambda e, kc=kc, s_=s_: e.matmul(pst[:], ones.t[:], s_.t[:], start=(kc == 0), stop=(kc == 1)), reads=[ones.b, s_.b], writes=[pst_b])
        P.op("dve", lambda e, r=r: e.tensor_scalar(out=r.t[:], in0=pst[:], scalar1=1.0 / KV_RANK, scalar2=EPS, op0=ALU.mult, op1=ALU.add), reads=[pst_b], writes=[r.b])
        P.op("act", lambda e, r=r: e.activation(out=r.t[:], in_=r.t[:], func=AF.Sqrt), reads=[r.b], writes=[r.b])
        P.op("dve", lambda e, r=r: e.reciprocal(r.t[:], r.t[:]), reads=[r.b], writes=[r.b])
        for kc in range(2):
            P.op("dve", lambda e, kv=kv, kc=kc, r=r, bs=bs: e.scalar_tensor_tensor(out=kvn.t[:, kc, bs], in0=kv.t[:, kc, :], scalar=g.t[:, kc:kc + 1], in1=r.t[:],
                                                                                   op0=ALU.mult, op1=ALU.mult), reads=[kv.b, g.b, r.b], writes=[kvn.b])
    Kh = TB(cx, [128, S], BF16, "dKh")
    Vh = TB(cx, [128, S // 128, 128], BF16, "dVh")
    pL = [cx.ps(name="dpL%d" % i) for i in range(2)]
    pL_b = [Buf("dpL%d" % i) for i in range(2)]
    pOD = [cx.ps(name="dpOD%d" % i) for i in range(2)]
    pOD_b = [Buf("dpOD%d" % i) for i in range(2)]
    pDD = [cx.ps(name="dpDD%d" % i) for i in range(2)]
    pDD_b = [Buf("dpDD%d" % i) for i in range(2)]
    selb = [TB(cx, [128, 64, 128], BF16, "dsel%d" % i) for i in range(2)]
    eL = [TB(cx, [128, 512], BF16, "deL%d" % i) for i in range(2)]
    pT = [TB(cx, [128, 512], BF16, "dpT%d" % i) for i in range(2)]
    rec = [TB(cx, [128, 128], F32, "drec%d" % i) for i in range(2)]
    ost = [TB(cx, [128, NSLOT * 128], F32, "dost%d" % i) for i in range(2)]
    odst = ybT.rearrange("(h p) t -> p h t", p=128)
    n = 0
    nsel = 0
    ng = 0
    nod = 0
    for h in range(B_HEADS):
        for blk in range(NKB):
            bs = slice(blk * 512, (blk + 1) * 512)
            pp, pp_b = pkv[n % 2], pkv_b[n % 2]
            n += 1
            for kc in range(2):
                P.op("pe", lambda e, pp=pp, kc=kc, h=h, bs=bs: e.matmul(pp[:], Wb.t[:, kc, h * 128:(h + 1) * 128], kvn.t[:, kc, bs], start=(kc == 0), stop=(kc == 1)),
                     reads=[Wb.b, kvn.b], writes=[pp_b])
            P.op("act", lambda e, pp=pp, bs=bs: e.activation(out=Kh.t[:, bs], in_=pp[:], func=AF.Copy), reads=[pp_b], writes=[Kh.b])
        for g4 in range(S // 512):
            pp, pp_b = pkv[n % 2], pkv_b[n % 2]
            n += 1
            for j in range(4):
                tl = g4 * 4 + j
                for kc in range(2):
                    P.op("pe", lambda e, pp=pp, kc=kc, h=h, tl=tl, j=j: e.matmul(
                        pp[:, j * 128:(j + 1) * 128], kvn.t[:, kc, tl * 128:(tl + 1) * 128], Wb.t[:, kc, 1024 + h * 128:1024 + (h + 1) * 128],
                        start=(kc == 0), stop=(kc == 1)), reads=[Wb.b, kvn.b], writes=[pp_b])
            P.op("dve", lambda e, pp=pp, g4=g4: e.tensor_copy(Vh.t[:, g4 * 4:(g4 + 1) * 4, :].rearrange("p a b -> p (a b)"), pp[:]), reads=[pp_b], writes=[Vh.b])
        oh = ost[h % 2]
        for i in range(NSLOT):
            ntile = slot_tiles(i)
            sb_ = selb[nsel % 2]
            nsel += 1
            o0 = slot_off(i)
            P.dma("sp", lambda e, sb_=sb_, o0=o0, ntile=ntile: e.dma_start(out=sb_.t[:, 0:ntile, :], in_=selT[:, o0:o0 + ntile, :]), writes=[sb_.b])
            od, od_b = pOD[nod % 2], pOD_b[nod % 2]
            dn, dn_b = pDD[nod % 2], pDD_b[nod % 2]
            rc = rec[nod % 2]
            nod += 1
            qs = slice(i * 128, (i + 1) * 128)
            for g4 in range(ntile // 4):
                pl, pl_b = pL[ng % 2], pL_b[ng % 2]
                el, pt = eL[ng % 2], pT[ng % 2]
                ng += 1
                for j in range(4):
                    tl = g4 * 4 + j
                    r = tl - 8 * i
                    P.op("pe", lambda e, pl=pl, j=j, tl=tl, h=h, qs=qs, r=r: e.matmul(
                        pl[:, j * 128:(j + 1) * 128], Kh.t[:, tl * 128:(tl + 1) * 128], qb.t[:, h, qs], start=True, stop=False),
                        reads=[Kh.b, qb.b], writes=[pl_b])
                    if r >= 0:
                        P.op("pe", lambda e, pl=pl, j=j, r=r, h=h: e.matmul(
                            pl[:, j * 128:(j + 1) * 128], idb.t[:], cr.t[:, r, h, :], start=False, stop=False),
                            reads=[idb.b, cr.b], writes=[pl_b])
                    P.op("pe", lambda e, pl=pl, j=j, h=h, qs=qs: e.matmul(
                        pl[:, j * 128:(j + 1) * 128], ones1.t[:], rrow.t[:, h, qs], start=False, stop=False),
                        reads=[ones1.b, rrow.b], writes=[pl_b])
                    P.op("pe", lambda e, pl=pl, j=j, tl=tl, sb_=sb_: e.matmul(
                        pl[:, j * 128:(j + 1) * 128], idb.t[:], sb_.t[:, tl, :], start=False, stop=True),
                        reads=[idb.b, sb_.b], writes=[pl_b])
                for j in range(4):
                    tl = g4 * 4 + j
                    ix = tl - 8 * i + 56
                    P.op("act", lambda e, pl=pl, pt=pt, j=j, h=h, ix=ix: e.activation(
                        out=pt.t[:, j * 128:(j + 1) * 128], in_=pl[:, j * 128:(j + 1) * 128], func=AF.Exp, bias=bc.t[:, h, ix:ix + 1], scale=1.0),
                        reads=[pl_b, bc.b], writes=[pt.b])
                for j in range(4):
                    tl = g4 * 4 + j
                    P.op("pe", lambda e, od=od, pt=pt, j=j, tl=tl, ntile=ntile: e.matmul(
                        od[:, 0:128], Vh.t[:, tl, :], pt.t[:, j * 128:(j + 1) * 128], start=(tl == 0), stop=(tl == ntile - 1)),
                        reads=[Vh.b, pt.b], writes=[od_b])
                for j in range(4):
                    tl = g4 * 4 + j
                    P.op("pe", lambda e, dn=dn, pt=pt, j=j, tl=tl, ntile=ntile: e.matmul(
                        dn[:, 0:128], ones.t[:], pt.t[:, j * 128:(j + 1) * 128], start=(tl == 0), stop=(tl == ntile - 1)),
                        reads=[ones.b, pt.b], writes=[dn_b])
            P.op("dve", lambda e, dn=dn, rc=rc: e.reciprocal(rc.t[:], dn[:, 0:128]), reads=[dn_b], writes=[rc.b])
            P.op("dve", lambda e, od=od, rc=rc, oh=oh, qs=qs: e.tensor_tensor(out=oh.t[:, qs], in0=od[:, 0:128], in1=rc.t[:], op=ALU.mult),
                 reads=[od_b, rc.b], writes=[oh.b])
        P.dma("sp", lambda e, h=h, oh=oh: e.dma_start(out=odst[:, h, :], in_=oh.t[:]), reads=[oh.b])


def build_C2():
    import contextlib
    nc = bass.Bass("TRN2", target_bir_lowering=False)
    with contextlib.ExitStack() as st:
        cx = Ctx(nc, st)
        kvT = cx.din("kvT", [KV_RANK, SEQ])
        kvg = cx.din("kvg", [128, 2])
        wkv = cx.din("wkv", [KV_RANK, 2048])
        qT = cx.din("qT", [1024, NSLOT * 128])
        selT = cx.din("selT", [128, NT_TOTAL, 128], BF16)
        biascol = cx.din("biascol", [128, B_HEADS, 64])
        corr = cx.din("corr", [128, 8, B_HEADS, 128])
        ident = cx.din("ident", [128, 128])
        ybT = cx.dout("ybT", [1024, NSLOT * 128])
        ndminT = cx.din("ndminT", [1, NSLOT * 128])
        qoff = cx.din("qoff", [1, 128])
        emit_dsa_attn(cx, kvT, kvg, wkv, qT, selT, biascol, corr, ident, ybT, ndminT, qoff)
        cx.finish()
    return nc


def build_D0A1():
    import contextlib
    nc = bass.Bass("TRN2", target_bir_lowering=False)
    with contextlib.ExitStack() as st:
        cx = Ctx(nc, st)
        P = cx.P
        hT = cx.din("hT", [D, TOKC])
        mod0 = cx.din("mod0", [128, 144])
        gT0 = cx.din("gT0", [128, 3, KC])
        yaT = cx.din("yaT", [1024, TOKC])
        ybT = cx.din("ybT", [1024, TOKC])
        w_glu = cx.din("w_glu", [1024, 1024])
        bgT = cx.din("bgT", [128, 8])
        w_out = cx.din("w_out", [D, D])
        wg0 = cx.din("wg0", [D, DFF])
        wu0 = cx.din("wu0", [D, DFF])
        wd0 = cx.din("wd0", [DFF, D])
        mod1 = cx.din("mod1", [128, 144])
        gT1 = cx.din("gT1", [128, 3, KC])
        wg1 = cx.din("wg1", [D, DFF])
        wu1 = cx.din("wu1", [D, DFF])
        wd1 = cx.din("wd1", [DFF, D])
        w_qkv = cx.din("w_qkv", [D, 3 * D])
        hT_out = cx.dout("hT_out", [D, TOKC])
        qkvT = cx.dout("qkvT", [3 * D, TOKC])
        co = Core(cx)
        co.load_h(hT)
        co.load_mod(mod0, gT0)
        co.mixer0_out(yaT, ybT, w_glu, bgT, w_out)
        co.ffn(2, wg0, wu0, wd0)
        co.load_mod(mod1, gT1)
        co.ffn(0, wg1, wu1, wd1)
        co.store_h(hT_out)
        for t0 in range(0, TOKC, TT):
            co.adaln_full(1, t0)
            co.proj(w_qkv, 3 * D, t0, co.out_epilogue(qkvT, t0))
        cx.finish()
    return nc


def build_D1():
    import contextlib
    nc = bass.Bass("TRN2", target_bir_lowering=False)
    with contextlib.ExitStack() as st:
        cx = Ctx(nc, st)
        hT = cx.din("hT", [D, TOKC])
        mod1 = cx.din("mod1", [128, 144])
        gT1 = cx.din("gT1", [128, 3, KC])
        oT = cx.din("oT", [D, TOKC])
        w_out = cx.din("w_out", [D, D])
        wg = cx.din("wg", [D, DFF])
        wu = cx.din("wu", [D, DFF])
        wd = cx.din("wd", [DFF, D])
        gfT = cx.din("gfT", [128, KC])
        outT = cx.dout("outT", [D, TOKC])
        co = Core(cx)
        co.load_h(hT)
        co.load_mod(mod1, gT1)
        co.mixer1_out(oT, w_out)
        co.ffn(2, wg, wu, wd)
        co.final_norm(gfT, outT)
        cx.finish()
    return nc


MODW = 9 * D // NCORES
MODC = MODW // 128


def build_M():
    import contextlib
    nc = bass.Bass("TRN2", target_bir_lowering=False)
    with contextlib.ExitStack() as st:
        cx = Ctx(nc, st)
        P = cx.P
        condT = cx.din("condT", [128, KC])
        aw = cx.din("aw", [2, D, MODW])
        abT = cx.din("abT", [128, 2, MODC])
        modc = cx.dout("modc", [128, 2, MODC])
        ws = Stream(cx)
        c32 = TB(cx, [128, KC], F32, "mc32")
        cb = TB(cx, [128, KC], BF16, "mcb")
        ab = TB(cx, [128, 2, MODC], F32, "mab")
        mo = TB(cx, [128, 2, MODC], F32, "mmo")
        pm = cx.ps(name="mpm")
        pm_b = Buf("mpm")
        P.dma("sp", lambda e: e.dma_start(out=c32.t[:], in_=condT), writes=[c32.b])
        P.dma("sp", lambda e: e.dma_start(out=ab.t[:], in_=abT), writes=[ab.b])
        P.op("act", lambda e: e.activation(out=cb.t[:], in_=c32.t[:], func=AF.Silu), reads=[c32.b], writes=[cb.b])
        for l in range(2):
            src = aw[l].rearrange("(k p) n -> p k n", p=128)
            for nb in range((MODW + 511) // 512):
                ncol = min(512, MODW - nb * 512)
                v, wb = ws.load(src[:, :, nb * 512:nb * 512 + ncol], KC, ncol)
                for j in range(ncol // 128):
                    col = l * MODC + nb * 4 + j
                    for k in range(KC):
                        P.op("pe", lambda e, v=v, j=j, k=k, col=col: e.matmul(
                            pm[:, col:col + 1], v[:, k, j * 128:(j + 1) * 128], cb.t[:, k:k + 1], start=(k == 0), stop=(k == KC - 1)),
                            reads=[wb, cb.b], writes=[pm_b])
        P.op("dve", lambda e: e.tensor_tensor(out=mo.t[:].rearrange("p a b -> p (a b)"), in0=pm[:, 0:2 * MODC],
                                              in1=ab.t[:].rearrange("p a b -> p (a b)"), op=ALU.add), reads=[pm_b, ab.b], writes=[mo.b])
        P.dma("sp", lambda e: e.dma_start(out=modc, in_=mo.t[:]), reads=[mo.b])
        cx.finish()
    return nc


def _fm(v):
    return np.ascontiguousarray(np.asarray(v).reshape(-1, 128).T)


def _gT(norm_g_layer):
    return np.ascontiguousarray(np.asarray(norm_g_layer).reshape(3, KC, 128).transpose(2, 0, 1))


_SAVE = None


def _run(nc, ims):
    return run_bass_kernel_spmd(nc, ims, core_ids=list(range(NCORES))).results


def kernel(**inp):
    inp = {k: np.asarray(v) for k, v in inp.items()}
    f32 = np.float32
    x = inp['x'][0]
    cores = range(NCORES)
    tokc = [slice(c * TOKC, (c + 1) * TOKC) for c in cores]
    condT = _fm(inp['c'][0])
    sv = _SAVE if _SAVE is not None else {}
    if 'M' not in sv:
        ims = [{"condT": condT, "aw": np.ascontiguousarray(inp['ada_w'][:, :, c * MODW:(c + 1) * MODW]),
                "abT": np.ascontiguousarray(inp['ada_b'][:, c * MODW:(c + 1) * MODW].reshape(2, MODC, 128).transpose(2, 0, 1))} for c in cores]
        r = _run(build_M(), ims)
        sv['M'] = [np.ascontiguousarray(np.concatenate([r[c]["modc"][:, l, :] for c in cores], axis=1)) for l in range(2)]
    mod0, mod1 = sv['M']
    if 'A0' not in sv:
        ims = [{"xT": np.ascontiguousarray(x[tokc[c]].T), "mod0": mod0,
                "gT": _gT(inp['norm_g'][0]), "wg": inp['ffn_w_gate'][0, 0], "wu": inp['ffn_w_up'][0, 0], "wd": inp['ffn_w_down'][0, 0],
                "w_in": inp['ab_w_in'][0]} for c in cores]
        r = _run(build_A0(), ims)
        sv['A0'] = {"hT": [r[c]["hT_out"] for c in cores],
                    "pT": np.concatenate([r[c]["pT_out"] for c in cores], axis=1)}
    hT0, pT = sv['A0']["hT"], sv['A0']["pT"]
    if 'S5' not in sv:
        ims = []
        for c in cores:
            im = s5_host_layout(inp, c)
            im["uT"] = np.ascontiguousarray(pT[c * 128:(c + 1) * 128, :])
            ims.append(im)
        r = _run(build_S5(), ims)
        sv['S5'] = np.concatenate([r[c]["yT"] for c in cores], axis=0)
    yaT = sv['S5']
    toks = [np.concatenate([np.arange(128) + 128 * (8 * i + c) for i in range(NSLOT)]) for c in cores]
    ident = np.eye(128, dtype=f32)
    if 'C1' not in sv:
        pow2 = np.tile((0.5 ** np.arange(NBIS + 1)).astype(f32), (128, 1))
        ims = [{"kidxT": np.ascontiguousarray(pT[3328:3392, :]), "qidxT": np.ascontiguousarray(pT[2304:3328, toks[c]]),
                "widx": np.ascontiguousarray(pT[3392:3408, toks[c]].reshape(16, NSLOT, 128).transpose(2, 1, 0)),
                "adm": dsa_adm_mask(c), "pow2": pow2, "ident": ident, "negpos": dsa_negpos(c)} for c in cores]
        r = _run(build_C1(), ims)
        sv['C1'] = [(r[c]["selT"], r[c]["ndmin"]) for c in cores]
    if 'C2' not in sv:
        ims = []
        for c in cores:
            bcol, corr = dsa_alibi_tables(c)
            ims.append({"kvT": np.ascontiguousarray(pT[2048:2304, :]), "kvg": _fm(inp['dsa_kv_norm_g'][0]), "wkv": inp['dsa_w_kv_up'][0],
                        "qT": np.ascontiguousarray(pT[1024:2048, toks[c]]), "selT": sv['C1'][c][0], "biascol": bcol, "corr": corr, "ident": ident,
                        "ndminT": np.ascontiguousarray(sv['C1'][c][1].T.reshape(1, -1)),
                        "qoff": (64 - np.arange(128, dtype=f32)).reshape(1, 128)})
        r = _run(build_C2(), ims)
        ybT = np.zeros((1024, SEQ), f32)
        for c in cores:
            ybT[:, toks[c]] = r[c]["ybT"]
        sv['C2'] = ybT
    ybT = sv['C2']
    if 'D0A1' not in sv:
        ims = [{"hT": hT0[c], "mod0": mod0, "gT0": _gT(inp['norm_g'][0]), "yaT": np.ascontiguousarray(yaT[:, tokc[c]]),
                "ybT": np.ascontiguousarray(ybT[:, tokc[c]]), "w_glu": inp['s5_w_glu'][0], "bgT": _fm(inp['s5_b_glu'][0]),
                "w_out": inp['ab_w_out'][0], "wg0": inp['ffn_w_gate'][0, 1], "wu0": inp['ffn_w_up'][0, 1], "wd0": inp['ffn_w_down'][0, 1],
                "mod1": mod1, "gT1": _gT(inp['norm_g'][1]),
                "wg1": inp['ffn_w_gate'][1, 0], "wu1": inp['ffn_w_up'][1, 0], "wd1": inp['ffn_w_down'][1, 0], "w_qkv": inp['c_w_qkv'][0]}
               for c in cores]
        r = _run(build_D0A1(), ims)
        sv['D0A1'] = {"hT": [r[c]["hT_out"] for c in cores],
                      "qkvT": np.concatenate([r[c]["qkvT"] for c in cores], axis=1)}
    hT1, qkvT = sv['D0A1']["hT"], sv['D0A1']["qkvT"]
    if 'ATT' not in sv:
        biasT = att_bias_table(inp['c_rel_bias'][0])
        kpad = np.concatenate([np.zeros((D, HALO), f32), qkvT[D:2 * D]], axis=1)
        vpad = np.concatenate([np.zeros((D, HALO), f32), qkvT[2 * D:3 * D]], axis=1)
        ims = [{"qT": np.ascontiguousarray(qkvT[0:D, tokc[c]]), "kT": np.ascontiguousarray(kpad[:, c * TOKC:(c + 1) * TOKC + HALO]),
                "v": np.ascontiguousarray(vpad[:, c * TOKC:(c + 1) * TOKC + HALO].T), "biasT": biasT,
                "vones": np.full((128, 128), 0.0 if c == 0 else 1.0, f32)} for c in cores]
        r = _run(build_ATT(), ims)
        sv['ATT'] = [r[c]["oT"] for c in cores]
    oT = sv['ATT']
    ims = [{"hT": hT1[c], "mod1": mod1, "gT1": _gT(inp['norm_g'][1]), "oT": oT[c], "w_out": inp['c_w_out'][0],
            "wg": inp['ffn_w_gate'][1, 1], "wu": inp['ffn_w_up'][1, 1], "wd": inp['ffn_w_down'][1, 1], "gfT": _fm(inp['final_norm_g'])}
           for c in cores]
    r = _run(build_D1(), ims)
    out = np.zeros((1, SEQ, D), f32)
    for c in cores:
        out[0, tokc[c], :] = r[c]["outT"].T
    return out
```

```python
import numpy as np
import concourse.bass as bass
import concourse.mybir as mybir
from concourse.bass_utils import run_bass_kernel_spmd

F32 = mybir.dt.float32
BF16 = mybir.dt.bfloat16
AF = mybir.ActivationFunctionType
ALU = mybir.AluOpType
AX = mybir.AxisListType

NCORES = 8


class Buf:
    __slots__ = ("name", "lw", "rd")

    def __init__(self, name):
        self.name = name
        self.lw = None
        self.rd = {}


class Prog:
    ENG = ("pe", "act", "dve", "pool", "sp")

    def __init__(self, nc, n_dma_sems=12):
        self.nc = nc
        self.ops = {e: [] for e in self.ENG}
        self.cnt = {e: 0 for e in self.ENG}
        self.seen = {e: {} for e in self.ENG}
        self.n_dma_sems = n_dma_sems
        self.dma_cnt = [0] * n_dma_sems
        self.dma_rr = 0
        self.sems = {}
        self._stack = None

    def _deps(self, eng, reads, writes):
        need = {}

        def add(kc):
            if kc is None:
                return
            k, c = kc
            if need.get(k, 0) < c:
                need[k] = c

        for b in reads:
            add(b.lw)
        for b in writes:
            add(b.lw)
            for k, c in b.rd.items():
                add((k, c))
        out = []
        for k, c in need.items():
            if k == "pe" and eng == "pe":
                continue
            if self.seen[eng].get(k, 0) >= c:
                continue
            self.seen[eng][k] = c
            out.append((k, c))
        return out

    def op(self, eng, fn, reads=(), writes=()):
        waits = self._deps(eng, reads, writes)
        self.cnt[eng] += 1
        me = (eng, self.cnt[eng])
        for b in reads:
            b.rd[eng] = me[1]
        for b in writes:
            b.lw = me
            b.rd = {}
        self.ops[eng].append((waits, fn, (eng, 1)))

    def dma(self, eng, fn, reads=(), writes=()):
        s = self.dma_rr
        self.dma_rr = (self.dma_rr + 1) % self.n_dma_sems
        key = "dma%d" % s
        waits = self._deps(eng, reads, writes)
        prev = self.dma_cnt[s]
        if prev and self.seen[eng].get(key, 0) < prev:
            self.seen[eng][key] = prev
            waits.append((key, prev))
        self.dma_cnt[s] += 1
        me = (key, self.dma_cnt[s])
        for b in reads:
            b.rd[key] = me[1]
        for b in writes:
            b.lw = me
            b.rd = {}
        self.ops[eng].append((waits, fn, (key, 16)))

    def final_wait(self, eng, bufs):
        waits = self._deps(eng, bufs, ())
        self.ops[eng].append((waits, None, None))

    def emit(self):
        nc = self.nc
        import contextlib
        with contextlib.ExitStack() as st:
            keys = list(self.ENG) + ["dma%d" % i for i in range(self.n_dma_sems)]
            sem = {k: st.enter_context(nc.semaphore("s_" + k)) for k in keys}
            block = st.enter_context(nc.Block())
            mult = {k: (16 if k.startswith("dma") else 1) for k in keys}

            def run(engname):
                def body(e):
                    for waits, fn, inc in self.ops[engname]:
                        for k, c in waits:
                            e.wait_ge(sem[k], c * mult[k])
                        if fn is not None:
                            ins = fn(e)
                            ins.then_inc(sem[inc[0]], inc[1])
                return body

            block.tensor(run("pe"))
            block.scalar(run("act"))
            block.vector(run("dve"))
            block.gpsimd(run("pool"))
            block.sync(run("sp"))


D = 2048
KC = D // 128
DFF = 5504
FC = DFF // 128
SEQ = 8192
TOKC = SEQ // NCORES
TT = 512
EPS = 1e-6
D_IN_AB = 3408


class Ctx:
    def __init__(self, nc, st):
        self.nc = nc
        self.st = st
        self.P = Prog(nc)
        self._n = 0
        self.outs = []

    def sb(self, shape, dt, name=None):
        self._n += 1
        return self.st.enter_context(self.nc.sbuf_tensor("sb_" + (name or str(self._n)), list(shape), dt))

    def ps(self, shape=(128, 512), dt=F32, name=None):
        self._n += 1
        return self.st.enter_context(self.nc.psum_tensor("ps_" + (name or str(self._n)), list(shape), dt))

    def din(self, name, shape, dt=F32):
        return self.nc.dram_tensor(name, list(shape), dt, kind="ExternalInput").ap()

    def dout(self, name, shape, dt=F32):
        return self.nc.dram_tensor(name, list(shape), dt, kind="ExternalOutput").ap()

    def finish(self):
        P = self.P
        waits = [("dma%d" % i, P.dma_cnt[i]) for i in range(P.n_dma_sems) if P.dma_cnt[i]]
        waits += [(e, P.cnt[e]) for e in ("pe", "act", "dve", "pool") if P.cnt[e]]
        P.ops["sp"].append((waits, None, None))
        P.emit()


class Stream:
    def __init__(self, cx, nslots=3, elems=8192):
        self.cx = cx
        self.n = nslots
        self.elems = elems
        self.t = cx.sb([128, nslots, elems], BF16, "wbuf")
        self.bufs = [Buf("w%d" % i) for i in range(nslots)]
        self.i = 0

    def load(self, src_ap, k, n, rows=128):
        s = self.i
        self.i = (self.i + 1) % self.n
        view = self.t[0:rows, s, 0:k * n].rearrange("p (k n) -> p k n", k=k)
        b = self.bufs[s]
        self.cx.P.dma("pool", lambda e, v=view, a=src_ap: e.dma_start(out=v, in_=a), writes=[b])
        return view, b


class Core:
    def __init__(self, cx, ntok=TOKC):
        self.cx = cx
        P = cx.P
        self.ntok = ntok
        self.hT = cx.sb([128, KC, ntok], F32, "hT")
        self.hT_b = [Buf("hT%d" % k) for k in range(KC)]
        self.hn = cx.sb([128, KC, TT], BF16, "hn")
        self.hn_b = Buf("hn")
        self.A = cx.sb([128, FC, TT], BF16, "A")
        self.A_b = [Buf("A%d" % f) for f in range(FC)]
        self.ws = Stream(cx)
        self.ones = cx.sb([128, 128], BF16, "ones")
        self.ones_b = Buf("ones")
        P.op("pool", lambda e: e.memset(self.ones[:], 1.0), writes=[self.ones_b])
        self.sq = [cx.sb([128, TT], BF16, "sq%d" % i) for i in range(2)]
        self.sq_b = [Buf("sq%d" % i) for i in range(2)]
        self.tmp = [cx.sb([128, TT], F32, "tmp%d" % i) for i in range(3)]
        self.tmp_b = [Buf("tmp%d" % i) for i in range(3)]
        self.tmp_i = 0
        self.rstd = cx.sb([128, TT], F32, "rstd")
        self.rstd_b = Buf("rstd")
        self.rtmp = cx.sb([128, TT], F32, "rtmp")
        self.rtmp_b = Buf("rtmp")
        self.pgu = [cx.ps(name="pgu%d" % i) for i in range(4)]
        self.pgu_b = [Buf("pgu%d" % i) for i in range(4)]
        self.pacc = [cx.ps(name="pacc%d" % i) for i in range(2)]
        self.pacc_b = [Buf("pacc%d" % i) for i in range(2)]
        self.pacc_i = 0
        self.pst = cx.ps(name="pst")
        self.pst_b = Buf("pst")
        self.modT = cx.sb([128, 144], F32, "modT")
        self.mod_b = Buf("mod")
        self.vec = cx.sb([128, 3, 3, KC], F32, "vec")
        self.vec_b = Buf("vec")

    def next_tmp(self):
        i = self.tmp_i
        self.tmp_i = (i + 1) % len(self.tmp)
        return self.tmp[i], self.tmp_b[i]

    def next_acc(self):
        i = self.pacc_i
        self.pacc_i = (i + 1) % len(self.pacc)
        return self.pacc[i], self.pacc_b[i]

    def load_h(self, hT_dram):
        P = self.cx.P
        src = hT_dram.rearrange("(k p) t -> p k t", p=128)
        for k in range(KC):
            P.dma("sp", lambda e, k=k: e.dma_start(out=self.hT[:, k, :], in_=src[:, k, :]), writes=[self.hT_b[k]])

    def store_h(self, out_dram):
        P = self.cx.P
        dst = out_dram.rearrange("(k p) t -> p k t", p=128)
        for k in range(KC):
            P.dma("sp", lambda e, k=k: e.dma_start(out=dst[:, k, :], in_=self.hT[:, k, :]), reads=[self.hT_b[k]])

    def compute_mod(self, condT_dram, ada_w, ada_bT_dram, gT_dram):
        cx, P = self.cx, self.cx.P
        self._modn = getattr(self, "_modn", 0) + 1
        sfx = str(self._modn)
        c32 = cx.sb([128, KC], F32, "c32" + sfx)
        cb = cx.sb([128, KC], BF16, "cb" + sfx)
        abT = cx.sb([128, 144], F32, "abT" + sfx)
        gT = cx.sb([128, 3, KC], F32, "gT" + sfx)
        b_c32, b_cb, b_ab, b_g = Buf("c32"), Buf("cb"), Buf("abT"), Buf("gT")
        P.dma("sp", lambda e: e.dma_start(out=c32[:], in_=condT_dram), writes=[b_c32])
        P.dma("sp", lambda e: e.dma_start(out=abT[:], in_=ada_bT_dram), writes=[b_ab])
        P.dma("sp", lambda e: e.dma_start(out=gT[:], in_=gT_dram), writes=[b_g])
        P.op("act", lambda e: e.activation(out=cb[:], in_=c32[:], func=AF.Silu), reads=[b_c32], writes=[b_cb])
        wsrc = ada_w.rearrange("(k p) n -> p k n", p=128)
        pm = self.pacc[0]
        pm_b = self.pacc_b[0]
        for nb in range(36):
            view, wb = self.ws.load(wsrc[:, :, nb * 512:(nb + 1) * 512], KC, 512)
            for j in range(4):
                col = nb * 4 + j
                for k in range(KC):
                    P.op("pe", lambda e, v=view, j=j, k=k, col=col: e.matmul(
                        pm[:, col:col + 1], v[:, k, j * 128:(j + 1) * 128], cb[:, k:k + 1],
                        start=(k == 0), stop=(k == KC - 1)), reads=[wb, b_cb], writes=[pm_b])
        P.op("dve", lambda e: e.tensor_tensor(out=self.modT[:], in0=pm[:, 0:144], in1=abT[:], op=ALU.add),
             reads=[pm_b, b_ab], writes=[self.mod_b])
        self.derive_vec(gT, b_g)

    def load_mod(self, mod_dram, gT_dram):
        cx, P = self.cx, self.cx.P
        self._modn = getattr(self, "_modn", 0) + 1
        gT = cx.sb([128, 3, KC], F32, "gT" + str(self._modn))
        b_g = Buf("gT")
        P.dma("sp", lambda e: e.dma_start(out=gT[:], in_=gT_dram), writes=[b_g])
        P.dma("sp", lambda e: e.dma_start(out=self.modT[:], in_=mod_dram), writes=[self.mod_b])
        self.derive_vec(gT, b_g)

    def derive_vec(self, gT, b_g):
        P = self.cx.P
        for s in range(3):
            sh = self.modT[:, (s * 3 + 0) * KC:(s * 3 + 1) * KC]
            sc = self.modT[:, (s * 3 + 1) * KC:(s * 3 + 2) * KC]
            ga = self.modT[:, (s * 3 + 2) * KC:(s * 3 + 3) * KC]
            P.op("dve", lambda e, s=s, sc=sc: e.scalar_tensor_tensor(
                out=self.vec[:, s, 0, :], in0=sc, scalar=1.0, in1=gT[:, s, :], op0=ALU.add, op1=ALU.mult),
                reads=[self.mod_b, b_g], writes=[self.vec_b])
            P.op("dve", lambda e, s=s, sh=sh: e.tensor_copy(self.vec[:, s, 1, :], sh),
                 reads=[self.mod_b], writes=[self.vec_b])
            cgate = 1.0 if s == 1 else 0.5
            P.op("dve", lambda e, s=s, ga=ga, cgate=cgate: e.tensor_scalar(
                out=self.vec[:, s, 2, :], in0=ga, scalar1=cgate, scalar2=None, op0=ALU.mult),
                reads=[self.mod_b], writes=[self.vec_b])

    def adaln(self, sub, t0, plain_g=None):
        cx, P = self.cx, self.cx.P
        for k in range(KC):
            i = k % 2
            P.op("act", lambda e, k=k, i=i: e.activation(out=self.sq[i][:], in_=self.hT[:, k, t0:t0 + TT], func=AF.Square),
                 reads=[self.hT_b[k]], writes=[self.sq_b[i]])
            P.op("pe", lambda e, k=k, i=i: e.matmul(self.pst[:], self.ones[:], self.sq[i][:], start=(k == 0), stop=(k == KC - 1)),
                 reads=[self.sq_b[i], self.ones_b], writes=[self.pst_b])
        P.op("dve", lambda e: e.tensor_scalar(out=self.rtmp[:], in0=self.pst[:], scalar1=1.0 / D, scalar2=EPS,
                                              op0=ALU.mult, op1=ALU.add), reads=[self.pst_b], writes=[self.rtmp_b])
        P.op("act", lambda e: e.activation(out=self.rtmp[:], in_=self.rtmp[:], func=AF.Sqrt),
             reads=[self.rtmp_b], writes=[self.rtmp_b])
        P.op("dve", lambda e: e.reciprocal(self.rstd[:], self.rtmp[:]), reads=[self.rtmp_b], writes=[self.rstd_b])

    def adaln_apply(self, sub, t0, k, out_ap, out_bufs, gs_ap=None, shift_ap=None):
        P = self.cx.P
        tmp, tb = self.next_tmp()
        gs = gs_ap if gs_ap is not None else self.vec[:, sub, 0, k:k + 1]
        P.op("dve", lambda e: e.scalar_tensor_tensor(out=tmp[:], in0=self.hT[:, k, t0:t0 + TT], scalar=gs,
                                                     in1=self.rstd[:], op0=ALU.mult, op1=ALU.mult),
             reads=[self.hT_b[k], self.rstd_b, self.vec_b], writes=[tb])
        if shift_ap is None and gs_ap is None:
            shift_ap = self.vec[:, sub, 1, k:k + 1]
        if shift_ap is not None:
            P.op("act", lambda e: e.activation(out=out_ap, in_=tmp[:], func=AF.Identity, bias=shift_ap, scale=1.0),
                 reads=[tb, self.vec_b], writes=out_bufs)
        else:
            P.op("act", lambda e: e.activation(out=out_ap, in_=tmp[:], func=AF.Copy), reads=[tb], writes=out_bufs)

    def adaln_full(self, sub, t0):
        self.adaln(sub, t0)
        for k in range(KC):
            self.adaln_apply(sub, t0, k, self.hn[:, k, :], [self.hn_b])

    def ffn(self, sub, w_gate, w_up, w_down):
        cx, P = self.cx, self.cx.P
        wg_src = w_gate.rearrange("(k p) n -> p k n", p=128)
        wu_src = w_up.rearrange("(k p) n -> p k n", p=128)
        wd_src = w_down.rearrange("(f p) n -> p f n", p=128)
        for t0 in range(0, self.ntok, TT):
            self.adaln_full(sub, t0)
            gi = 0
            for nb in range((FC + 1) // 2):
                ncol = min(256, DFF - nb * 256)
                nj = ncol // 128
                vg, bg = self.ws.load(wg_src[:, :, nb * 256:nb * 256 + ncol], KC, ncol)
                vu, bu = self.ws.load(wu_src[:, :, nb * 256:nb * 256 + ncol], KC, ncol)
                for j in range(nj):
                    f = nb * 2 + j
                    pg, pg_b = self.pgu[gi], self.pgu_b[gi]
                    pu, pu_b = self.pgu[gi + 1], self.pgu_b[gi + 1]
                    gi = (gi + 2) % 4
                    for k in range(KC):
                        P.op("pe", lambda e, k=k, j=j, vg=vg, pg=pg: e.matmul(
                            pg[:], vg[:, k, j * 128:(j + 1) * 128], self.hn[:, k, :], start=(k == 0), stop=(k == KC - 1)),
                            reads=[bg, self.hn_b], writes=[pg_b])
                    for k in range(KC):
                        P.op("pe", lambda e, k=k, j=j, vu=vu, pu=pu: e.matmul(
                            pu[:], vu[:, k, j * 128:(j + 1) * 128], self.hn[:, k, :], start=(k == 0), stop=(k == KC - 1)),
                            reads=[bu, self.hn_b], writes=[pu_b])
                    tmp, tb = self.next_tmp()
                    P.op("act", lambda e, tmp=tmp, pg=pg: e.activation(out=tmp[:], in_=pg[:], func=AF.Silu),
                         reads=[pg_b], writes=[tb])
                    P.op("dve", lambda e, tmp=tmp, pu=pu, f=f: e.tensor_tensor(out=self.A[:, f, :], in0=tmp[:], in1=pu[:], op=ALU.mult),
                         reads=[tb, pu_b], writes=[self.A_b[f]])
            for dc in range(KC):
                vd, bd = self.ws.load(wd_src[:, :, dc * 128:(dc + 1) * 128], FC, 128)
                py, py_b = self.next_acc()
                for f in range(FC):
                    P.op("pe", lambda e, f=f, vd=vd, py=py: e.matmul(
                        py[:], vd[:, f, :], self.A[:, f, :], start=(f == 0), stop=(f == FC - 1)),
                        reads=[bd, self.A_b[f]], writes=[py_b])
                P.op("dve", lambda e, dc=dc, py=py, t0=t0: e.scalar_tensor_tensor(
                    out=self.hT[:, dc, t0:t0 + TT], in0=py[:], scalar=self.vec[:, sub, 2, dc:dc + 1],
                    in1=self.hT[:, dc, t0:t0 + TT], op0=ALU.mult, op1=ALU.add),
                    reads=[py_b, self.vec_b, self.hT_b[dc]], writes=[self.hT_b[dc]])

    def proj(self, w, ncols, t0, epilogue, xsrc=None, xbufs=None, nk=KC):
        cx, P = self.cx, self.cx.P
        xsrc = self.hn if xsrc is None else xsrc
        xbufs = [self.hn_b] * nk if xbufs is None else xbufs
        src = w.rearrange("(k p) n -> p k n", p=128)
        for nb in range((ncols + 511) // 512):
            nc_ = min(512, ncols - nb * 512)
            v, b = self.ws.load(src[:, :, nb * 512:nb * 512 + nc_], nk, nc_)
            for j in range((nc_ + 127) // 128):
                rows = min(128, nc_ - j * 128)
                pa, pa_b = self.next_acc()
                for k in range(nk):
                    P.op("pe", lambda e, k=k, j=j, v=v, pa=pa, rows=rows: e.matmul(
                        pa[0:rows, :], v[:, k, j * 128:j * 128 + rows], xsrc[:, k, :], start=(k == 0), stop=(k == nk - 1)),
                        reads=[b, xbufs[k]], writes=[pa_b])
                epilogue(nb * 4 + j, rows, pa, pa_b)


    def resid_epilogue(self, sub, t0):
        P = self.cx.P

        def epi(c, rows, pa, pa_b):
            P.op("dve", lambda e: e.scalar_tensor_tensor(
                out=self.hT[:, c, t0:t0 + TT], in0=pa[:], scalar=self.vec[:, sub, 2, c:c + 1],
                in1=self.hT[:, c, t0:t0 + TT], op0=ALU.mult, op1=ALU.add),
                reads=[pa_b, self.vec_b, self.hT_b[c]], writes=[self.hT_b[c]])
        return epi

    def out_epilogue(self, dst, t0):
        cx, P = self.cx, self.cx.P
        if not hasattr(self, "_stg"):
            self._stg = [cx.sb([128, TT], F32, "ostg%d" % i) for i in range(2)]
            self._stg_b = [Buf("ostg%d" % i) for i in range(2)]
            self._stg_i = 0

        def epi(c, rows, pa, pa_b):
            i = self._stg_i
            self._stg_i = (i + 1) % 2
            stg, sb_ = self._stg[i], self._stg_b[i]
            P.op("act", lambda e: e.activation(out=stg[0:rows, :], in_=pa[0:rows, :], func=AF.Copy), reads=[pa_b], writes=[sb_])
            P.dma("sp", lambda e: e.dma_start(out=dst[c * 128:c * 128 + rows, t0:t0 + TT], in_=stg[0:rows, :]), reads=[sb_])
        return epi

    def mixer0_out(self, yaT, ybT, w_glu, bgT, w_out):
        cx, P = self.cx, self.cx.P
        bg = cx.sb([128, 8], F32, "bglu")
        bg_b = Buf("bglu")
        P.dma("sp", lambda e: e.dma_start(out=bg[:], in_=bgT), writes=[bg_b])
        ya_src = yaT.rearrange("(k p) t -> p k t", p=128)
        yb_src = ybT.rearrange("(k p) t -> p k t", p=128)
        for t0 in range(0, self.ntok, TT):
            P.dma("pool", lambda e, t0=t0: e.dma_start(out=self.hn[:, 0:8, :], in_=ya_src[:, :, t0:t0 + TT]), writes=[self.hn_b])
            for k in range(8):
                P.dma("pool", lambda e, t0=t0, k=k: e.dma_start(out=self.A[:, 8 + k, :], in_=yb_src[:, k, t0:t0 + TT]), writes=[self.A_b[8 + k]])

            def epi(c, rows, pa, pa_b):
                tmp, tb = self.next_tmp()
                P.op("act", lambda e: e.activation(out=tmp[:], in_=pa[:], func=AF.Sigmoid, bias=bg[:, c:c + 1], scale=1.0),
                     reads=[pa_b, bg_b], writes=[tb])
                P.op("dve", lambda e: e.tensor_tensor(out=self.A[:, c, :], in0=tmp[:], in1=self.hn[:, c, :], op=ALU.mult),
                     reads=[tb, self.hn_b], writes=[self.A_b[c]])
            self.proj(w_glu, 1024, t0, epi, nk=8)
            self.proj(w_out, D, t0, self.resid_epilogue(1, t0), xsrc=self.A, xbufs=self.A_b[0:KC], nk=KC)

    def mixer1_out(self, oT, w_out):
        P = self.cx.P
        o_src = oT.rearrange("(k p) t -> p k t", p=128)
        for t0 in range(0, self.ntok, TT):
            P.dma("pool", lambda e, t0=t0: e.dma_start(out=self.hn[:], in_=o_src[:, :, t0:t0 + TT]), writes=[self.hn_b])
            self.proj(w_out, D, t0, self.resid_epilogue(1, t0))

    def final_norm(self, gfT, outT):
        cx, P = self.cx, self.cx.P
        gf = cx.sb([128, KC], F32, "gfin")
        gf_b = Buf("gfin")
        P.dma("sp", lambda e: e.dma_start(out=gf[:], in_=gfT), writes=[gf_b])
        dst = outT.rearrange("(k p) t -> p k t", p=128)
        for t0 in range(0, self.ntok, TT):
            self.adaln(0, t0)
            for k in range(KC):
                tmp, tb = self.next_tmp()
                P.op("dve", lambda e, k=k, tmp=tmp, t0=t0: e.scalar_tensor_tensor(
                    out=tmp[:], in0=self.hT[:, k, t0:t0 + TT], scalar=gf[:, k:k + 1], in1=self.rstd[:], op0=ALU.mult, op1=ALU.mult),
                    reads=[self.hT_b[k], self.rstd_b, gf_b], writes=[tb])
                P.dma("sp", lambda e, k=k, tmp=tmp, t0=t0: e.dma_start(out=dst[:, k, t0:t0 + TT], in_=tmp[:]), reads=[tb])


def build_A0():
    import contextlib
    nc = bass.Bass("TRN2", target_bir_lowering=False)
    with contextlib.ExitStack() as st:
        cx = Ctx(nc, st)
        P = cx.P
        xT = cx.din("xT", [D, TOKC])
        mod0 = cx.din("mod0", [128, 144])
        gT = cx.din("gT", [128, 3, KC])
        wg = cx.din("wg", [D, DFF])
        wu = cx.din("wu", [D, DFF])
        wd = cx.din("wd", [DFF, D])
        w_in = cx.din("w_in", [D, D_IN_AB])
        hT_out = cx.dout("hT_out", [D, TOKC])
        pT_out = cx.dout("pT_out", [D_IN_AB, TOKC])
        co = Core(cx)
        co.load_h(xT)
        co.load_mod(mod0, gT)
        co.ffn(0, wg, wu, wd)
        co.store_h(hT_out)
        stg = [cx.sb([128, TT], F32, "stg%d" % i) for i in range(2)]
        stg_b = [Buf("stg%d" % i) for i in range(2)]
        cnt = [0]
        for t0 in range(0, TOKC, TT):
            co.adaln_full(1, t0)

            def epi(c, rows, pa, pa_b, t0=t0):
                i = cnt[0] % 2
                cnt[0] += 1
                P.op("act", lambda e: e.activation(out=stg[i][0:rows, :], in_=pa[0:rows, :], func=AF.Copy),
                     reads=[pa_b], writes=[stg_b[i]])
                P.dma("sp", lambda e: e.dma_start(out=pT_out[c * 128:c * 128 + rows, t0:t0 + TT], in_=stg[i][0:rows, :]),
                      reads=[stg_b[i]])
            co.proj(w_in, D_IN_AB, t0, epi)
        cx.finish()
    return nc


HD = 128
C_HEADS = 16
HALO = 512


def att_bias_table(rel_bias):
    s_l = np.arange(128)[:, None, None]
    i = np.arange(5)[None, :, None]
    q_l = np.arange(128)[None, None, :]
    rel = 512 + q_l - 128 * i - s_l
    dchunk = q_l // 64 + 8 - 2 * i - s_l // 64
    ok = (dchunk >= 0) & (dchunk <= 8)
    idx = np.clip(rel, -256, 256) + 256
    tab = rel_bias[:, idx]
    tab = np.where(ok[None], tab, np.float32(-30000.0)).astype(np.float32)
    return np.ascontiguousarray(tab.transpose(1, 0, 2, 3).reshape(128, 16, 640))


def emit_attention(cx, qT, kT, v, biasT, vones, oT, ntok=TOKC):
    P = cx.P
    NQT = ntok // 128
    NKT = NQT + 4
    qb = cx.sb([128, C_HEADS, ntok], BF16, "qb")
    kb = cx.sb([128, C_HEADS, ntok + HALO], BF16, "kb")
    vb = cx.sb([128, NKT, D], BF16, "vb")
    bias = cx.sb([128, C_HEADS, 640], F32, "bias")
    ones = cx.sb([128, 128], BF16, "aones")
    von = cx.sb([128, 128], BF16, "vones")
    qb_b = [Buf("qb%d" % h) for h in range(C_HEADS)]
    kb_b = [Buf("kb%d" % h) for h in range(C_HEADS)]
    vb_b = [Buf("vb%d" % t) for t in range(NKT)]
    bias_b, ones_b, von_b = Buf("bias"), Buf("ones"), Buf("von")
    P.op("dve", lambda e: e.memset(ones[:], 1.0), writes=[ones_b])
    P.dma("pool", lambda e: e.dma_start(out=von[:], in_=vones), writes=[von_b])
    P.dma("sp", lambda e: e.dma_start(out=bias[:], in_=biasT), writes=[bias_b])
    qsrc = qT.rearrange("(h p) t -> p h t", p=128)
    ksrc = kT.rearrange("(h p) t -> p h t", p=128)
    vsrc = v.rearrange("(n p) d -> p n d", p=128)
    for h in range(C_HEADS):
        P.dma("pool", lambda e, h=h: e.dma_start(out=qb[:, h, :], in_=qsrc[:, h, :]), writes=[qb_b[h]])
        P.dma("pool", lambda e, h=h: e.dma_start(out=kb[:, h, :], in_=ksrc[:, h, :]), writes=[kb_b[h]])
    for t in range(NKT):
        P.dma("pool", lambda e, t=t: e.dma_start(out=vb[:, t, :], in_=vsrc[:, t, :]), writes=[vb_b[t]])
    psA = [cx.ps(name="attA%d" % i) for i in range(2)]
    psB = [cx.ps(name="attB%d" % i) for i in range(2)]
    psO = [cx.ps(name="attO%d" % i) for i in range(2)]
    psA_b = [Buf("psA%d" % i) for i in range(2)]
    psB_b = [Buf("psB%d" % i) for i in range(2)]
    psO_b = [Buf("psO%d" % i) for i in range(2)]
    tmp = [cx.sb([128, 640], F32, "atmp%d" % i) for i in range(2)]
    tmp_b = [Buf("atmp%d" % i) for i in range(2)]
    pT = [cx.sb([128, 640], BF16, "apT%d" % i) for i in range(2)]
    pT_b = [Buf("apT%d" % i) for i in range(2)]
    rec = [cx.sb([128, 128], F32, "arec%d" % i) for i in range(2)]
    rec_b = [Buf("arec%d" % i) for i in range(2)]
    ost = [cx.sb([128, ntok], F32, "aost%d" % i) for i in range(2)]
    ost_b = [Buf("aost%d" % i) for i in range(2)]
    odst = oT.rearrange("(h p) t -> p h t", p=128)
    scale = float(HD) ** -0.5
    def stage_a(h, qt, a):
        qs = slice(qt * 128, (qt + 1) * 128)
        for i in range(5):
            kt = qt + i
            dst = psA[a][:, i * 128:(i + 1) * 128] if i < 4 else psB[a][:, 0:128]
            dst_b = psA_b[a] if i < 4 else psB_b[a]
            P.op("pe", lambda e, dst=dst, kt=kt: e.matmul(
                dst, kb[:, h, kt * 128:(kt + 1) * 128], qb[:, h, qs], start=True, stop=True),
                reads=[kb_b[h], qb_b[h]], writes=[dst_b])
        P.op("dve", lambda e: e.scalar_tensor_tensor(
            out=tmp[a][:, 0:512], in0=psA[a][:, 0:512], scalar=scale, in1=bias[:, h, 0:512], op0=ALU.mult, op1=ALU.add),
            reads=[psA_b[a], bias_b], writes=[tmp_b[a]])
        P.op("dve", lambda e: e.scalar_tensor_tensor(
            out=tmp[a][:, 512:640], in0=psB[a][:, 0:128], scalar=scale, in1=bias[:, h, 512:640], op0=ALU.mult, op1=ALU.add),
            reads=[psB_b[a], bias_b], writes=[tmp_b[a]])
        P.op("act", lambda e: e.activation(out=pT[a][:], in_=tmp[a][:], func=AF.Exp),
             reads=[tmp_b[a]], writes=[pT_b[a]])

    def stage_b(h, qt, a):
        qs = slice(qt * 128, (qt + 1) * 128)
        oi = h % 2
        for i in range(5):
            kt = qt + i
            P.op("pe", lambda e, kt=kt, i=i: e.matmul(
                psO[a][:, 0:128], vb[:, kt, h * 128:(h + 1) * 128], pT[a][:, i * 128:(i + 1) * 128],
                start=(i == 0), stop=(i == 4)), reads=[vb_b[kt], pT_b[a]], writes=[psO_b[a]])
        for i in range(5):
            kt = qt + i
            on = von if kt < 4 else ones
            on_b = von_b if kt < 4 else ones_b
            P.op("pe", lambda e, on=on, i=i: e.matmul(
                psO[a][:, 128:256], on[:], pT[a][:, i * 128:(i + 1) * 128],
                start=(i == 0), stop=(i == 4)), reads=[on_b, pT_b[a]], writes=[psO_b[a]])
        P.op("dve", lambda e: e.reciprocal(rec[a][:], psO[a][:, 128:256]), reads=[psO_b[a]], writes=[rec_b[a]])
        P.op("dve", lambda e: e.tensor_tensor(
            out=ost[oi][:, qs], in0=psO[a][:, 0:128], in1=rec[a][:], op=ALU.mult),
            reads=[psO_b[a], rec_b[a]], writes=[ost_b[oi]])
        if qt == NQT - 1:
            P.dma("sp", lambda e: e.dma_start(out=odst[:, h, :], in_=ost[oi][:]), reads=[ost_b[oi]])

    units = [(h, qt) for h in range(C_HEADS) for qt in range(NQT)]
    stage_a(units[0][0], units[0][1], 0)
    for k in range(1, len(units)):
        stage_a(units[k][0], units[k][1], k % 2)
        stage_b(units[k - 1][0], units[k - 1][1], (k - 1) % 2)
    stage_b(units[-1][0], units[-1][1], (len(units) - 1) % 2)


def build_ATT():
    import contextlib
    nc = bass.Bass("TRN2", target_bir_lowering=False)
    with contextlib.ExitStack() as st:
        cx = Ctx(nc, st)
        qT = cx.din("qT", [D, TOKC])
        kT = cx.din("kT", [D, TOKC + HALO])
        v = cx.din("v", [TOKC + HALO, D])
        biasT = cx.din("biasT", [128, C_HEADS, 640])
        vones = cx.din("vones", [128, 128])
        oT = cx.dout("oT", [D, TOKC])
        emit_attention(cx, qT, kT, v, biasT, vones, oT)
        cx.finish()
    return nc


class TB:
    def __init__(self, cx, shape, dt, name):
        self.t = cx.sb(shape, dt, name)
        self.b = Buf(name)


S5_LC = 512
S5_NJ = 4


def s5_host_layout(inp, core):
    g0 = core * 8
    lam = np.zeros((128, S5_NJ, 3), np.float32)
    Bre = np.zeros((128, S5_NJ, 128), np.float32)
    Bim = np.zeros((128, S5_NJ, 128), np.float32)
    Cre = np.zeros((128, S5_NJ, 128), np.float32)
    Cim = np.zeros((128, S5_NJ, 128), np.float32)
    for j in range(S5_NJ):
        for gl in range(2):
            g8 = 2 * j + gl
            g = g0 + g8
            sl = slice(gl * 64, gl * 64 + 64)
            lam[sl, j, 0] = inp['s5_lam_re'][0, g]
            lam[sl, j, 1] = inp['s5_lam_im'][0, g]
            lam[sl, j, 2] = inp['s5_log_dt'][0, g]
            Bre[16 * g8:16 * g8 + 16, j, sl] = inp['s5_b_re'][0, g].T
            Bim[16 * g8:16 * g8 + 16, j, sl] = inp['s5_b_im'][0, g].T
            Cre[sl, j, 16 * g8:16 * g8 + 16] = inp['s5_c_re'][0, g].T
            Cim[sl, j, 16 * g8:16 * g8 + 16] = inp['s5_c_im'][0, g].T
    dsk = np.ascontiguousarray(inp['s5_d'][0, core * 128:(core + 1) * 128].reshape(128, 1))
    return {"lam": lam, "Bre": Bre, "Bim": Bim, "Cre": Cre, "Cim": Cim, "dsk": dsk}


def emit_s5(cx, uT, lam, Bre, Bim, Cre, Cim, dsk, yT, L=SEQ):
    P = cx.P
    LC, NJ = S5_LC, S5_NJ
    NCH = L // LC
    u32 = TB(cx, [128, L], F32, "u32")
    ub = TB(cx, [128, L], BF16, "ub")
    for c4 in range(4):
        sl = slice(c4 * (L // 4), (c4 + 1) * (L // 4))
        P.dma("sp", lambda e, sl=sl: e.dma_start(out=u32.t[:, sl], in_=uT[:, sl]), writes=[u32.b])
    for c4 in range(4):
        sl = slice(c4 * (L // 4), (c4 + 1) * (L // 4))
        P.op("act", lambda e, sl=sl: e.activation(out=ub.t[:, sl], in_=u32.t[:, sl], func=AF.Copy), reads=[u32.b], writes=[ub.b])
    lm = TB(cx, [128, NJ, 3], F32, "lam")
    P.dma("sp", lambda e: e.dma_start(out=lm.t[:], in_=lam), writes=[lm.b])
    dk = TB(cx, [128, 1], F32, "dsk")
    P.dma("sp", lambda e: e.dma_start(out=dk.t[:], in_=dsk), writes=[dk.b])
    mats = {}
    for nm, src in (("Bre", Bre), ("Bim", Bim), ("Cre", Cre), ("Cim", Cim)):
        m = TB(cx, [128, NJ, 128], BF16, "m" + nm)
        P.dma("pool", lambda e, m=m, src=src: e.dma_start(out=m.t[:], in_=src), writes=[m.b])
        mats[nm] = m
    P.op("dve", lambda e: e.tensor_scalar(out=mats["Cim"].t[:], in0=mats["Cim"].t[:], scalar1=-1.0, scalar2=None, op0=ALU.mult),
         reads=[mats["Cim"].b], writes=[mats["Cim"].b])
    def sc(name):
        return TB(cx, [128, NJ], F32, name)
    dt, lrdt, th, mag, den, rden = sc("dt"), sc("lrdt"), sc("th"), sc("mag"), sc("den"), sc("rden")
    abre, abim, nr, fre, fim, t1, t2 = sc("abre"), sc("abim"), sc("nr"), sc("fre"), sc("fim"), sc("t1"), sc("t2")
    lr, li, ldt = lm.t[:, :, 0], lm.t[:, :, 1], lm.t[:, :, 2]
    P.op("act", lambda e: e.activation(out=dt.t[:], in_=ldt, func=AF.Exp), reads=[lm.b], writes=[dt.b])
    P.op("dve", lambda e: e.tensor_tensor(out=lrdt.t[:], in0=lr, in1=dt.t[:], op=ALU.mult), reads=[lm.b, dt.b], writes=[lrdt.b])
    P.op("dve", lambda e: e.tensor_tensor(out=th.t[:], in0=li, in1=dt.t[:], op=ALU.mult), reads=[lm.b, dt.b], writes=[th.b])
    P.op("act", lambda e: e.activation(out=mag.t[:], in_=lrdt.t[:], func=AF.Exp), reads=[lrdt.b], writes=[mag.b])
    NLV = 16
    Wre = TB(cx, [128, NLV, NJ], F32, "Wre")
    Wim = TB(cx, [128, NLV, NJ], F32, "Wim")
    hpi = TB(cx, [128, 1], F32, "hpi")
    P.op("dve", lambda e: e.memset(hpi.t[:], float(np.pi / 2)), writes=[hpi.b])
    P.op("act", lambda e: e.activation(out=Wim.t[:, 0, :], in_=th.t[:], func=AF.Sin, scale=1.0 / 64), reads=[th.b], writes=[Wim.b])
    P.op("act", lambda e: e.activation(out=Wre.t[:, 0, :], in_=th.t[:], func=AF.Sin, scale=1.0 / 64, bias=hpi.t[:, 0:1]),
         reads=[th.b, hpi.b], writes=[Wre.b])
    for lv in range(1, NLV):
        a, b_ = Wre.t[:, lv - 1, :], Wim.t[:, lv - 1, :]
        P.op("dve", lambda e, a=a: e.tensor_tensor(out=t1.t[:], in0=a, in1=a, op=ALU.mult), reads=[Wre.b], writes=[t1.b])
        P.op("dve", lambda e, b_=b_: e.tensor_tensor(out=t2.t[:], in0=b_, in1=b_, op=ALU.mult), reads=[Wim.b], writes=[t2.b])
        P.op("dve", lambda e, lv=lv: e.tensor_tensor(out=Wre.t[:, lv, :], in0=t1.t[:], in1=t2.t[:], op=ALU.subtract),
             reads=[t1.b, t2.b], writes=[Wre.b])
        P.op("dve", lambda e, lv=lv, a=a, b_=b_: e.scalar_tensor_tensor(out=Wim.t[:, lv, :], in0=a, scalar=2.0, in1=b_, op0=ALU.mult, op1=ALU.mult),
             reads=[Wre.b, Wim.b], writes=[Wim.b])
    cth, sth = Wre.t[:, 6, :], Wim.t[:, 6, :]
    P.op("dve", lambda e: e.tensor_tensor(out=abre.t[:], in0=mag.t[:], in1=cth, op=ALU.mult), reads=[mag.b, Wre.b], writes=[abre.b])
    P.op("dve", lambda e: e.tensor_tensor(out=abim.t[:], in0=mag.t[:], in1=sth, op=ALU.mult), reads=[mag.b, Wim.b], writes=[abim.b])
    P.op("dve", lambda e: e.tensor_scalar(out=nr.t[:], in0=abre.t[:], scalar1=-1.0, scalar2=None, op0=ALU.add), reads=[abre.b], writes=[nr.b])
    P.op("dve", lambda e: e.tensor_tensor(out=t1.t[:], in0=lr, in1=lr, op=ALU.mult), reads=[lm.b], writes=[t1.b])
    P.op("dve", lambda e: e.tensor_tensor(out=t2.t[:], in0=li, in1=li, op=ALU.mult), reads=[lm.b], writes=[t2.b])
    P.op("dve", lambda e: e.tensor_tensor(out=den.t[:], in0=t1.t[:], in1=t2.t[:], op=ALU.add), reads=[t1.b, t2.b], writes=[den.b])
    P.op("dve", lambda e: e.reciprocal(rden.t[:], den.t[:]), reads=[den.b], writes=[rden.b])
    P.op("dve", lambda e: e.tensor_tensor(out=t1.t[:], in0=nr.t[:], in1=lr, op=ALU.mult), reads=[nr.b, lm.b], writes=[t1.b])
    P.op("dve", lambda e: e.tensor_tensor(out=t2.t[:], in0=abim.t[:], in1=li, op=ALU.mult), reads=[abim.b, lm.b], writes=[t2.b])
    P.op("dve", lambda e: e.tensor_tensor(out=t1.t[:], in0=t1.t[:], in1=t2.t[:], op=ALU.add), reads=[t1.b, t2.b], writes=[t1.b])
    P.op("dve", lambda e: e.tensor_tensor(out=fre.t[:], in0=t1.t[:], in1=rden.t[:], op=ALU.mult), reads=[t1.b, rden.b], writes=[fre.b])
    P.op("dve", lambda e: e.tensor_tensor(out=t1.t[:], in0=abim.t[:], in1=lr, op=ALU.mult), reads=[abim.b, lm.b], writes=[t1.b])
    P.op("dve", lambda e: e.tensor_tensor(out=t2.t[:], in0=nr.t[:], in1=li, op=ALU.mult), reads=[nr.b, lm.b], writes=[t2.b])
    P.op("dve", lambda e: e.tensor_tensor(out=t1.t[:], in0=t1.t[:], in1=t2.t[:], op=ALU.subtract), reads=[t1.b, t2.b], writes=[t1.b])
    P.op("dve", lambda e: e.tensor_tensor(out=fim.t[:], in0=t1.t[:], in1=rden.t[:], op=ALU.mult), reads=[t1.b, rden.b], writes=[fim.b])
    cosT = TB(cx, [128, NJ, LC], F32, "cosT")
    sinT = TB(cx, [128, NJ, LC], F32, "sinT")
    Gre = TB(cx, [128, NJ, LC], F32, "Gre")
    Gim = TB(cx, [128, NJ, LC], F32, "Gim")
    rho = TB(cx, [128, NJ, LC], F32, "rho")
    tw = TB(cx, [128, LC], F32, "tw")
    P.op("pool", lambda e: e.memset(cosT.t[:, :, 0:1], 1.0), writes=[cosT.b])
    P.op("pool", lambda e: e.memset(sinT.t[:, :, 0:1], 0.0), writes=[sinT.b])
    P.op("pool", lambda e: e.memset(rho.t[:], 1.0), writes=[rho.b])
    for j in range(NJ):
        P.op("dve", lambda e, j=j: e.tensor_scalar(out=rho.t[:, j, :], in0=rho.t[:, j, :], scalar1=mag.t[:, j:j + 1], scalar2=None, op0=ALU.mult),
             reads=[rho.b, mag.b], writes=[rho.b])
        m = 1
        lv = 6
        while m < LC:
            wre, wim = Wre.t[:, lv, j:j + 1], Wim.t[:, lv, j:j + 1]
            src_re, src_im = cosT.t[:, j, 0:m], sinT.t[:, j, 0:m]
            P.op("dve", lambda e, m=m, wim=wim, src_im=src_im: e.tensor_scalar(out=tw.t[:, 0:m], in0=src_im, scalar1=wim, scalar2=None, op0=ALU.mult),
                 reads=[sinT.b, Wim.b], writes=[tw.b])
            P.op("dve", lambda e, m=m, j=j, wre=wre, src_re=src_re: e.scalar_tensor_tensor(
                out=cosT.t[:, j, m:2 * m], in0=src_re, scalar=wre, in1=tw.t[:, 0:m], op0=ALU.mult, op1=ALU.subtract),
                reads=[cosT.b, Wre.b, tw.b], writes=[cosT.b])
            P.op("dve", lambda e, m=m, wre=wre, src_im=src_im: e.tensor_scalar(out=tw.t[:, 0:m], in0=src_im, scalar1=wre, scalar2=None, op0=ALU.mult),
                 reads=[sinT.b, Wre.b], writes=[tw.b])
            P.op("dve", lambda e, m=m, j=j, wim=wim, src_re=src_re: e.scalar_tensor_tensor(
                out=sinT.t[:, j, m:2 * m], in0=src_re, scalar=wim, in1=tw.t[:, 0:m], op0=ALU.mult, op1=ALU.add),
                reads=[cosT.b, Wim.b, tw.b], writes=[sinT.b])
            m *= 2
            lv += 1
        P.op("dve", lambda e, j=j: e.tensor_scalar(out=tw.t[:], in0=sinT.t[:, j, :], scalar1=fim.t[:, j:j + 1], scalar2=None, op0=ALU.mult),
             reads=[sinT.b, fim.b], writes=[tw.b])
        P.op("dve", lambda e, j=j: e.scalar_tensor_tensor(out=Gre.t[:, j, :], in0=cosT.t[:, j, :], scalar=fre.t[:, j:j + 1], in1=tw.t[:],
                                                          op0=ALU.mult, op1=ALU.add), reads=[cosT.b, fre.b, tw.b], writes=[Gre.b])
        P.op("dve", lambda e, j=j: e.tensor_scalar(out=tw.t[:], in0=sinT.t[:, j, :], scalar1=fre.t[:, j:j + 1], scalar2=None, op0=ALU.mult),
             reads=[sinT.b, fre.b], writes=[tw.b])
        P.op("dve", lambda e, j=j: e.scalar_tensor_tensor(out=Gim.t[:, j, :], in0=cosT.t[:, j, :], scalar=fim.t[:, j:j + 1], in1=tw.t[:],
                                                          op0=ALU.mult, op1=ALU.subtract), reads=[cosT.b, fim.b, tw.b], writes=[Gim.b])
    Ere, Eim = Wre.t[:, 15, :], Wim.t[:, 15, :]
    NW = 3
    W = []
    for i in range(NW):
        d = {}
        for nm in ("sre", "sim", "a", "b", "a2", "b2", "cre", "cim", "zre", "zim"):
            d[nm] = TB(cx, [128, LC], F32, "%s%d" % (nm, i))
        for nm in ("xre", "xim"):
            d[nm] = TB(cx, [128, LC], BF16, "%s%d" % (nm, i))
        d["pre"] = cx.ps(name="s5pre%d" % i)
        d["pim"] = cx.ps(name="s5pim%d" % i)
        d["pre_b"], d["pim_b"] = Buf("pre"), Buf("pim")
        W.append(d)
    psY = [cx.ps(name="s5y%d" % i) for i in range(2)]
    psY_b = [Buf("psY%d" % i) for i in range(2)]
    init = [[TB(cx, [128, 2], F32, "init%d_%d" % (j, k)) for k in range(2)] for j in range(NJ)]
    for j in range(NJ):
        P.op("pool", lambda e, j=j: e.memset(init[j][0].t[:], 0.0), writes=[init[j][0].b])
    ct = TB(cx, [128, 2], F32, "ct")
    yw = [{nm: TB(cx, [128, LC], F32, "y%s%d" % (nm, i)) for nm in ("y", "y2", "v", "s", "o")} for i in range(2)]
    un = 0
    for ch in range(NCH):
        ts_ = slice(ch * LC, (ch + 1) * LC)
        py, py_b = psY[ch % 2], psY_b[ch % 2]
        for j in range(NJ):
            w = W[un % NW]
            un += 1
            ini, nini = init[j][ch % 2], init[j][(ch + 1) % 2]
            P.op("pe", lambda e, w=w, j=j, ts_=ts_: e.matmul(w["pre"][:], mats["Bre"].t[:, j, :], ub.t[:, ts_], start=True, stop=True),
                 reads=[mats["Bre"].b, ub.b], writes=[w["pre_b"]])
            P.op("pe", lambda e, w=w, j=j, ts_=ts_: e.matmul(w["pim"][:], mats["Bim"].t[:, j, :], ub.t[:, ts_], start=True, stop=True),
                 reads=[mats["Bim"].b, ub.b], writes=[w["pim_b"]])
            P.op("act", lambda e, w=w: e.activation(out=w["sre"].t[:], in_=w["pre"][:], func=AF.Copy), reads=[w["pre_b"]], writes=[w["sre"].b])
            P.op("act", lambda e, w=w: e.activation(out=w["sim"].t[:], in_=w["pim"][:], func=AF.Copy), reads=[w["pim_b"]], writes=[w["sim"].b])
            def tt(eng, out, in0, in1, op):
                P.op(eng, lambda e: e.tensor_tensor(out=out.t[:], in0=in0[0], in1=in1[0], op=op),
                     reads=[in0[1], in1[1]], writes=[out.b])
            gre, gim = (Gre.t[:, j, :], Gre.b), (Gim.t[:, j, :], Gim.b)
            cs, sn = (cosT.t[:, j, :], cosT.b), (sinT.t[:, j, :], sinT.b)
            S = lambda x: (x.t[:], x.b)
            tt("pool", w["a2"], gre, S(w["sre"]), ALU.mult)
            tt("pool", w["b2"], gim, S(w["sim"]), ALU.mult)
            tt("pool", w["cre"], S(w["a2"]), S(w["b2"]), ALU.subtract)
            tt("pool", w["a2"], gre, S(w["sim"]), ALU.mult)
            tt("pool", w["b2"], gim, S(w["sre"]), ALU.mult)
            tt("pool", w["cim"], S(w["a2"]), S(w["b2"]), ALU.add)
            P.op("dve", lambda e, w=w, j=j, ini=ini: e.tensor_tensor_scan(w["zre"].t[:], rho.t[:, j, :], w["cre"].t[:], ini.t[:, 0:1], ALU.mult, ALU.add),
                 reads=[rho.b, w["cre"].b, ini.b], writes=[w["zre"].b])
            P.op("dve", lambda e, w=w, j=j, ini=ini: e.tensor_tensor_scan(w["zim"].t[:], rho.t[:, j, :], w["cim"].t[:], ini.t[:, 1:2], ALU.mult, ALU.add),
                 reads=[rho.b, w["cim"].b, ini.b], writes=[w["zim"].b])
            zre_e, zim_e = w["zre"].t[:, LC - 1:LC], w["zim"].t[:, LC - 1:LC]
            ere, eim = Ere[:, j:j + 1], Eim[:, j:j + 1]
            P.op("dve", lambda e, zim_e=zim_e, eim=eim: e.tensor_scalar(out=ct.t[:, 0:1], in0=zim_e, scalar1=eim, scalar2=None, op0=ALU.mult),
                 reads=[w["zim"].b, Wim.b], writes=[ct.b])
            P.op("dve", lambda e, zre_e=zre_e, ere=ere, nini=nini: e.scalar_tensor_tensor(
                out=nini.t[:, 0:1], in0=zre_e, scalar=ere, in1=ct.t[:, 0:1], op0=ALU.mult, op1=ALU.subtract),
                reads=[w["zre"].b, Wre.b, ct.b], writes=[nini.b])
            P.op("dve", lambda e, zre_e=zre_e, eim=eim: e.tensor_scalar(out=ct.t[:, 1:2], in0=zre_e, scalar1=eim, scalar2=None, op0=ALU.mult),
                 reads=[w["zre"].b, Wim.b], writes=[ct.b])
            P.op("dve", lambda e, zim_e=zim_e, ere=ere, nini=nini: e.scalar_tensor_tensor(
                out=nini.t[:, 1:2], in0=zim_e, scalar=ere, in1=ct.t[:, 1:2], op0=ALU.mult, op1=ALU.add),
                reads=[w["zim"].b, Wre.b, ct.b], writes=[nini.b])
            tt("dve", w["a"], cs, S(w["zre"]), ALU.mult)
            tt("dve", w["b"], sn, S(w["zim"]), ALU.mult)
            tt("dve", w["xre"], S(w["a"]), S(w["b"]), ALU.subtract)
            tt("dve", w["a"], sn, S(w["zre"]), ALU.mult)
            tt("dve", w["b"], cs, S(w["zim"]), ALU.mult)
            tt("dve", w["xim"], S(w["a"]), S(w["b"]), ALU.add)
            P.op("pe", lambda e, w=w, j=j, py=py: e.matmul(py[:], mats["Cre"].t[:, j, :], w["xre"].t[:], start=(j == 0), stop=False),
                 reads=[mats["Cre"].b, w["xre"].b], writes=[py_b])
            P.op("pe", lambda e, w=w, j=j, py=py: e.matmul(py[:], mats["Cim"].t[:, j, :], w["xim"].t[:], start=False, stop=(j == NJ - 1)),
                 reads=[mats["Cim"].b, w["xim"].b], writes=[py_b])
        Y = yw[ch % 2]
        P.op("dve", lambda e, Y=Y, py=py, ts_=ts_: e.scalar_tensor_tensor(out=Y["y"].t[:], in0=u32.t[:, ts_], scalar=dk.t[:, 0:1], in1=py[:],
                                                                         op0=ALU.mult, op1=ALU.add), reads=[u32.b, dk.b, py_b], writes=[Y["y"].b])
        P.op("pool", lambda e, Y=Y: e.tensor_tensor(out=Y["y2"].t[:], in0=Y["y"].t[:], in1=Y["y"].t[:], op=ALU.mult), reads=[Y["y"].b], writes=[Y["y2"].b])
        P.op("pool", lambda e, Y=Y: e.tensor_scalar(out=Y["y2"].t[:], in0=Y["y2"].t[:], scalar1=0.044715, scalar2=1.0, op0=ALU.mult, op1=ALU.add),
             reads=[Y["y2"].b], writes=[Y["y2"].b])
        P.op("pool", lambda e, Y=Y: e.tensor_tensor(out=Y["v"].t[:], in0=Y["y2"].t[:], in1=Y["y"].t[:], op=ALU.mult), reads=[Y["y2"].b, Y["y"].b], writes=[Y["v"].b])
        P.op("act", lambda e, Y=Y: e.activation(out=Y["s"].t[:], in_=Y["v"].t[:], func=AF.Sigmoid, scale=1.5957691216057308),
             reads=[Y["v"].b], writes=[Y["s"].b])
        P.op("pool", lambda e, Y=Y: e.tensor_tensor(out=Y["o"].t[:], in0=Y["s"].t[:], in1=Y["y"].t[:], op=ALU.mult), reads=[Y["s"].b, Y["y"].b], writes=[Y["o"].b])
        P.dma("sp", lambda e, Y=Y, ts_=ts_: e.dma_start(out=yT[:, ts_], in_=Y["o"].t[:]), reads=[Y["o"].b])


def build_S5():
    import contextlib
    nc = bass.Bass("TRN2", target_bir_lowering=False)
    with contextlib.ExitStack() as st:
        cx = Ctx(nc, st)
        uT = cx.din("uT", [128, SEQ])
        lam = cx.din("lam", [128, S5_NJ, 3])
        Bre = cx.din("Bre", [128, S5_NJ, 128])
        Bim = cx.din("Bim", [128, S5_NJ, 128])
        Cre = cx.din("Cre", [128, S5_NJ, 128])
        Cim = cx.din("Cim", [128, S5_NJ, 128])
        dsk = cx.din("dsk", [128, 1])
        yT = cx.dout("yT", [128, SEQ])
        emit_s5(cx, uT, lam, Bre, Bim, Cre, Cim, dsk, yT)
        cx.finish()
    return nc


NSLOT = 8
TOPK = 256
NBIS = 16


def slot_tiles(i):
    return 8 * (i + 1)


def slot_off(i):
    return 4 * i * (i + 1)


NT_TOTAL = slot_off(NSLOT)


def dsa_negpos(core):
    return (np.arange(SEQ)[None, :] - 128 * core - np.arange(128)[:, None]).astype(np.float32)


def dsa_adm_mask(core):
    r = np.arange(1024)[None, :]
    ql = np.arange(128)[:, None]
    kch = r // 64
    qch = (128 * core + ql) // 64
    return np.where(kch <= qch, 0.0, -1e30).astype(np.float32)


DBG = {"nslot": 8, "bis": NBIS, "tr": True, "idx": True}


def emit_dsa_index(cx, kidxT, qidxT, widx, adm, pow2, ident, selT_out, negpos=None, dmin_out=None):
    P = cx.P
    S = SEQ
    kb = TB(cx, [64, S], BF16, "ikb")
    qb = TB(cx, [64, 16, NSLOT * 128], BF16, "iqb")
    for c4 in range(4):
        sl = slice(c4 * 2048, (c4 + 1) * 2048)
        P.dma("pool", lambda e, sl=sl: e.dma_start(out=kb.t[:, sl], in_=kidxT[:, sl]), writes=[kb.b])
    qsrc = qidxT.rearrange("(h d) t -> d h t", d=64)
    for h4 in range(4):
        P.dma("pool", lambda e, h4=h4: e.dma_start(out=qb.t[:, h4 * 4:(h4 + 1) * 4, :], in_=qsrc[:, h4 * 4:(h4 + 1) * 4, :]), writes=[qb.b])
    w = TB(cx, [128, NSLOT, 16], F32, "iw")
    wa = TB(cx, [128, NSLOT, 16], F32, "iwa")
    wsg = TB(cx, [128, NSLOT, 16], F32, "iws")
    am = TB(cx, [128, 1024], F32, "iadm")
    p2 = TB(cx, [128, NBIS + 1], F32, "ip2")
    idf = TB(cx, [128, 128], F32, "iidf")
    idb = TB(cx, [128, 128], BF16, "iidb")
    P.dma("sp", lambda e: e.dma_start(out=w.t[:], in_=widx), writes=[w.b])
    P.dma("sp", lambda e: e.dma_start(out=am.t[:], in_=adm), writes=[am.b])
    P.dma("sp", lambda e: e.dma_start(out=p2.t[:], in_=pow2), writes=[p2.b])
    P.dma("sp", lambda e: e.dma_start(out=idf.t[:], in_=ident), writes=[idf.b])
    P.op("dve", lambda e: e.tensor_copy(idb.t[:], idf.t[:]), reads=[idf.b], writes=[idb.b])
    P.op("act", lambda e: e.activation(out=wa.t[:], in_=w.t[:], func=AF.Abs), reads=[w.b], writes=[wa.b])
    P.op("dve", lambda e: e.tensor_scalar(out=wsg.t[:], in0=w.t[:], scalar1=0.0, scalar2=2.0, op0=ALU.is_ge, op1=ALU.mult),
         reads=[w.b], writes=[wsg.b])
    P.op("dve", lambda e: e.tensor_scalar(out=wsg.t[:], in0=wsg.t[:], scalar1=-1.0, scalar2=None, op0=ALU.add), reads=[wsg.b], writes=[wsg.b])
    sc = TB(cx, [128, S], F32, "isc")
    jk = TB(cx, [128, S], BF16, "ijk")
    rr = [TB(cx, [128, 512], F32, "irr%d" % i) for i in range(3)]
    ps = [cx.ps(name="ips%d" % i) for i in range(4)]
    ps_b = [Buf("ips%d" % i) for i in range(4)]
    pst = [cx.ps([128, 512], BF16, name="ipt%d" % i) for i in range(2)]
    pst_b = [Buf("ipt%d" % i) for i in range(2)]
    stg = [TB(cx, [128, 4, 128], BF16, "istg%d" % i) for i in range(2)]
    M = TB(cx, [128, 1], F32, "iM")
    steps = TB(cx, [128, NBIS + 1], F32, "isteps")
    nsteps = TB(cx, [128, NBIS + 1], F32, "insteps")
    mid = [TB(cx, [128, 1], F32, "imid%d" % i) for i in range(2)]
    cnt = TB(cx, [128, 1], F32, "icnt")
    dd = TB(cx, [128, 1], F32, "idd")
    n = 0
    nt = 0
    for i in range(DBG["nslot"]):
        Si = 1024 * (i + 1)
        qs = slice(i * 128, (i + 1) * 128)
        for kbk in range(Si // 512 if DBG["idx"] else 0):
            ks = slice(kbk * 512, (kbk + 1) * 512)
            for h in range(16):
                pp, pp_b = ps[n % 4], ps_b[n % 4]
                r = rr[n % 3]
                n += 1
                P.op("pe", lambda e, pp=pp, h=h, qs=qs, ks=ks: e.matmul(pp[:], qb.t[:, h, qs], kb.t[:, ks], start=True, stop=True),
                     reads=[qb.b, kb.b], writes=[pp_b])
                P.op("act", lambda e, pp=pp, r=r, i=i, h=h: e.activation(out=r.t[:], in_=pp[:], func=AF.Relu, scale=wa.t[:, i, h:h + 1]),
                     reads=[pp_b, wa.b], writes=[r.b])
                if h == 0:
                    P.op("dve", lambda e, r=r, i=i, h=h, ks=ks: e.tensor_scalar(out=sc.t[:, ks], in0=r.t[:], scalar1=wsg.t[:, i, h:h + 1], scalar2=None, op0=ALU.mult),
                         reads=[r.b, wsg.b], writes=[sc.b])
                else:
                    P.op("dve", lambda e, r=r, i=i, h=h, ks=ks: e.scalar_tensor_tensor(out=sc.t[:, ks], in0=r.t[:], scalar=wsg.t[:, i, h:h + 1], in1=sc.t[:, ks],
                                                                                       op0=ALU.mult, op1=ALU.add), reads=[r.b, wsg.b, sc.b], writes=[sc.b])
        P.op("dve", lambda e, Si=Si: e.tensor_reduce(out=M.t[:], in_=sc.t[:, 0:Si], axis=AX.X, op=ALU.max, apply_absolute_value=True), reads=[sc.b], writes=[M.b])
        P.op("dve", lambda e: e.tensor_scalar(out=M.t[:], in0=M.t[:], scalar1=1.001, scalar2=1e-20, op0=ALU.mult, op1=ALU.add), reads=[M.b], writes=[M.b])
        P.op("dve", lambda e, Si=Si: e.tensor_tensor(out=sc.t[:, Si - 1024:Si], in0=sc.t[:, Si - 1024:Si], in1=am.t[:], op=ALU.add),
             reads=[sc.b, am.b], writes=[sc.b])
        P.op("dve", lambda e: e.tensor_scalar(out=steps.t[:], in0=p2.t[:], scalar1=M.t[:, 0:1], scalar2=None, op0=ALU.mult), reads=[p2.b, M.b], writes=[steps.b])
        P.op("dve", lambda e: e.tensor_scalar(out=nsteps.t[:], in0=steps.t[:], scalar1=-1.0, scalar2=None, op0=ALU.mult), reads=[steps.b], writes=[nsteps.b])
        P.op("dve", lambda e: e.memset(mid[0].t[:], 0.0), writes=[mid[0].b])
        for k in range(DBG["bis"]):
            m0, m1 = mid[k % 2], mid[(k + 1) % 2]
            P.op("dve", lambda e, Si=Si, m0=m0: e.tensor_scalar(out=jk.t[:, 0:Si], in0=sc.t[:, 0:Si], scalar1=m0.t[:, 0:1], scalar2=0.0,
                                                              op0=ALU.is_gt, op1=ALU.add, accum_out=cnt.t[:, 0:1]),
                 reads=[sc.b, m0.b], writes=[jk.b, cnt.b])
            P.op("dve", lambda e, k=k: e.tensor_scalar(out=dd.t[:], in0=cnt.t[:], scalar1=float(TOPK), scalar2=steps.t[:, k:k + 1],
                                                       op0=ALU.is_ge, op1=ALU.mult), reads=[cnt.b, steps.b], writes=[dd.b])
            P.op("dve", lambda e, k=k, m0=m0, m1=m1: e.scalar_tensor_tensor(out=m1.t[:], in0=dd.t[:], scalar=nsteps.t[:, k + 1:k + 2], in1=m0.t[:],
                                                                          op0=ALU.add, op1=ALU.add), reads=[dd.b, nsteps.b, m0.b], writes=[m1.b])
        mf = mid[DBG["bis"] % 2]
        P.op("dve", lambda e, Si=Si, mf=mf: e.tensor_scalar(out=jk.t[:, 0:Si], in0=sc.t[:, 0:Si], scalar1=mf.t[:, 0:1], scalar2=-30000.0,
                                                          op0=ALU.is_le, op1=ALU.mult), reads=[sc.b, mf.b], writes=[jk.b])
        if negpos is not None:
            if i == 0:
                npos = TB(cx, [128, S], F32, "inpos")
                P.dma("sp", lambda e: e.dma_start(out=npos.t[:], in_=negpos), writes=[npos.b])
                offs = TB(cx, [128, NSLOT], F32, "ioffs")
                for ii in range(NSLOT):
                    P.op("pool", lambda e, ii=ii: e.memset(offs.t[:, ii:ii + 1], -1024.0 * ii), writes=[offs.b])
                dm = TB(cx, [128, NSLOT], F32, "idmin")
                emit_dsa_index._st = (npos, offs, dm)
            npos, offs, dm = emit_dsa_index._st
            P.op("act", lambda e, Si=Si, i=i: e.activation(out=sc.t[:, 0:Si], in_=npos.t[:, 0:Si], func=AF.Abs, bias=offs.t[:, i:i + 1], scale=1.0),
                 reads=[npos.b, offs.b, sc.b], writes=[sc.b])
            P.op("pool", lambda e, Si=Si: e.tensor_tensor(out=sc.t[:, 0:Si], in0=jk.t[:, 0:Si], in1=sc.t[:, 0:Si], op=ALU.subtract),
                 reads=[jk.b, sc.b], writes=[sc.b])
            P.op("dve", lambda e, Si=Si, i=i: e.tensor_reduce(out=dm.t[:, i:i + 1], in_=sc.t[:, 0:Si], axis=AX.X, op=ALU.max),
                 reads=[sc.b], writes=[dm.b])
            if i == DBG["nslot"] - 1:
                P.dma("sp", lambda e: e.dma_start(out=dmin_out, in_=dm.t[:]), reads=[dm.b])
        for g in range(Si // 512 if DBG["tr"] else 0):
            pt, pt_b = pst[nt % 2], pst_b[nt % 2]
            sg = stg[nt % 2]
            nt += 1
            for j in range(4):
                tix = g * 4 + j
                P.op("pe", lambda e, pt=pt, j=j, tix=tix: e.transpose(pt[:, j * 128:(j + 1) * 128], jk.t[:, tix * 128:(tix + 1) * 128], idb.t[:]),
                     reads=[jk.b, idb.b], writes=[pt_b])
            P.op("act", lambda e, pt=pt, sg=sg: e.activation(out=sg.t[:].rearrange("p a b -> p (a b)"), in_=pt[:], func=AF.Copy),
                 reads=[pt_b], writes=[sg.b])
            t0 = slot_off(i) + g * 4
            P.dma("sp", lambda e, sg=sg, t0=t0: e.dma_start(out=selT_out[:, t0:t0 + 4, :], in_=sg.t[:]), reads=[sg.b])


def build_C1():
    import contextlib
    nc = bass.Bass("TRN2", target_bir_lowering=False)
    with contextlib.ExitStack() as st:
        cx = Ctx(nc, st)
        kidxT = cx.din("kidxT", [64, SEQ])
        qidxT = cx.din("qidxT", [1024, NSLOT * 128])
        widx = cx.din("widx", [128, NSLOT, 16])
        adm = cx.din("adm", [128, 1024])
        pow2 = cx.din("pow2", [128, NBIS + 1])
        ident = cx.din("ident", [128, 128])
        selT = cx.dout("selT", [128, NT_TOTAL, 128], BF16)
        negpos = cx.din("negpos", [128, SEQ])
        dmin = cx.dout("ndmin", [128, NSLOT])
        emit_dsa_index(cx, kidxT, qidxT, widx, adm, pow2, ident, selT, negpos, dmin)
        cx.finish()
    return nc


B_HEADS = 8
KV_RANK = 256


def dsa_alibi_tables(core):
    slopes = (2.0 ** (-8.0 * (np.arange(8) + 1) / 8)).astype(np.float32)
    s_l = np.arange(128)[:, None, None]
    idx = np.arange(64)[None, None, :]
    rel = s_l + 128 * (idx - 56 - core) - 64
    biascol = (slopes[None, :, None] * np.minimum(rel, 63)).astype(np.float32)
    corr = np.zeros((128, 8, 8, 128), np.float32)
    sl = np.arange(128)[:, None]
    ql = np.arange(128)[None, :]
    corr[:, core, :, :] = -2.0 * slopes[None, :, None] * np.maximum(sl - ql, 0)[:, None, :]
    return biascol, corr


def emit_dsa_attn(cx, kvT, kvg, wkv, qT, selT, biascol, corr, ident, ybT, ndminT=None, qoff=None):
    P = cx.P
    slopes = [2.0 ** (-8.0 * (h + 1) / 8) for h in range(B_HEADS)]
    nd = TB(cx, [1, NSLOT * 128], F32, "dnd")
    qo = TB(cx, [1, 128], F32, "dqo")
    rrow = TB(cx, [1, B_HEADS, NSLOT * 128], BF16, "drrow")
    ones1 = TB(cx, [1, 128], BF16, "dones1")
    P.dma("sp", lambda e: e.dma_start(out=nd.t[:], in_=ndminT), writes=[nd.b])
    P.dma("sp", lambda e: e.dma_start(out=qo.t[:], in_=qoff), writes=[qo.b])
    P.op("pool", lambda e: e.memset(ones1.t[:], 1.0), writes=[ones1.b])
    for i in range(NSLOT):
        P.op("pool", lambda e, i=i: e.tensor_tensor(out=nd.t[:, i * 128:(i + 1) * 128], in0=qo.t[:], in1=nd.t[:, i * 128:(i + 1) * 128], op=ALU.subtract),
             reads=[qo.b, nd.b], writes=[nd.b])
    for h in range(B_HEADS):
        P.op("pool", lambda e, h=h: e.tensor_scalar(out=rrow.t[:, h, :], in0=nd.t[:], scalar1=float(slopes[h]), scalar2=None, op0=ALU.mult),
             reads=[nd.b], writes=[rrow.b])
    S = SEQ
    NKB = S // 512
    kvn = TB(cx, [128, 2, S], BF16, "dkvn")
    Wb = TB(cx, [128, 2, 2048], BF16, "dW")
    P.dma("pool", lambda e: e.dma_start(out=Wb.t[:], in_=wkv.rearrange("(k p) n -> p k n", p=128)), writes=[Wb.b])
    g = TB(cx, [128, 2], F32, "dg")
    P.dma("sp", lambda e: e.dma_start(out=g.t[:], in_=kvg), writes=[g.b])
    bc = TB(cx, [128, B_HEADS, 64], F32, "dbc")
    P.dma("sp", lambda e: e.dma_start(out=bc.t[:], in_=biascol), writes=[bc.b])
    cr = TB(cx, [128, 8, B_HEADS, 128], BF16, "dcorr")
    P.dma("pool", lambda e: e.dma_start(out=cr.t[:], in_=corr), writes=[cr.b])
    idb = TB(cx, [128, 128], BF16, "didb")
    P.dma("pool", lambda e: e.dma_start(out=idb.t[:], in_=ident), writes=[idb.b])
    ones = TB(cx, [128, 128], BF16, "dones")
    P.op("dve", lambda e: e.memset(ones.t[:], 1.0), writes=[ones.b])
    qb = TB(cx, [128, B_HEADS, NSLOT * 128], BF16, "dqb")
    P.dma("pool", lambda e: e.dma_start(out=qb.t[:], in_=qT.rearrange("(h p) t -> p h t", p=128)), writes=[qb.b])
    P.op("act", lambda e: e.activation(out=qb.t[:], in_=qb.t[:], func=AF.Identity, scale=float(HD) ** -0.5), reads=[qb.b], writes=[qb.b])
    kv32 = [TB(cx, [128, 2, 512], F32, "dkv32_%d" % i) for i in range(2)]
    sq = [TB(cx, [128, 512], BF16, "dsq%d" % i) for i in range(2)]
    rt = [TB(cx, [128, 512], F32, "drt%d" % i) for i in range(2)]
    pkv = [cx.ps(name="dpkv%d" % i) for i in range(2)]
    pkv_b = [Buf("dpkv%d" % i) for i in range(2)]
    pst, pst_b = pkv[0], pkv_b[0]
    kvsrc = kvT.rearrange("(k p) t -> p k t", p=128)
    for blk in range(NKB):
        bs = slice(blk * 512, (blk + 1) * 512)
        kv = kv32[blk % 2]
        r = rt[blk % 2]
        P.dma("sp", lambda e, kv=kv, bs=bs: e.dma_start(out=kv.t[:], in_=kvsrc[:, :, bs]), writes=[kv.b])
        for kc in range(2):
            s_ = sq[kc]
            P.op("act", lambda e, kv=kv, kc=kc, s_=s_: e.activation(out=s_.t[:], in_=kv.t[:, kc, :], func=AF.Square), reads=[kv.b], writes=[s_.b])
            P.op("pe", lambda e, kc=kc, s_=s_: e.matmul(pst[:], ones.t[:], s_.t[:], start=(kc == 0), stop=(kc == 1)), reads=[ones.b, s_.b], writes=[pst_b])
        P.op("dve", lambda e, r=r: e.tensor_scalar(out=r.t[:], in0=pst[:], scalar1=1.0 / KV_RANK, scalar2=EPS, op0=ALU.mult, op1=ALU.add), reads=[pst_b], writes=[r.b])
        P.op("act", lambda e, r=r: e.activation(out=r.t[:], in_=r.t[:], func=AF.Sqrt), reads=[r.b], writes=[r.b])
        P.op("dve", lambda e, r=r: e.reciprocal(r.t[:], r.t[:]), reads=[r.b], writes=[r.b])
        for kc in range(2):
            P.op("dve", lambda e, kv=kv, kc=kc, r=r, bs=bs: e.scalar_tensor_tensor(out=kvn.t[:, kc, bs], in0=kv.t[:, kc, :], scalar=g.t[:, kc:kc + 1], in1=r.t[:],
                                                                                   op0=ALU.mult, op1=ALU.mult), reads=[kv.b, g.b, r.b], writes=[kvn.b])
    Kh = TB(cx, [128, S], BF16, "dKh")
    Vh = TB(cx, [128, S // 128, 128], BF16, "dVh")
    pL = [cx.ps(name="dpL%d" % i) for i in range(2)]
    pL_b = [Buf("dpL%d" % i) for i in range(2)]
    pOD = [cx.ps(name="dpOD%d" % i) for i in range(2)]
    pOD_b = [Buf("dpOD%d" % i) for i in range(2)]
    pDD = [cx.ps(name="dpDD%d" % i) for i in range(2)]
    pDD_b = [Buf("dpDD%d" % i) for i in range(2)]
    selb = [TB(cx, [128, 64, 128], BF16, "dsel%d" % i) for i in range(2)]
    eL = [TB(cx, [128, 512], BF16, "deL%d" % i) for i in range(2)]
    pT = [TB(cx, [128, 512], BF16, "dpT%d" % i) for i in range(2)]
    rec = [TB(cx, [128, 128], F32, "drec%d" % i) for i in range(2)]
    ost = [TB(cx, [128, NSLOT * 128], F32, "dost%d" % i) for i in range(2)]
    odst = ybT.rearrange("(h p) t -> p h t", p=128)
    n = 0
    nsel = 0
    ng = 0
    nod = 0
    for h in range(B_HEADS):
        for blk in range(NKB):
            bs = slice(blk * 512, (blk + 1) * 512)
            pp, pp_b = pkv[n % 2], pkv_b[n % 2]
            n += 1
            for kc in range(2):
                P.op("pe", lambda e, pp=pp, kc=kc, h=h, bs=bs: e.matmul(pp[:], Wb.t[:, kc, h * 128:(h + 1) * 128], kvn.t[:, kc, bs], start=(kc == 0), stop=(kc == 1)),
                     reads=[Wb.b, kvn.b], writes=[pp_b])
            P.op("act", lambda e, pp=pp, bs=bs: e.activation(out=Kh.t[:, bs], in_=pp[:], func=AF.Copy), reads=[pp_b], writes=[Kh.b])
        for g4 in range(S // 512):
            pp, pp_b = pkv[n % 2], pkv_b[n % 2]
            n += 1
            for j in range(4):
                tl = g4 * 4 + j
                for kc in range(2):
                    P.op("pe", lambda e, pp=pp, kc=kc, h=h, tl=tl, j=j: e.matmul(
                        pp[:, j * 128:(j + 1) * 128], kvn.t[:, kc, tl * 128:(tl + 1) * 128], Wb.t[:, kc, 1024 + h * 128:1024 + (h + 1) * 128],
                        start=(kc == 0), stop=(kc == 1)), reads=[Wb.b, kvn.b], writes=[pp_b])
            P.op("dve", lambda e, pp=pp, g4=g4: e.tensor_copy(Vh.t[:, g4 * 4:(g4 + 1) * 4, :].rearrange("p a b -> p (a b)"), pp[:]), reads=[pp_b], writes=[Vh.b])
        oh = ost[h % 2]
        items = []
        for i in range(NSLOT):
            ntile = slot_tiles(i)
            ctx = {"i": i, "ntile": ntile, "sb": selb[nsel % 2], "od": pOD[nod % 2], "od_b": pOD_b[nod % 2],
                   "dn": pDD[nod % 2], "dn_b": pDD_b[nod % 2], "rc": rec[nod % 2], "qs": slice(i * 128, (i + 1) * 128)}
            nsel += 1
            nod += 1
            for g4 in range(ntile // 4):
                items.append((ctx, g4, ng % 2))
                ng += 1

        def stage_a(item, h=h):
            ctx, g4, bi = item
            i, ntile, sb_, qs = ctx["i"], ctx["ntile"], ctx["sb"], ctx["qs"]
            pl, pl_b, pt = pL[bi], pL_b[bi], pT[bi]
            if g4 == 0:
                o0 = slot_off(i)
                P.dma("sp", lambda e: e.dma_start(out=sb_.t[:, 0:ntile, :], in_=selT[:, o0:o0 + ntile, :]), writes=[sb_.b])
            for j in range(4):
                tl = g4 * 4 + j
                r = tl - 8 * i
                P.op("pe", lambda e, j=j, tl=tl: e.matmul(
                    pl[:, j * 128:(j + 1) * 128], Kh.t[:, tl * 128:(tl + 1) * 128], qb.t[:, h, qs], start=True, stop=False),
                    reads=[Kh.b, qb.b], writes=[pl_b])
                if r >= 0:
                    P.op("pe", lambda e, j=j, r=r: e.matmul(
                        pl[:, j * 128:(j + 1) * 128], idb.t[:], cr.t[:, r, h, :], start=False, stop=False),
                        reads=[idb.b, cr.b], writes=[pl_b])
                P.op("pe", lambda e, j=j: e.matmul(
                    pl[:, j * 128:(j + 1) * 128], ones1.t[:], rrow.t[:, h, qs], start=False, stop=False),
                    reads=[ones1.b, rrow.b], writes=[pl_b])
                P.op("pe", lambda e, j=j, tl=tl: e.matmul(
                    pl[:, j * 128:(j + 1) * 128], idb.t[:], sb_.t[:, tl, :], start=False, stop=True),
                    reads=[idb.b, sb_.b], writes=[pl_b])
            for j in range(4):
                tl = g4 * 4 + j
                ix = tl - 8 * i + 56
                P.op("act", lambda e, j=j, ix=ix: e.activation(
                    out=pt.t[:, j * 128:(j + 1) * 128], in_=pl[:, j * 128:(j + 1) * 128], func=AF.Exp, bias=bc.t[:, h, ix:ix + 1], scale=1.0),
                    reads=[pl_b, bc.b], writes=[pt.b])

        def stage_b(item, oh=oh):
            ctx, g4, bi = item
            ntile, od, od_b, dn, dn_b, rc, qs = ctx["ntile"], ctx["od"], ctx["od_b"], ctx["dn"], ctx["dn_b"], ctx["rc"], ctx["qs"]
            pt = pT[bi]
            for j in range(4):
                tl = g4 * 4 + j
                P.op("pe", lambda e, j=j, tl=tl: e.matmul(
                    od[:, 0:128], Vh.t[:, tl, :], pt.t[:, j * 128:(j + 1) * 128], start=(tl == 0), stop=(tl == ntile - 1)),
                    reads=[Vh.b, pt.b], writes=[od_b])
            for j in range(4):
                tl = g4 * 4 + j
                P.op("pe", lambda e, j=j, tl=tl: e.matmul(
                    dn[:, 0:128], ones.t[:], pt.t[:, j * 128:(j + 1) * 128], start=(tl == 0), stop=(tl == ntile - 1)),
                    reads=[ones.b, pt.b], writes=[dn_b])
            if g4 == ntile // 4 - 1:
                P.op("dve", lambda e: e.reciprocal(rc.t[:], dn[:, 0:128]), reads=[dn_b], writes=[rc.b])
                P.op("dve", lambda e: e.tensor_tensor(out=oh.t[:, qs], in0=od[:, 0:128], in1=rc.t[:], op=ALU.mult),
                     reads=[od_b, rc.b], writes=[oh.b])

        stage_a(items[0])
        for k in range(1, len(items)):
            stage_a(items[k])
            stage_b(items[k - 1])
        stage_b(items[-1])
        P.dma("sp", lambda e, h=h, oh=oh: e.dma_start(out=odst[:, h, :], in_=oh.t[:]), reads=[oh.b])


def build_C2():
    import contextlib
    nc = bass.Bass("TRN2", target_bir_lowering=False)
    with contextlib.ExitStack() as st:
        cx = Ctx(nc, st)
        kvT = cx.din("kvT", [KV_RANK, SEQ])
        kvg = cx.din("kvg", [128, 2])
        wkv = cx.din("wkv", [KV_RANK, 2048])
        qT = cx.din("qT", [1024, NSLOT * 128])
        selT = cx.din("selT", [128, NT_TOTAL, 128], BF16)
        biascol = cx.din("biascol", [128, B_HEADS, 64])
        corr = cx.din("corr", [128, 8, B_HEADS, 128])
        ident = cx.din("ident", [128, 128])
        ybT = cx.dout("ybT", [1024, NSLOT * 128])
        ndminT = cx.din("ndminT", [1, NSLOT * 128])
        qoff = cx.din("qoff", [1, 128])
        emit_dsa_attn(cx, kvT, kvg, wkv, qT, selT, biascol, corr, ident, ybT, ndminT, qoff)
        cx.finish()
    return nc


def build_D0A1():
    import contextlib
    nc = bass.Bass("TRN2", target_bir_lowering=False)
    with contextlib.ExitStack() as st:
        cx = Ctx(nc, st)
        P = cx.P
        hT = cx.din("hT", [D, TOKC])
        mod0 = cx.din("mod0", [128, 144])
        gT0 = cx.din("gT0", [128, 3, KC])
        yaT = cx.din("yaT", [1024, TOKC])
        ybT = cx.din("ybT", [1024, TOKC])
        w_glu = cx.din("w_glu", [1024, 1024])
        bgT = cx.din("bgT", [128, 8])
        w_out = cx.din("w_out", [D, D])
        wg0 = cx.din("wg0", [D, DFF])
        wu0 = cx.din("wu0", [D, DFF])
        wd0 = cx.din("wd0", [DFF, D])
        mod1 = cx.din("mod1", [128, 144])
        gT1 = cx.din("gT1", [128, 3, KC])
        wg1 = cx.din("wg1", [D, DFF])
        wu1 = cx.din("wu1", [D, DFF])
        wd1 = cx.din("wd1", [DFF, D])
        w_qkv = cx.din("w_qkv", [D, 3 * D])
        hT_out = cx.dout("hT_out", [D, TOKC])
        qkvT = cx.dout("qkvT", [3 * D, TOKC])
        co = Core(cx)
        co.load_h(hT)
        co.load_mod(mod0, gT0)
        co.mixer0_out(yaT, ybT, w_glu, bgT, w_out)
        co.ffn(2, wg0, wu0, wd0)
        co.load_mod(mod1, gT1)
        co.ffn(0, wg1, wu1, wd1)
        co.store_h(hT_out)
        for t0 in range(0, TOKC, TT):
            co.adaln_full(1, t0)
            co.proj(w_qkv, 3 * D, t0, co.out_epilogue(qkvT, t0))
        cx.finish()
    return nc


def build_D1():
    import contextlib
    nc = bass.Bass("TRN2", target_bir_lowering=False)
    with contextlib.ExitStack() as st:
        cx = Ctx(nc, st)
        hT = cx.din("hT", [D, TOKC])
        mod1 = cx.din("mod1", [128, 144])
        gT1 = cx.din("gT1", [128, 3, KC])
        oT = cx.din("oT", [D, TOKC])
        w_out = cx.din("w_out", [D, D])
        wg = cx.din("wg", [D, DFF])
        wu = cx.din("wu", [D, DFF])
        wd = cx.din("wd", [DFF, D])
        gfT = cx.din("gfT", [128, KC])
        outT = cx.dout("outT", [D, TOKC])
        co = Core(cx)
        co.load_h(hT)
        co.load_mod(mod1, gT1)
        co.mixer1_out(oT, w_out)
        co.ffn(2, wg, wu, wd)
        co.final_norm(gfT, outT)
        cx.finish()
    return nc


MODW = 9 * D // NCORES
MODC = MODW // 128


def build_M():
    import contextlib
    nc = bass.Bass("TRN2", target_bir_lowering=False)
    with contextlib.ExitStack() as st:
        cx = Ctx(nc, st)
        P = cx.P
        condT = cx.din("condT", [128, KC])
        aw = cx.din("aw", [2, D, MODW])
        abT = cx.din("abT", [128, 2, MODC])
        modc = cx.dout("modc", [128, 2, MODC])
        ws = Stream(cx)
        c32 = TB(cx, [128, KC], F32, "mc32")
        cb = TB(cx, [128, KC], BF16, "mcb")
        ab = TB(cx, [128, 2, MODC], F32, "mab")
        mo = TB(cx, [128, 2, MODC], F32, "mmo")
        pm = cx.ps(name="mpm")
        pm_b = Buf("mpm")
        P.dma("sp", lambda e: e.dma_start(out=c32.t[:], in_=condT), writes=[c32.b])
        P.dma("sp", lambda e: e.dma_start(out=ab.t[:], in_=abT), writes=[ab.b])
        P.op("act", lambda e: e.activation(out=cb.t[:], in_=c32.t[:], func=AF.Silu), reads=[c32.b], writes=[cb.b])
        for l in range(2):
            src = aw[l].rearrange("(k p) n -> p k n", p=128)
            for nb in range((MODW + 511) // 512):
                ncol = min(512, MODW - nb * 512)
                v, wb = ws.load(src[:, :, nb * 512:nb * 512 + ncol], KC, ncol)
                for j in range(ncol // 128):
                    col = l * MODC + nb * 4 + j
                    for k in range(KC):
                        P.op("pe", lambda e, v=v, j=j, k=k, col=col: e.matmul(
                            pm[:, col:col + 1], v[:, k, j * 128:(j + 1) * 128], cb.t[:, k:k + 1], start=(k == 0), stop=(k == KC - 1)),
                            reads=[wb, cb.b], writes=[pm_b])
        P.op("dve", lambda e: e.tensor_tensor(out=mo.t[:].rearrange("p a b -> p (a b)"), in0=pm[:, 0:2 * MODC],
                                              in1=ab.t[:].rearrange("p a b -> p (a b)"), op=ALU.add), reads=[pm_b, ab.b], writes=[mo.b])
        P.dma("sp", lambda e: e.dma_start(out=modc, in_=mo.t[:]), reads=[mo.b])
        cx.finish()
    return nc


def _fm(v):
    return np.ascontiguousarray(np.asarray(v).reshape(-1, 128).T)


def _gT(norm_g_layer):
    return np.ascontiguousarray(np.asarray(norm_g_layer).reshape(3, KC, 128).transpose(2, 0, 1))


_SAVE = None


def _run(nc, ims):
    return run_bass_kernel_spmd(nc, ims, core_ids=list(range(NCORES))).results


def kernel(**inp):
    inp = {k: np.asarray(v) for k, v in inp.items()}
    f32 = np.float32
    x = inp['x'][0]
    cores = range(NCORES)
    tokc = [slice(c * TOKC, (c + 1) * TOKC) for c in cores]
    condT = _fm(inp['c'][0])
    sv = _SAVE if _SAVE is not None else {}
    if 'M' not in sv:
        ims = [{"condT": condT, "aw": np.ascontiguousarray(inp['ada_w'][:, :, c * MODW:(c + 1) * MODW]),
                "abT": np.ascontiguousarray(inp['ada_b'][:, c * MODW:(c + 1) * MODW].reshape(2, MODC, 128).transpose(2, 0, 1))} for c in cores]
        r = _run(build_M(), ims)
        sv['M'] = [np.ascontiguousarray(np.concatenate([r[c]["modc"][:, l, :] for c in cores], axis=1)) for l in range(2)]
    mod0, mod1 = sv['M']
    if 'A0' not in sv:
        ims = [{"xT": np.ascontiguousarray(x[tokc[c]].T), "mod0": mod0,
                "gT": _gT(inp['norm_g'][0]), "wg": inp['ffn_w_gate'][0, 0], "wu": inp['ffn_w_up'][0, 0], "wd": inp['ffn_w_down'][0, 0],
                "w_in": inp['ab_w_in'][0]} for c in cores]
        r = _run(build_A0(), ims)
        sv['A0'] = {"hT": [r[c]["hT_out"] for c in cores],
                    "pT": np.concatenate([r[c]["pT_out"] for c in cores], axis=1)}
    hT0, pT = sv['A0']["hT"], sv['A0']["pT"]
    if 'S5' not in sv:
        ims = []
        for c in cores:
            im = s5_host_layout(inp, c)
            im["uT"] = np.ascontiguousarray(pT[c * 128:(c + 1) * 128, :])
            ims.append(im)
        r = _run(build_S5(), ims)
        sv['S5'] = np.concatenate([r[c]["yT"] for c in cores], axis=0)
    yaT = sv['S5']
    toks = [np.concatenate([np.arange(128) + 128 * (8 * i + c) for i in range(NSLOT)]) for c in cores]
    ident = np.eye(128, dtype=f32)
    if 'C1' not in sv:
        pow2 = np.tile((0.5 ** np.arange(NBIS + 1)).astype(f32), (128, 1))
        ims = [{"kidxT": np.ascontiguousarray(pT[3328:3392, :]), "qidxT": np.ascontiguousarray(pT[2304:3328, toks[c]]),
                "widx": np.ascontiguousarray(pT[3392:3408, toks[c]].reshape(16, NSLOT, 128).transpose(2, 1, 0)),
                "adm": dsa_adm_mask(c), "pow2": pow2, "ident": ident, "negpos": dsa_negpos(c)} for c in cores]
        r = _run(build_C1(), ims)
        sv['C1'] = [(r[c]["selT"], r[c]["ndmin"]) for c in cores]
    if 'C2' not in sv:
        ims = []
        for c in cores:
            bcol, corr = dsa_alibi_tables(c)
            ims.append({"kvT": np.ascontiguousarray(pT[2048:2304, :]), "kvg": _fm(inp['dsa_kv_norm_g'][0]), "wkv": inp['dsa_w_kv_up'][0],
                        "qT": np.ascontiguousarray(pT[1024:2048, toks[c]]), "selT": sv['C1'][c][0], "biascol": bcol, "corr": corr, "ident": ident,
                        "ndminT": np.ascontiguousarray(sv['C1'][c][1].T.reshape(1, -1)),
                        "qoff": (64 - np.arange(128, dtype=f32)).reshape(1, 128)})
        r = _run(build_C2(), ims)
        ybT = np.zeros((1024, SEQ), f32)
        for c in cores:
            ybT[:, toks[c]] = r[c]["ybT"]
        sv['C2'] = ybT
    ybT = sv['C2']
    if 'D0A1' not in sv:
        ims = [{"hT": hT0[c], "mod0": mod0, "gT0": _gT(inp['norm_g'][0]), "yaT": np.ascontiguousarray(yaT[:, tokc[c]]),
                "ybT": np.ascontiguousarray(ybT[:, tokc[c]]), "w_glu": inp['s5_w_glu'][0], "bgT": _fm(inp['s5_b_glu'][0]),
                "w_out": inp['ab_w_out'][0], "wg0": inp['ffn_w_gate'][0, 1], "wu0": inp['ffn_w_up'][0, 1], "wd0": inp['ffn_w_down'][0, 1],
                "mod1": mod1, "gT1": _gT(inp['norm_g'][1]),
                "wg1": inp['ffn_w_gate'][1, 0], "wu1": inp['ffn_w_up'][1, 0], "wd1": inp['ffn_w_down'][1, 0], "w_qkv": inp['c_w_qkv'][0]}
               for c in cores]
        r = _run(build_D0A1(), ims)
        sv['D0A1'] = {"hT": [r[c]["hT_out"] for c in cores],
                      "qkvT": np.concatenate([r[c]["qkvT"] for c in cores], axis=1)}
    hT1, qkvT = sv['D0A1']["hT"], sv['D0A1']["qkvT"]
    if 'ATT' not in sv:
        biasT = att_bias_table(inp['c_rel_bias'][0])
        kpad = np.concatenate([np.zeros((D, HALO), f32), qkvT[D:2 * D]], axis=1)
        vpad = np.concatenate([np.zeros((D, HALO), f32), qkvT[2 * D:3 * D]], axis=1)
        ims = [{"qT": np.ascontiguousarray(qkvT[0:D, tokc[c]]), "kT": np.ascontiguousarray(kpad[:, c * TOKC:(c + 1) * TOKC + HALO]),
                "v": np.ascontiguousarray(vpad[:, c * TOKC:(c + 1) * TOKC + HALO].T), "biasT": biasT,
                "vones": np.full((128, 128), 0.0 if c == 0 else 1.0, f32)} for c in cores]
        r = _run(build_ATT(), ims)
        sv['ATT'] = [r[c]["oT"] for c in cores]
    oT = sv['ATT']
    ims = [{"hT": hT1[c], "mod1": mod1, "gT1": _gT(inp['norm_g'][1]), "oT": oT[c], "w_out": inp['c_w_out'][0],
            "wg": inp['ffn_w_gate'][1, 1], "wu": inp['ffn_w_up'][1, 1], "wd": inp['ffn_w_down'][1, 1], "gfT": _fm(inp['final_norm_g'])}
           for c in cores]
    r = _run(build_D1(), ims)
    out = np.zeros((1, SEQ, D), f32)
    for c in cores:
        out[0, tokc[c], :] = r[c]["outT"].T
    return out
```

```python
import numpy as np
import concourse.bass as bass
import concourse.mybir as mybir
from concourse.bass_utils import run_bass_kernel_spmd

F32 = mybir.dt.float32
BF16 = mybir.dt.bfloat16
AF = mybir.ActivationFunctionType
ALU = mybir.AluOpType
AX = mybir.AxisListType

NCORES = 8


class Buf:
    __slots__ = ("name", "lw", "rd")

    def __init__(self, name):
        self.name = name
        self.lw = None
        self.rd = {}


class Prog:
    ENG = ("pe", "act", "dve", "pool", "sp")

    def __init__(self, nc, n_dma_sems=12):
        self.nc = nc
        self.ops = {e: [] for e in self.ENG}
        self.cnt = {e: 0 for e in self.ENG}
        self.seen = {e: {} for e in self.ENG}
        self.n_dma_sems = n_dma_sems
        self.dma_cnt = [0] * n_dma_sems
        self.dma_rr = 0
        self.sems = {}
        self._stack = None

    def _deps(self, eng, reads, writes):
        need = {}

        def add(kc):
            if kc is None:
                return
            k, c = kc
            if need.get(k, 0) < c:
                need[k] = c

        for b in reads:
            add(b.lw)
        for b in writes:
            add(b.lw)
            for k, c in b.rd.items():
                add((k, c))
        out = []
        for k, c in need.items():
            if k == "pe" and eng == "pe":
                continue
            if self.seen[eng].get(k, 0) >= c:
                continue
            self.seen[eng][k] = c
            out.append((k, c))
        return out

    def op(self, eng, fn, reads=(), writes=()):
        waits = self._deps(eng, reads, writes)
        self.cnt[eng] += 1
        me = (eng, self.cnt[eng])
        for b in reads:
            b.rd[eng] = me[1]
        for b in writes:
            b.lw = me
            b.rd = {}
        self.ops[eng].append((waits, fn, (eng, 1)))

    def dma(self, eng, fn, reads=(), writes=()):
        s = self.dma_rr
        self.dma_rr = (self.dma_rr + 1) % self.n_dma_sems
        key = "dma%d" % s
        waits = self._deps(eng, reads, writes)
        prev = self.dma_cnt[s]
        if prev and self.seen[eng].get(key, 0) < prev:
            self.seen[eng][key] = prev
            waits.append((key, prev))
        self.dma_cnt[s] += 1
        me = (key, self.dma_cnt[s])
        for b in reads:
            b.rd[key] = me[1]
        for b in writes:
            b.lw = me
            b.rd = {}
        self.ops[eng].append((waits, fn, (key, 16)))

    def final_wait(self, eng, bufs):
        waits = self._deps(eng, bufs, ())
        self.ops[eng].append((waits, None, None))

    def emit(self):
        nc = self.nc
        import contextlib
        with contextlib.ExitStack() as st:
            keys = list(self.ENG) + ["dma%d" % i for i in range(self.n_dma_sems)]
            sem = {k: st.enter_context(nc.semaphore("s_" + k)) for k in keys}
            block = st.enter_context(nc.Block())
            mult = {k: (16 if k.startswith("dma") else 1) for k in keys}

            def run(engname):
                def body(e):
                    for waits, fn, inc in self.ops[engname]:
                        for k, c in waits:
                            e.wait_ge(sem[k], c * mult[k])
                        if fn is not None:
                            ins = fn(e)
                            ins.then_inc(sem[inc[0]], inc[1])
                return body

            block.tensor(run("pe"))
            block.scalar(run("act"))
            block.vector(run("dve"))
            block.gpsimd(run("pool"))
            block.sync(run("sp"))


D = 2048
KC = D // 128
DFF = 5504
FC = DFF // 128
SEQ = 8192
TOKC = SEQ // NCORES
TT = 512
EPS = 1e-6
D_IN_AB = 3408


class Ctx:
    def __init__(self, nc, st):
        self.nc = nc
        self.st = st
        self.P = Prog(nc)
        self._n = 0
        self.outs = []

    def sb(self, shape, dt, name=None):
        self._n += 1
        return self.st.enter_context(self.nc.sbuf_tensor("sb_" + (name or str(self._n)), list(shape), dt))

    def ps(self, shape=(128, 512), dt=F32, name=None):
        self._n += 1
        return self.st.enter_context(self.nc.psum_tensor("ps_" + (name or str(self._n)), list(shape), dt))

    def din(self, name, shape, dt=F32):
        return self.nc.dram_tensor(name, list(shape), dt, kind="ExternalInput").ap()

    def dout(self, name, shape, dt=F32):
        return self.nc.dram_tensor(name, list(shape), dt, kind="ExternalOutput").ap()

    def finish(self):
        P = self.P
        waits = [("dma%d" % i, P.dma_cnt[i]) for i in range(P.n_dma_sems) if P.dma_cnt[i]]
        waits += [(e, P.cnt[e]) for e in ("pe", "act", "dve", "pool") if P.cnt[e]]
        P.ops["sp"].append((waits, None, None))
        P.emit()


class Stream:
    def __init__(self, cx, nslots=3, elems=8192):
        self.cx = cx
        self.n = nslots
        self.elems = elems
        self.t = cx.sb([128, nslots, elems], BF16, "wbuf")
        self.bufs = [Buf("w%d" % i) for i in range(nslots)]
        self.i = 0

    def load(self, src_ap, k, n, rows=128):
        s = self.i
        self.i = (self.i + 1) % self.n
        view = self.t[0:rows, s, 0:k * n].rearrange("p (k n) -> p k n", k=k)
        b = self.bufs[s]
        self.cx.P.dma("pool", lambda e, v=view, a=src_ap: e.dma_start(out=v, in_=a), writes=[b])
        return view, b


class Core:
    def __init__(self, cx, ntok=TOKC):
        self.cx = cx
        P = cx.P
        self.ntok = ntok
        self.hT = cx.sb([128, KC, ntok], F32, "hT")
        self.hT_b = [Buf("hT%d" % k) for k in range(KC)]
        self.hn = cx.sb([128, KC, TT], BF16, "hn")
        self.hn_b = Buf("hn")
        self.A = cx.sb([128, FC, TT], BF16, "A")
        self.A_b = [Buf("A%d" % f) for f in range(FC)]
        self.ws = Stream(cx)
        self.ones = cx.sb([128, 128], BF16, "ones")
        self.ones_b = Buf("ones")
        P.op("pool", lambda e: e.memset(self.ones[:], 1.0), writes=[self.ones_b])
        self.sq = [cx.sb([128, TT], BF16, "sq%d" % i) for i in range(2)]
        self.sq_b = [Buf("sq%d" % i) for i in range(2)]
        self.tmp = [cx.sb([128, TT], F32, "tmp%d" % i) for i in range(3)]
        self.tmp_b = [Buf("tmp%d" % i) for i in range(3)]
        self.tmp_i = 0
        self.rstd = cx.sb([128, TT], F32, "rstd")
        self.rstd_b = Buf("rstd")
        self.rtmp = cx.sb([128, TT], F32, "rtmp")
        self.rtmp_b = Buf("rtmp")
        self.pgu = [cx.ps(name="pgu%d" % i) for i in range(4)]
        self.pgu_b = [Buf("pgu%d" % i) for i in range(4)]
        self.pacc = [cx.ps(name="pacc%d" % i) for i in range(2)]
        self.pacc_b = [Buf("pacc%d" % i) for i in range(2)]
        self.pacc_i = 0
        self.pst = cx.ps(name="pst")
        self.pst_b = Buf("pst")
        self.modT = cx.sb([128, 144], F32, "modT")
        self.mod_b = Buf("mod")
        self.vec = cx.sb([128, 3, 3, KC], F32, "vec")
        self.vec_b = Buf("vec")

    def next_tmp(self):
        i = self.tmp_i
        self.tmp_i = (i + 1) % len(self.tmp)
        return self.tmp[i], self.tmp_b[i]

    def next_acc(self):
        i = self.pacc_i
        self.pacc_i = (i + 1) % len(self.pacc)
        return self.pacc[i], self.pacc_b[i]

    def load_h(self, hT_dram):
        P = self.cx.P
        src = hT_dram.rearrange("(k p) t -> p k t", p=128)
        for k in range(KC):
            P.dma("sp", lambda e, k=k: e.dma_start(out=self.hT[:, k, :], in_=src[:, k, :]), writes=[self.hT_b[k]])

    def store_h(self, out_dram):
        P = self.cx.P
        dst = out_dram.rearrange("(k p) t -> p k t", p=128)
        for k in range(KC):
            P.dma("sp", lambda e, k=k: e.dma_start(out=dst[:, k, :], in_=self.hT[:, k, :]), reads=[self.hT_b[k]])

    def compute_mod(self, condT_dram, ada_w, ada_bT_dram, gT_dram):
        cx, P = self.cx, self.cx.P
        self._modn = getattr(self, "_modn", 0) + 1
        sfx = str(self._modn)
        c32 = cx.sb([128, KC], F32, "c32" + sfx)
        cb = cx.sb([128, KC], BF16, "cb" + sfx)
        abT = cx.sb([128, 144], F32, "abT" + sfx)
        gT = cx.sb([128, 3, KC], F32, "gT" + sfx)
        b_c32, b_cb, b_ab, b_g = Buf("c32"), Buf("cb"), Buf("abT"), Buf("gT")
        P.dma("sp", lambda e: e.dma_start(out=c32[:], in_=condT_dram), writes=[b_c32])
        P.dma("sp", lambda e: e.dma_start(out=abT[:], in_=ada_bT_dram), writes=[b_ab])
        P.dma("sp", lambda e: e.dma_start(out=gT[:], in_=gT_dram), writes=[b_g])
        P.op("act", lambda e: e.activation(out=cb[:], in_=c32[:], func=AF.Silu), reads=[b_c32], writes=[b_cb])
        wsrc = ada_w.rearrange("(k p) n -> p k n", p=128)
        pm = self.pacc[0]
        pm_b = self.pacc_b[0]
        for nb in range(36):
            view, wb = self.ws.load(wsrc[:, :, nb * 512:(nb + 1) * 512], KC, 512)
            for j in range(4):
                col = nb * 4 + j
                for k in range(KC):
                    P.op("pe", lambda e, v=view, j=j, k=k, col=col: e.matmul(
                        pm[:, col:col + 1], v[:, k, j * 128:(j + 1) * 128], cb[:, k:k + 1],
                        start=(k == 0), stop=(k == KC - 1)), reads=[wb, b_cb], writes=[pm_b])
        P.op("dve", lambda e: e.tensor_tensor(out=self.modT[:], in0=pm[:, 0:144], in1=abT[:], op=ALU.add),
             reads=[pm_b, b_ab], writes=[self.mod_b])
        self.derive_vec(gT, b_g)

    def load_mod(self, mod_dram, gT_dram):
        cx, P = self.cx, self.cx.P
        self._modn = getattr(self, "_modn", 0) + 1
        gT = cx.sb([128, 3, KC], F32, "gT" + str(self._modn))
        b_g = Buf("gT")
        P.dma("sp", lambda e: e.dma_start(out=gT[:], in_=gT_dram), writes=[b_g])
        P.dma("sp", lambda e: e.dma_start(out=self.modT[:], in_=mod_dram), writes=[self.mod_b])
        self.derive_vec(gT, b_g)

    def derive_vec(self, gT, b_g):
        P = self.cx.P
        for s in range(3):
            sh = self.modT[:, (s * 3 + 0) * KC:(s * 3 + 1) * KC]
            sc = self.modT[:, (s * 3 + 1) * KC:(s * 3 + 2) * KC]
            ga = self.modT[:, (s * 3 + 2) * KC:(s * 3 + 3) * KC]
            P.op("dve", lambda e, s=s, sc=sc: e.scalar_tensor_tensor(
                out=self.vec[:, s, 0, :], in0=sc, scalar=1.0, in1=gT[:, s, :], op0=ALU.add, op1=ALU.mult),
                reads=[self.mod_b, b_g], writes=[self.vec_b])
            P.op("dve", lambda e, s=s, sh=sh: e.tensor_copy(self.vec[:, s, 1, :], sh),
                 reads=[self.mod_b], writes=[self.vec_b])
            cgate = 1.0 if s == 1 else 0.5
            P.op("dve", lambda e, s=s, ga=ga, cgate=cgate: e.tensor_scalar(
                out=self.vec[:, s, 2, :], in0=ga, scalar1=cgate, scalar2=None, op0=ALU.mult),
                reads=[self.mod_b], writes=[self.vec_b])

    def adaln(self, sub, t0, plain_g=None):
        cx, P = self.cx, self.cx.P
        for k in range(KC):
            i = k % 2
            P.op("act", lambda e, k=k, i=i: e.activation(out=self.sq[i][:], in_=self.hT[:, k, t0:t0 + TT], func=AF.Square),
                 reads=[self.hT_b[k]], writes=[self.sq_b[i]])
            P.op("pe", lambda e, k=k, i=i: e.matmul(self.pst[:], self.ones[:], self.sq[i][:], start=(k == 0), stop=(k == KC - 1)),
                 reads=[self.sq_b[i], self.ones_b], writes=[self.pst_b])
        P.op("dve", lambda e: e.tensor_scalar(out=self.rtmp[:], in0=self.pst[:], scalar1=1.0 / D, scalar2=EPS,
                                              op0=ALU.mult, op1=ALU.add), reads=[self.pst_b], writes=[self.rtmp_b])
        P.op("act", lambda e: e.activation(out=self.rtmp[:], in_=self.rtmp[:], func=AF.Sqrt),
             reads=[self.rtmp_b], writes=[self.rtmp_b])
        P.op("dve", lambda e: e.reciprocal(self.rstd[:], self.rtmp[:]), reads=[self.rtmp_b], writes=[self.rstd_b])

    def adaln_apply(self, sub, t0, k, out_ap, out_bufs, gs_ap=None, shift_ap=None):
        P = self.cx.P
        tmp, tb = self.next_tmp()
        gs = gs_ap if gs_ap is not None else self.vec[:, sub, 0, k:k + 1]
        P.op("dve", lambda e: e.scalar_tensor_tensor(out=tmp[:], in0=self.hT[:, k, t0:t0 + TT], scalar=gs,
                                                     in1=self.rstd[:], op0=ALU.mult, op1=ALU.mult),
             reads=[self.hT_b[k], self.rstd_b, self.vec_b], writes=[tb])
        if shift_ap is None and gs_ap is None:
            shift_ap = self.vec[:, sub, 1, k:k + 1]
        if shift_ap is not None:
            P.op("act", lambda e: e.activation(out=out_ap, in_=tmp[:], func=AF.Identity, bias=shift_ap, scale=1.0),
                 reads=[tb, self.vec_b], writes=out_bufs)
        else:
            P.op("act", lambda e: e.activation(out=out_ap, in_=tmp[:], func=AF.Copy), reads=[tb], writes=out_bufs)

    def adaln_full(self, sub, t0):
        self.adaln(sub, t0)
        for k in range(KC):
            self.adaln_apply(sub, t0, k, self.hn[:, k, :], [self.hn_b])

    def ffn(self, sub, w_gate, w_up, w_down):
        cx, P = self.cx, self.cx.P
        wg_src = w_gate.rearrange("(k p) n -> p k n", p=128)
        wu_src = w_up.rearrange("(k p) n -> p k n", p=128)
        wd_src = w_down.rearrange("(f p) n -> p f n", p=128)
        for t0 in range(0, self.ntok, TT):
            self.adaln_full(sub, t0)
            gi = 0
            for nb in range((FC + 1) // 2):
                ncol = min(256, DFF - nb * 256)
                nj = ncol // 128
                vg, bg = self.ws.load(wg_src[:, :, nb * 256:nb * 256 + ncol], KC, ncol)
                vu, bu = self.ws.load(wu_src[:, :, nb * 256:nb * 256 + ncol], KC, ncol)
                for j in range(nj):
                    f = nb * 2 + j
                    pg, pg_b = self.pgu[gi], self.pgu_b[gi]
                    pu, pu_b = self.pgu[gi + 1], self.pgu_b[gi + 1]
                    gi = (gi + 2) % 4
                    for k in range(KC):
                        P.op("pe", lambda e, k=k, j=j, vg=vg, pg=pg: e.matmul(
                            pg[:], vg[:, k, j * 128:(j + 1) * 128], self.hn[:, k, :], start=(k == 0), stop=(k == KC - 1)),
                            reads=[bg, self.hn_b], writes=[pg_b])
                    for k in range(KC):
                        P.op("pe", lambda e, k=k, j=j, vu=vu, pu=pu: e.matmul(
                            pu[:], vu[:, k, j * 128:(j + 1) * 128], self.hn[:, k, :], start=(k == 0), stop=(k == KC - 1)),
                            reads=[bu, self.hn_b], writes=[pu_b])
                    tmp, tb = self.next_tmp()
                    P.op("act", lambda e, tmp=tmp, pg=pg: e.activation(out=tmp[:], in_=pg[:], func=AF.Silu),
                         reads=[pg_b], writes=[tb])
                    P.op("dve", lambda e, tmp=tmp, pu=pu, f=f: e.tensor_tensor(out=self.A[:, f, :], in0=tmp[:], in1=pu[:], op=ALU.mult),
                         reads=[tb, pu_b], writes=[self.A_b[f]])
            for dc in range(KC):
                vd, bd = self.ws.load(wd_src[:, :, dc * 128:(dc + 1) * 128], FC, 128)
                py, py_b = self.next_acc()
                for f in range(FC):
                    P.op("pe", lambda e, f=f, vd=vd, py=py: e.matmul(
                        py[:], vd[:, f, :], self.A[:, f, :], start=(f == 0), stop=(f == FC - 1)),
                        reads=[bd, self.A_b[f]], writes=[py_b])
                P.op("dve", lambda e, dc=dc, py=py, t0=t0: e.scalar_tensor_tensor(
                    out=self.hT[:, dc, t0:t0 + TT], in0=py[:], scalar=self.vec[:, sub, 2, dc:dc + 1],
                    in1=self.hT[:, dc, t0:t0 + TT], op0=ALU.mult, op1=ALU.add),
                    reads=[py_b, self.vec_b, self.hT_b[dc]], writes=[self.hT_b[dc]])

    def proj(self, w, ncols, t0, epilogue, xsrc=None, xbufs=None, nk=KC):
        cx, P = self.cx, self.cx.P
        xsrc = self.hn if xsrc is None else xsrc
        xbufs = [self.hn_b] * nk if xbufs is None else xbufs
        src = w.rearrange("(k p) n -> p k n", p=128)
        for nb in range((ncols + 511) // 512):
            nc_ = min(512, ncols - nb * 512)
            v, b = self.ws.load(src[:, :, nb * 512:nb * 512 + nc_], nk, nc_)
            for j in range((nc_ + 127) // 128):
                rows = min(128, nc_ - j * 128)
                pa, pa_b = self.next_acc()
                for k in range(nk):
                    P.op("pe", lambda e, k=k, j=j, v=v, pa=pa, rows=rows: e.matmul(
                        pa[0:rows, :], v[:, k, j * 128:j * 128 + rows], xsrc[:, k, :], start=(k == 0), stop=(k == nk - 1)),
                        reads=[b, xbufs[k]], writes=[pa_b])
                epilogue(nb * 4 + j, rows, pa, pa_b)


    def resid_epilogue(self, sub, t0):
        P = self.cx.P

        def epi(c, rows, pa, pa_b):
            P.op("dve", lambda e: e.scalar_tensor_tensor(
                out=self.hT[:, c, t0:t0 + TT], in0=pa[:], scalar=self.vec[:, sub, 2, c:c + 1],
                in1=self.hT[:, c, t0:t0 + TT], op0=ALU.mult, op1=ALU.add),
                reads=[pa_b, self.vec_b, self.hT_b[c]], writes=[self.hT_b[c]])
        return epi

    def out_epilogue(self, dst, t0):
        cx, P = self.cx, self.cx.P
        if not hasattr(self, "_stg"):
            self._stg = [cx.sb([128, TT], F32, "ostg%d" % i) for i in range(2)]
            self._stg_b = [Buf("ostg%d" % i) for i in range(2)]
            self._stg_i = 0

        def epi(c, rows, pa, pa_b):
            i = self._stg_i
            self._stg_i = (i + 1) % 2
            stg, sb_ = self._stg[i], self._stg_b[i]
            P.op("act", lambda e: e.activation(out=stg[0:rows, :], in_=pa[0:rows, :], func=AF.Copy), reads=[pa_b], writes=[sb_])
            P.dma("sp", lambda e: e.dma_start(out=dst[c * 128:c * 128 + rows, t0:t0 + TT], in_=stg[0:rows, :]), reads=[sb_])
        return epi

    def mixer0_out(self, yaT, ybT, w_glu, bgT, w_out):
        cx, P = self.cx, self.cx.P
        bg = cx.sb([128, 8], F32, "bglu")
        bg_b = Buf("bglu")
        P.dma("sp", lambda e: e.dma_start(out=bg[:], in_=bgT), writes=[bg_b])
        ya_src = yaT.rearrange("(k p) t -> p k t", p=128)
        yb_src = ybT.rearrange("(k p) t -> p k t", p=128)
        for t0 in range(0, self.ntok, TT):
            P.dma("pool", lambda e, t0=t0: e.dma_start(out=self.hn[:, 0:8, :], in_=ya_src[:, :, t0:t0 + TT]), writes=[self.hn_b])
            for k in range(8):
                P.dma("pool", lambda e, t0=t0, k=k: e.dma_start(out=self.A[:, 8 + k, :], in_=yb_src[:, k, t0:t0 + TT]), writes=[self.A_b[8 + k]])

            def epi(c, rows, pa, pa_b):
                tmp, tb = self.next_tmp()
                P.op("act", lambda e: e.activation(out=tmp[:], in_=pa[:], func=AF.Sigmoid, bias=bg[:, c:c + 1], scale=1.0),
                     reads=[pa_b, bg_b], writes=[tb])
                P.op("dve", lambda e: e.tensor_tensor(out=self.A[:, c, :], in0=tmp[:], in1=self.hn[:, c, :], op=ALU.mult),
                     reads=[tb, self.hn_b], writes=[self.A_b[c]])
            self.proj(w_glu, 1024, t0, epi, nk=8)
            self.proj(w_out, D, t0, self.resid_epilogue(1, t0), xsrc=self.A, xbufs=self.A_b[0:KC], nk=KC)

    def mixer1_out(self, oT, w_out):
        P = self.cx.P
        o_src = oT.rearrange("(k p) t -> p k t", p=128)
        for t0 in range(0, self.ntok, TT):
            P.dma("pool", lambda e, t0=t0: e.dma_start(out=self.hn[:], in_=o_src[:, :, t0:t0 + TT]), writes=[self.hn_b])
            self.proj(w_out, D, t0, self.resid_epilogue(1, t0))

    def final_norm(self, gfT, outT):
        cx, P = self.cx, self.cx.P
        gf = cx.sb([128, KC], F32, "gfin")
        gf_b = Buf("gfin")
        P.dma("sp", lambda e: e.dma_start(out=gf[:], in_=gfT), writes=[gf_b])
        dst = outT.rearrange("(k p) t -> p k t", p=128)
        for t0 in range(0, self.ntok, TT):
            self.adaln(0, t0)
            for k in range(KC):
                tmp, tb = self.next_tmp()
                P.op("dve", lambda e, k=k, tmp=tmp, t0=t0: e.scalar_tensor_tensor(
                    out=tmp[:], in0=self.hT[:, k, t0:t0 + TT], scalar=gf[:, k:k + 1], in1=self.rstd[:], op0=ALU.mult, op1=ALU.mult),
                    reads=[self.hT_b[k], self.rstd_b, gf_b], writes=[tb])
                P.dma("sp", lambda e, k=k, tmp=tmp, t0=t0: e.dma_start(out=dst[:, k, t0:t0 + TT], in_=tmp[:]), reads=[tb])


def build_A0():
    import contextlib
    nc = bass.Bass("TRN2", target_bir_lowering=False)
    with contextlib.ExitStack() as st:
        cx = Ctx(nc, st)
        P = cx.P
        xT = cx.din("xT", [D, TOKC])
        mod0 = cx.din("mod0", [128, 144])
        gT = cx.din("gT", [128, 3, KC])
        wg = cx.din("wg", [D, DFF])
        wu = cx.din("wu", [D, DFF])
        wd = cx.din("wd", [DFF, D])
        w_in = cx.din("w_in", [D, D_IN_AB])
        hT_out = cx.dout("hT_out", [D, TOKC])
        pT_out = cx.dout("pT_out", [D_IN_AB, TOKC])
        co = Core(cx)
        co.load_h(xT)
        co.load_mod(mod0, gT)
        co.ffn(0, wg, wu, wd)
        co.store_h(hT_out)
        stg = [cx.sb([128, TT], F32, "stg%d" % i) for i in range(2)]
        stg_b = [Buf("stg%d" % i) for i in range(2)]
        cnt = [0]
        for t0 in range(0, TOKC, TT):
            co.adaln_full(1, t0)

            def epi(c, rows, pa, pa_b, t0=t0):
                i = cnt[0] % 2
                cnt[0] += 1
                P.op("act", lambda e: e.activation(out=stg[i][0:rows, :], in_=pa[0:rows, :], func=AF.Copy),
                     reads=[pa_b], writes=[stg_b[i]])
                P.dma("sp", lambda e: e.dma_start(out=pT_out[c * 128:c * 128 + rows, t0:t0 + TT], in_=stg[i][0:rows, :]),
                      reads=[stg_b[i]])
            co.proj(w_in, D_IN_AB, t0, epi)
        cx.finish()
    return nc


HD = 128
C_HEADS = 16
HALO = 512


def att_bias_table(rel_bias):
    s_l = np.arange(128)[:, None, None]
    i = np.arange(5)[None, :, None]
    q_l = np.arange(128)[None, None, :]
    rel = 512 + q_l - 128 * i - s_l
    dchunk = q_l // 64 + 8 - 2 * i - s_l // 64
    ok = (dchunk >= 0) & (dchunk <= 8)
    idx = np.clip(rel, -256, 256) + 256
    tab = rel_bias[:, idx]
    tab = np.where(ok[None], tab, np.float32(-30000.0)).astype(np.float32)
    return np.ascontiguousarray(tab.transpose(1, 0, 2, 3).reshape(128, 16, 640))


def emit_attention(cx, qT, kT, v, biasT, vones, oT, ntok=TOKC):
    P = cx.P
    NQT = ntok // 128
    NKT = NQT + 4
    qb = cx.sb([128, C_HEADS, ntok], BF16, "qb")
    kb = cx.sb([128, C_HEADS, ntok + HALO], BF16, "kb")
    vb = cx.sb([128, NKT, D], BF16, "vb")
    bias = cx.sb([128, C_HEADS, 640], F32, "bias")
    ones = cx.sb([128, 128], BF16, "aones")
    von = cx.sb([128, 128], BF16, "vones")
    qb_b = [Buf("qb%d" % h) for h in range(C_HEADS)]
    kb_b = [Buf("kb%d" % h) for h in range(C_HEADS)]
    vb_b = [Buf("vb%d" % t) for t in range(NKT)]
    bias_b, ones_b, von_b = Buf("bias"), Buf("ones"), Buf("von")
    P.op("dve", lambda e: e.memset(ones[:], 1.0), writes=[ones_b])
    P.dma("pool", lambda e: e.dma_start(out=von[:], in_=vones), writes=[von_b])
    P.dma("sp", lambda e: e.dma_start(out=bias[:], in_=biasT), writes=[bias_b])
    qsrc = qT.rearrange("(h p) t -> p h t", p=128)
    ksrc = kT.rearrange("(h p) t -> p h t", p=128)
    vsrc = v.rearrange("(n p) d -> p n d", p=128)
    for h in range(C_HEADS):
        P.dma("pool", lambda e, h=h: e.dma_start(out=qb[:, h, :], in_=qsrc[:, h, :]), writes=[qb_b[h]])
        P.dma("pool", lambda e, h=h: e.dma_start(out=kb[:, h, :], in_=ksrc[:, h, :]), writes=[kb_b[h]])
    for t in range(NKT):
        P.dma("pool", lambda e, t=t: e.dma_start(out=vb[:, t, :], in_=vsrc[:, t, :]), writes=[vb_b[t]])
    psA = [cx.ps(name="attA%d" % i) for i in range(2)]
    psB = [cx.ps(name="attB%d" % i) for i in range(2)]
    psO = [cx.ps(name="attO%d" % i) for i in range(2)]
    psA_b = [Buf("psA%d" % i) for i in range(2)]
    psB_b = [Buf("psB%d" % i) for i in range(2)]
    psO_b = [Buf("psO%d" % i) for i in range(2)]
    tmp = [cx.sb([128, 640], F32, "atmp%d" % i) for i in range(2)]
    tmp_b = [Buf("atmp%d" % i) for i in range(2)]
    pT = [cx.sb([128, 640], BF16, "apT%d" % i) for i in range(2)]
    pT_b = [Buf("apT%d" % i) for i in range(2)]
    rec = [cx.sb([128, 128], F32, "arec%d" % i) for i in range(2)]
    rec_b = [Buf("arec%d" % i) for i in range(2)]
    ost = [cx.sb([128, ntok], F32, "aost%d" % i) for i in range(2)]
    ost_b = [Buf("aost%d" % i) for i in range(2)]
    odst = oT.rearrange("(h p) t -> p h t", p=128)
    scale = float(HD) ** -0.5
    def stage_a(h, qt, a):
        qs = slice(qt * 128, (qt + 1) * 128)
        for i in range(5):
            kt = qt + i
            dst = psA[a][:, i * 128:(i + 1) * 128] if i < 4 else psB[a][:, 0:128]
            dst_b = psA_b[a] if i < 4 else psB_b[a]
            P.op("pe", lambda e, dst=dst, kt=kt: e.matmul(
                dst, kb[:, h, kt * 128:(kt + 1) * 128], qb[:, h, qs], start=True, stop=True),
                reads=[kb_b[h], qb_b[h]], writes=[dst_b])
        P.op("dve", lambda e: e.scalar_tensor_tensor(
            out=tmp[a][:, 0:512], in0=psA[a][:, 0:512], scalar=scale, in1=bias[:, h, 0:512], op0=ALU.mult, op1=ALU.add),
            reads=[psA_b[a], bias_b], writes=[tmp_b[a]])
        P.op("dve", lambda e: e.scalar_tensor_tensor(
            out=tmp[a][:, 512:640], in0=psB[a][:, 0:128], scalar=scale, in1=bias[:, h, 512:640], op0=ALU.mult, op1=ALU.add),
            reads=[psB_b[a], bias_b], writes=[tmp_b[a]])
        P.op("act", lambda e: e.activation(out=pT[a][:], in_=tmp[a][:], func=AF.Exp),
             reads=[tmp_b[a]], writes=[pT_b[a]])

    def stage_b(h, qt, a):
        qs = slice(qt * 128, (qt + 1) * 128)
        oi = h % 2
        for i in range(5):
            kt = qt + i
            P.op("pe", lambda e, kt=kt, i=i: e.matmul(
                psO[a][:, 0:128], vb[:, kt, h * 128:(h + 1) * 128], pT[a][:, i * 128:(i + 1) * 128],
                start=(i == 0), stop=(i == 4)), reads=[vb_b[kt], pT_b[a]], writes=[psO_b[a]])
        for i in range(5):
            kt = qt + i
            on = von if kt < 4 else ones
            on_b = von_b if kt < 4 else ones_b
            P.op("pe", lambda e, on=on, i=i: e.matmul(
                psO[a][:, 128:256], on[:], pT[a][:, i * 128:(i + 1) * 128],
                start=(i == 0), stop=(i == 4)), reads=[on_b, pT_b[a]], writes=[psO_b[a]])
        P.op("dve", lambda e: e.reciprocal(rec[a][:], psO[a][:, 128:256]), reads=[psO_b[a]], writes=[rec_b[a]])
        P.op("dve", lambda e: e.tensor_tensor(
            out=ost[oi][:, qs], in0=psO[a][:, 0:128], in1=rec[a][:], op=ALU.mult),
            reads=[psO_b[a], rec_b[a]], writes=[ost_b[oi]])
        if qt == NQT - 1:
            P.dma("sp", lambda e: e.dma_start(out=odst[:, h, :], in_=ost[oi][:]), reads=[ost_b[oi]])

    units = [(h, qt) for h in range(C_HEADS) for qt in range(NQT)]
    stage_a(units[0][0], units[0][1], 0)
    for k in range(1, len(units)):
        stage_a(units[k][0], units[k][1], k % 2)
        stage_b(units[k - 1][0], units[k - 1][1], (k - 1) % 2)
    stage_b(units[-1][0], units[-1][1], (len(units) - 1) % 2)


def build_ATT():
    import contextlib
    nc = bass.Bass("TRN2", target_bir_lowering=False)
    with contextlib.ExitStack() as st:
        cx = Ctx(nc, st)
        qT = cx.din("qT", [D, TOKC])
        kT = cx.din("kT", [D, TOKC + HALO])
        v = cx.din("v", [TOKC + HALO, D])
        biasT = cx.din("biasT", [128, C_HEADS, 640])
        vones = cx.din("vones", [128, 128])
        oT = cx.dout("oT", [D, TOKC])
        emit_attention(cx, qT, kT, v, biasT, vones, oT)
        cx.finish()
    return nc


class TB:
    def __init__(self, cx, shape, dt, name):
        self.t = cx.sb(shape, dt, name)
        self.b = Buf(name)


S5_LC = 512
S5_NJ = 4


def s5_host_layout(inp, core):
    g0 = core * 8
    lam = np.zeros((128, S5_NJ, 3), np.float32)
    Bre = np.zeros((128, S5_NJ, 128), np.float32)
    Bim = np.zeros((128, S5_NJ, 128), np.float32)
    Cre = np.zeros((128, S5_NJ, 128), np.float32)
    Cim = np.zeros((128, S5_NJ, 128), np.float32)
    for j in range(S5_NJ):
        for gl in range(2):
            g8 = 2 * j + gl
            g = g0 + g8
            sl = slice(gl * 64, gl * 64 + 64)
            lam[sl, j, 0] = inp['s5_lam_re'][0, g]
            lam[sl, j, 1] = inp['s5_lam_im'][0, g]
            lam[sl, j, 2] = inp['s5_log_dt'][0, g]
            Bre[16 * g8:16 * g8 + 16, j, sl] = inp['s5_b_re'][0, g].T
            Bim[16 * g8:16 * g8 + 16, j, sl] = inp['s5_b_im'][0, g].T
            Cre[sl, j, 16 * g8:16 * g8 + 16] = inp['s5_c_re'][0, g].T
            Cim[sl, j, 16 * g8:16 * g8 + 16] = inp['s5_c_im'][0, g].T
    dsk = np.ascontiguousarray(inp['s5_d'][0, core * 128:(core + 1) * 128].reshape(128, 1))
    return {"lam": lam, "Bre": Bre, "Bim": Bim, "Cre": Cre, "Cim": Cim, "dsk": dsk}


def emit_s5(cx, uT, lam, Bre, Bim, Cre, Cim, dsk, yT, L=SEQ):
    P = cx.P
    LC, NJ = S5_LC, S5_NJ
    NCH = L // LC
    u32 = TB(cx, [128, L], F32, "u32")
    ub = TB(cx, [128, L], BF16, "ub")
    for c4 in range(4):
        sl = slice(c4 * (L // 4), (c4 + 1) * (L // 4))
        P.dma("sp", lambda e, sl=sl: e.dma_start(out=u32.t[:, sl], in_=uT[:, sl]), writes=[u32.b])
    for c4 in range(4):
        sl = slice(c4 * (L // 4), (c4 + 1) * (L // 4))
        P.op("act", lambda e, sl=sl: e.activation(out=ub.t[:, sl], in_=u32.t[:, sl], func=AF.Copy), reads=[u32.b], writes=[ub.b])
    lm = TB(cx, [128, NJ, 3], F32, "lam")
    P.dma("sp", lambda e: e.dma_start(out=lm.t[:], in_=lam), writes=[lm.b])
    dk = TB(cx, [128, 1], F32, "dsk")
    P.dma("sp", lambda e: e.dma_start(out=dk.t[:], in_=dsk), writes=[dk.b])
    mats = {}
    for nm, src in (("Bre", Bre), ("Bim", Bim), ("Cre", Cre), ("Cim", Cim)):
        m = TB(cx, [128, NJ, 128], BF16, "m" + nm)
        P.dma("pool", lambda e, m=m, src=src: e.dma_start(out=m.t[:], in_=src), writes=[m.b])
        mats[nm] = m
    P.op("dve", lambda e: e.tensor_scalar(out=mats["Cim"].t[:], in0=mats["Cim"].t[:], scalar1=-1.0, scalar2=None, op0=ALU.mult),
         reads=[mats["Cim"].b], writes=[mats["Cim"].b])
    def sc(name):
        return TB(cx, [128, NJ], F32, name)
    dt, lrdt, th, mag, den, rden = sc("dt"), sc("lrdt"), sc("th"), sc("mag"), sc("den"), sc("rden")
    abre, abim, nr, fre, fim, t1, t2 = sc("abre"), sc("abim"), sc("nr"), sc("fre"), sc("fim"), sc("t1"), sc("t2")
    lr, li, ldt = lm.t[:, :, 0], lm.t[:, :, 1], lm.t[:, :, 2]
    P.op("act", lambda e: e.activation(out=dt.t[:], in_=ldt, func=AF.Exp), reads=[lm.b], writes=[dt.b])
    P.op("dve", lambda e: e.tensor_tensor(out=lrdt.t[:], in0=lr, in1=dt.t[:], op=ALU.mult), reads=[lm.b, dt.b], writes=[lrdt.b])
    P.op("dve", lambda e: e.tensor_tensor(out=th.t[:], in0=li, in1=dt.t[:], op=ALU.mult), reads=[lm.b, dt.b], writes=[th.b])
    P.op("act", lambda e: e.activation(out=mag.t[:], in_=lrdt.t[:], func=AF.Exp), reads=[lrdt.b], writes=[mag.b])
    NLV = 16
    Wre = TB(cx, [128, NLV, NJ], F32, "Wre")
    Wim = TB(cx, [128, NLV, NJ], F32, "Wim")
    hpi = TB(cx, [128, 1], F32, "hpi")
    P.op("dve", lambda e: e.memset(hpi.t[:], float(np.pi / 2)), writes=[hpi.b])
    P.op("act", lambda e: e.activation(out=Wim.t[:, 0, :], in_=th.t[:], func=AF.Sin, scale=1.0 / 64), reads=[th.b], writes=[Wim.b])
    P.op("act", lambda e: e.activation(out=Wre.t[:, 0, :], in_=th.t[:], func=AF.Sin, scale=1.0 / 64, bias=hpi.t[:, 0:1]),
         reads=[th.b, hpi.b], writes=[Wre.b])
    for lv in range(1, NLV):
        a, b_ = Wre.t[:, lv - 1, :], Wim.t[:, lv - 1, :]
        P.op("dve", lambda e, a=a: e.tensor_tensor(out=t1.t[:], in0=a, in1=a, op=ALU.mult), reads=[Wre.b], writes=[t1.b])
        P.op("dve", lambda e, b_=b_: e.tensor_tensor(out=t2.t[:], in0=b_, in1=b_, op=ALU.mult), reads=[Wim.b], writes=[t2.b])
        P.op("dve", lambda e, lv=lv: e.tensor_tensor(out=Wre.t[:, lv, :], in0=t1.t[:], in1=t2.t[:], op=ALU.subtract),
             reads=[t1.b, t2.b], writes=[Wre.b])
        P.op("dve", lambda e, lv=lv, a=a, b_=b_: e.scalar_tensor_tensor(out=Wim.t[:, lv, :], in0=a, scalar=2.0, in1=b_, op0=ALU.mult, op1=ALU.mult),
             reads=[Wre.b, Wim.b], writes=[Wim.b])
    cth, sth = Wre.t[:, 6, :], Wim.t[:, 6, :]
    P.op("dve", lambda e: e.tensor_tensor(out=abre.t[:], in0=mag.t[:], in1=cth, op=ALU.mult), reads=[mag.b, Wre.b], writes=[abre.b])
    P.op("dve", lambda e: e.tensor_tensor(out=abim.t[:], in0=mag.t[:], in1=sth, op=ALU.mult), reads=[mag.b, Wim.b], writes=[abim.b])
    P.op("dve", lambda e: e.tensor_scalar(out=nr.t[:], in0=abre.t[:], scalar1=-1.0, scalar2=None, op0=ALU.add), reads=[abre.b], writes=[nr.b])
    P.op("dve", lambda e: e.tensor_tensor(out=t1.t[:], in0=lr, in1=lr, op=ALU.mult), reads=[lm.b], writes=[t1.b])
    P.op("dve", lambda e: e.tensor_tensor(out=t2.t[:], in0=li, in1=li, op=ALU.mult), reads=[lm.b], writes=[t2.b])
    P.op("dve", lambda e: e.tensor_tensor(out=den.t[:], in0=t1.t[:], in1=t2.t[:], op=ALU.add), reads=[t1.b, t2.b], writes=[den.b])
    P.op("dve", lambda e: e.reciprocal(rden.t[:], den.t[:]), reads=[den.b], writes=[rden.b])
    P.op("dve", lambda e: e.tensor_tensor(out=t1.t[:], in0=nr.t[:], in1=lr, op=ALU.mult), reads=[nr.b, lm.b], writes=[t1.b])
    P.op("dve", lambda e: e.tensor_tensor(out=t2.t[:], in0=abim.t[:], in1=li, op=ALU.mult), reads=[abim.b, lm.b], writes=[t2.b])
    P.op("dve", lambda e: e.tensor_tensor(out=t1.t[:], in0=t1.t[:], in1=t2.t[:], op=ALU.add), reads=[t1.b, t2.b], writes=[t1.b])
    P.op("dve", lambda e: e.tensor_tensor(out=fre.t[:], in0=t1.t[:], in1=rden.t[:], op=ALU.mult), reads=[t1.b, rden.b], writes=[fre.b])
    P.op("dve", lambda e: e.tensor_tensor(out=t1.t[:], in0=abim.t[:], in1=lr, op=ALU.mult), reads=[abim.b, lm.b], writes=[t1.b])
    P.op("dve", lambda e: e.tensor_tensor(out=t2.t[:], in0=nr.t[:], in1=li, op=ALU.mult), reads=[nr.b, lm.b], writes=[t2.b])
    P.op("dve", lambda e: e.tensor_tensor(out=t1.t[:], in0=t1.t[:], in1=t2.t[:], op=ALU.subtract), reads=[t1.b, t2.b], writes=[t1.b])
    P.op("dve", lambda e: e.tensor_tensor(out=fim.t[:], in0=t1.t[:], in1=rden.t[:], op=ALU.mult), reads=[t1.b, rden.b], writes=[fim.b])
    cosT = TB(cx, [128, NJ, LC], F32, "cosT")
    sinT = TB(cx, [128, NJ, LC], F32, "sinT")
    Gre = TB(cx, [128, NJ, LC], F32, "Gre")
    Gim = TB(cx, [128, NJ, LC], F32, "Gim")
    rho = TB(cx, [128, NJ, LC], F32, "rho")
    tw = TB(cx, [128, LC], F32, "tw")
    P.op("pool", lambda e: e.memset(cosT.t[:, :, 0:1], 1.0), writes=[cosT.b])
    P.op("pool", lambda e: e.memset(sinT.t[:, :, 0:1], 0.0), writes=[sinT.b])
    P.op("pool", lambda e: e.memset(rho.t[:], 1.0), writes=[rho.b])
    for j in range(NJ):
        P.op("dve", lambda e, j=j: e.tensor_scalar(out=rho.t[:, j, :], in0=rho.t[:, j, :], scalar1=mag.t[:, j:j + 1], scalar2=None, op0=ALU.mult),
             reads=[rho.b, mag.b], writes=[rho.b])
        m = 1
        lv = 6
        while m < LC:
            wre, wim = Wre.t[:, lv, j:j + 1], Wim.t[:, lv, j:j + 1]
            src_re, src_im = cosT.t[:, j, 0:m], sinT.t[:, j, 0:m]
            P.op("dve", lambda e, m=m, wim=wim, src_im=src_im: e.tensor_scalar(out=tw.t[:, 0:m], in0=src_im, scalar1=wim, scalar2=None, op0=ALU.mult),
                 reads=[sinT.b, Wim.b], writes=[tw.b])
            P.op("dve", lambda e, m=m, j=j, wre=wre, src_re=src_re: e.scalar_tensor_tensor(
                out=cosT.t[:, j, m:2 * m], in0=src_re, scalar=wre, in1=tw.t[:, 0:m], op0=ALU.mult, op1=ALU.subtract),
                reads=[cosT.b, Wre.b, tw.b], writes=[cosT.b])
            P.op("dve", lambda e, m=m, wre=wre, src_im=src_im: e.tensor_scalar(out=tw.t[:, 0:m], in0=src_im, scalar1=wre, scalar2=None, op0=ALU.mult),
                 reads=[sinT.b, Wre.b], writes=[tw.b])
            P.op("dve", lambda e, m=m, j=j, wim=wim, src_re=src_re: e.scalar_tensor_tensor(
                out=sinT.t[:, j, m:2 * m], in0=src_re, scalar=wim, in1=tw.t[:, 0:m], op0=ALU.mult, op1=ALU.add),
                reads=[cosT.b, Wim.b, tw.b], writes=[sinT.b])
            m *= 2
            lv += 1
        P.op("dve", lambda e, j=j: e.tensor_scalar(out=tw.t[:], in0=sinT.t[:, j, :], scalar1=fim.t[:, j:j + 1], scalar2=None, op0=ALU.mult),
             reads=[sinT.b, fim.b], writes=[tw.b])
        P.op("dve", lambda e, j=j: e.scalar_tensor_tensor(out=Gre.t[:, j, :], in0=cosT.t[:, j, :], scalar=fre.t[:, j:j + 1], in1=tw.t[:],
                                                          op0=ALU.mult, op1=ALU.add), reads=[cosT.b, fre.b, tw.b], writes=[Gre.b])
        P.op("dve", lambda e, j=j: e.tensor_scalar(out=tw.t[:], in0=sinT.t[:, j, :], scalar1=fre.t[:, j:j + 1], scalar2=None, op0=ALU.mult),
             reads=[sinT.b, fre.b], writes=[tw.b])
        P.op("dve", lambda e, j=j: e.scalar_tensor_tensor(out=Gim.t[:, j, :], in0=cosT.t[:, j, :], scalar=fim.t[:, j:j + 1], in1=tw.t[:],
                                                          op0=ALU.mult, op1=ALU.subtract), reads=[cosT.b, fim.b, tw.b], writes=[Gim.b])
    Ere, Eim = Wre.t[:, 15, :], Wim.t[:, 15, :]
    NW = 3
    W = []
    for i in range(NW):
        d = {}
        for nm in ("sre", "sim", "a", "b", "a2", "b2", "cre", "cim", "zre", "zim"):
            d[nm] = TB(cx, [128, LC], F32, "%s%d" % (nm, i))
        for nm in ("xre", "xim"):
            d[nm] = TB(cx, [128, LC], BF16, "%s%d" % (nm, i))
        d["pre"] = cx.ps(name="s5pre%d" % i)
        d["pim"] = cx.ps(name="s5pim%d" % i)
        d["pre_b"], d["pim_b"] = Buf("pre"), Buf("pim")
        W.append(d)
    psY = [cx.ps(name="s5y%d" % i) for i in range(2)]
    psY_b = [Buf("psY%d" % i) for i in range(2)]
    init = [[TB(cx, [128, 2], F32, "init%d_%d" % (j, k)) for k in range(2)] for j in range(NJ)]
    for j in range(NJ):
        P.op("pool", lambda e, j=j: e.memset(init[j][0].t[:], 0.0), writes=[init[j][0].b])
    ct = TB(cx, [128, 2], F32, "ct")
    yw = [{nm: TB(cx, [128, LC], F32, "y%s%d" % (nm, i)) for nm in ("y", "y2", "v", "s", "o")} for i in range(2)]
    un = 0
    for ch in range(NCH):
        ts_ = slice(ch * LC, (ch + 1) * LC)
        py, py_b = psY[ch % 2], psY_b[ch % 2]
        for j in range(NJ):
            w = W[un % NW]
            un += 1
            ini, nini = init[j][ch % 2], init[j][(ch + 1) % 2]
            P.op("pe", lambda e, w=w, j=j, ts_=ts_: e.matmul(w["pre"][:], mats["Bre"].t[:, j, :], ub.t[:, ts_], start=True, stop=True),
                 reads=[mats["Bre"].b, ub.b], writes=[w["pre_b"]])
            P.op("pe", lambda e, w=w, j=j, ts_=ts_: e.matmul(w["pim"][:], mats["Bim"].t[:, j, :], ub.t[:, ts_], start=True, stop=True),
                 reads=[mats["Bim"].b, ub.b], writes=[w["pim_b"]])
            P.op("act", lambda e, w=w: e.activation(out=w["sre"].t[:], in_=w["pre"][:], func=AF.Copy), reads=[w["pre_b"]], writes=[w["sre"].b])
            P.op("act", lambda e, w=w: e.activation(out=w["sim"].t[:], in_=w["pim"][:], func=AF.Copy), reads=[w["pim_b"]], writes=[w["sim"].b])
            def tt(eng, out, in0, in1, op):
                P.op(eng, lambda e: e.tensor_tensor(out=out.t[:], in0=in0[0], in1=in1[0], op=op),
                     reads=[in0[1], in1[1]], writes=[out.b])
            gre, gim = (Gre.t[:, j, :], Gre.b), (Gim.t[:, j, :], Gim.b)
            cs, sn = (cosT.t[:, j, :], cosT.b), (sinT.t[:, j, :], sinT.b)
            S = lambda x: (x.t[:], x.b)
            tt("pool", w["a2"], gre, S(w["sre"]), ALU.mult)
            tt("pool", w["b2"], gim, S(w["sim"]), ALU.mult)
            tt("pool", w["cre"], S(w["a2"]), S(w["b2"]), ALU.subtract)
            tt("pool", w["a2"], gre, S(w["sim"]), ALU.mult)
            tt("pool", w["b2"], gim, S(w["sre"]), ALU.mult)
            tt("pool", w["cim"], S(w["a2"]), S(w["b2"]), ALU.add)
            P.op("dve", lambda e, w=w, j=j, ini=ini: e.tensor_tensor_scan(w["zre"].t[:], rho.t[:, j, :], w["cre"].t[:], ini.t[:, 0:1], ALU.mult, ALU.add),
                 reads=[rho.b, w["cre"].b, ini.b], writes=[w["zre"].b])
            P.op("dve", lambda e, w=w, j=j, ini=ini: e.tensor_tensor_scan(w["zim"].t[:], rho.t[:, j, :], w["cim"].t[:], ini.t[:, 1:2], ALU.mult, ALU.add),
                 reads=[rho.b, w["cim"].b, ini.b], writes=[w["zim"].b])
            zre_e, zim_e = w["zre"].t[:, LC - 1:LC], w["zim"].t[:, LC - 1:LC]
            ere, eim = Ere[:, j:j + 1], Eim[:, j:j + 1]
            P.op("dve", lambda e, zim_e=zim_e, eim=eim: e.tensor_scalar(out=ct.t[:, 0:1], in0=zim_e, scalar1=eim, scalar2=None, op0=ALU.mult),
                 reads=[w["zim"].b, Wim.b], writes=[ct.b])
            P.op("dve", lambda e, zre_e=zre_e, ere=ere, nini=nini: e.scalar_tensor_tensor(
                out=nini.t[:, 0:1], in0=zre_e, scalar=ere, in1=ct.t[:, 0:1], op0=ALU.mult, op1=ALU.subtract),
                reads=[w["zre"].b, Wre.b, ct.b], writes=[nini.b])
            P.op("dve", lambda e, zre_e=zre_e, eim=eim: e.tensor_scalar(out=ct.t[:, 1:2], in0=zre_e, scalar1=eim, scalar2=None, op0=ALU.mult),
                 reads=[w["zre"].b, Wim.b], writes=[ct.b])
            P.op("dve", lambda e, zim_e=zim_e, ere=ere, nini=nini: e.scalar_tensor_tensor(
                out=nini.t[:, 1:2], in0=zim_e, scalar=ere, in1=ct.t[:, 1:2], op0=ALU.mult, op1=ALU.add),
                reads=[w["zim"].b, Wre.b, ct.b], writes=[nini.b])
            tt("dve", w["a"], cs, S(w["zre"]), ALU.mult)
            tt("dve", w["b"], sn, S(w["zim"]), ALU.mult)
            tt("dve", w["xre"], S(w["a"]), S(w["b"]), ALU.subtract)
            tt("dve", w["a"], sn, S(w["zre"]), ALU.mult)
            tt("dve", w["b"], cs, S(w["zim"]), ALU.mult)
            tt("dve", w["xim"], S(w["a"]), S(w["b"]), ALU.add)
            P.op("pe", lambda e, w=w, j=j, py=py: e.matmul(py[:], mats["Cre"].t[:, j, :], w["xre"].t[:], start=(j == 0), stop=False),
                 reads=[mats["Cre"].b, w["xre"].b], writes=[py_b])
            P.op("pe", lambda e, w=w, j=j, py=py: e.matmul(py[:], mats["Cim"].t[:, j, :], w["xim"].t[:], start=False, stop=(j == NJ - 1)),
                 reads=[mats["Cim"].b, w["xim"].b], writes=[py_b])
        Y = yw[ch % 2]
        P.op("dve", lambda e, Y=Y, py=py, ts_=ts_: e.scalar_tensor_tensor(out=Y["y"].t[:], in0=u32.t[:, ts_], scalar=dk.t[:, 0:1], in1=py[:],
                                                                         op0=ALU.mult, op1=ALU.add), reads=[u32.b, dk.b, py_b], writes=[Y["y"].b])
        P.op("pool", lambda e, Y=Y: e.tensor_tensor(out=Y["y2"].t[:], in0=Y["y"].t[:], in1=Y["y"].t[:], op=ALU.mult), reads=[Y["y"].b], writes=[Y["y2"].b])
        P.op("pool", lambda e, Y=Y: e.tensor_scalar(out=Y["y2"].t[:], in0=Y["y2"].t[:], scalar1=0.044715, scalar2=1.0, op0=ALU.mult, op1=ALU.add),
             reads=[Y["y2"].b], writes=[Y["y2"].b])
        P.op("pool", lambda e, Y=Y: e.tensor_tensor(out=Y["v"].t[:], in0=Y["y2"].t[:], in1=Y["y"].t[:], op=ALU.mult), reads=[Y["y2"].b, Y["y"].b], writes=[Y["v"].b])
        P.op("act", lambda e, Y=Y: e.activation(out=Y["s"].t[:], in_=Y["v"].t[:], func=AF.Sigmoid, scale=1.5957691216057308),
             reads=[Y["v"].b], writes=[Y["s"].b])
        P.op("pool", lambda e, Y=Y: e.tensor_tensor(out=Y["o"].t[:], in0=Y["s"].t[:], in1=Y["y"].t[:], op=ALU.mult), reads=[Y["s"].b, Y["y"].b], writes=[Y["o"].b])
        P.dma("sp", lambda e, Y=Y, ts_=ts_: e.dma_start(out=yT[:, ts_], in_=Y["o"].t[:]), reads=[Y["o"].b])


def build_S5():
    import contextlib
    nc = bass.Bass("TRN2", target_bir_lowering=False)
    with contextlib.ExitStack() as st:
        cx = Ctx(nc, st)
        uT = cx.din("uT", [128, SEQ])
        lam = cx.din("lam", [128, S5_NJ, 3])
        Bre = cx.din("Bre", [128, S5_NJ, 128])
        Bim = cx.din("Bim", [128, S5_NJ, 128])
        Cre = cx.din("Cre", [128, S5_NJ, 128])
        Cim = cx.din("Cim", [128, S5_NJ, 128])
        dsk = cx.din("dsk", [128, 1])
        yT = cx.dout("yT", [128, SEQ])
        emit_s5(cx, uT, lam, Bre, Bim, Cre, Cim, dsk, yT)
        cx.finish()
    return nc


NSLOT = 8
TOPK = 256
NBIS = 16


def slot_tiles(i):
    return 8 * (i + 1)


def slot_off(i):
    return 4 * i * (i + 1)


NT_TOTAL = slot_off(NSLOT)


def dsa_negpos(core):
    return (np.arange(SEQ)[None, :] - 128 * core - np.arange(128)[:, None]).astype(np.float32)


def dsa_adm_mask(core):
    r = np.arange(1024)[None, :]
    ql = np.arange(128)[:, None]
    kch = r // 64
    qch = (128 * core + ql) // 64
    return np.where(kch <= qch, 0.0, -1e30).astype(np.float32)


DBG = {"nslot": 8, "bis": NBIS, "tr": True, "idx": True}


def emit_dsa_index(cx, kidxT, qidxT, widx, adm, pow2, ident, selT_out, negpos=None, dmin_out=None):
    P = cx.P
    S = SEQ
    kb = TB(cx, [64, S], BF16, "ikb")
    qb = TB(cx, [64, 16, NSLOT * 128], BF16, "iqb")
    for c4 in range(4):
        sl = slice(c4 * 2048, (c4 + 1) * 2048)
        P.dma("pool", lambda e, sl=sl: e.dma_start(out=kb.t[:, sl], in_=kidxT[:, sl]), writes=[kb.b])
    qsrc = qidxT.rearrange("(h d) t -> d h t", d=64)
    for h4 in range(4):
        P.dma("pool", lambda e, h4=h4: e.dma_start(out=qb.t[:, h4 * 4:(h4 + 1) * 4, :], in_=qsrc[:, h4 * 4:(h4 + 1) * 4, :]), writes=[qb.b])
    w = TB(cx, [128, NSLOT, 16], F32, "iw")
    wa = TB(cx, [128, NSLOT, 16], F32, "iwa")
    wsg = TB(cx, [128, NSLOT, 16], F32, "iws")
    am = TB(cx, [128, 1024], F32, "iadm")
    p2 = TB(cx, [128, NBIS + 1], F32, "ip2")
    idf = TB(cx, [128, 128], F32, "iidf")
    idb = TB(cx, [128, 128], BF16, "iidb")
    P.dma("sp", lambda e: e.dma_start(out=w.t[:], in_=widx), writes=[w.b])
    P.dma("sp", lambda e: e.dma_start(out=am.t[:], in_=adm), writes=[am.b])
    P.dma("sp", lambda e: e.dma_start(out=p2.t[:], in_=pow2), writes=[p2.b])
    P.dma("sp", lambda e: e.dma_start(out=idf.t[:], in_=ident), writes=[idf.b])
    P.op("dve", lambda e: e.tensor_copy(idb.t[:], idf.t[:]), reads=[idf.b], writes=[idb.b])
    P.op("act", lambda e: e.activation(out=wa.t[:], in_=w.t[:], func=AF.Abs), reads=[w.b], writes=[wa.b])
    P.op("dve", lambda e: e.tensor_scalar(out=wsg.t[:], in0=w.t[:], scalar1=0.0, scalar2=2.0, op0=ALU.is_ge, op1=ALU.mult),
         reads=[w.b], writes=[wsg.b])
    P.op("dve", lambda e: e.tensor_scalar(out=wsg.t[:], in0=wsg.t[:], scalar1=-1.0, scalar2=None, op0=ALU.add), reads=[wsg.b], writes=[wsg.b])
    sc = TB(cx, [128, S], F32, "isc")
    jk = TB(cx, [128, S], BF16, "ijk")
    rr = [TB(cx, [128, 512], F32, "irr%d" % i) for i in range(3)]
    ps = [cx.ps(name="ips%d" % i) for i in range(4)]
    ps_b = [Buf("ips%d" % i) for i in range(4)]
    pst = [cx.ps([128, 512], BF16, name="ipt%d" % i) for i in range(2)]
    pst_b = [Buf("ipt%d" % i) for i in range(2)]
    stg = [TB(cx, [128, 4, 128], BF16, "istg%d" % i) for i in range(2)]
    M = TB(cx, [128, 1], F32, "iM")
    steps = TB(cx, [128, NBIS + 1], F32, "isteps")
    nsteps = TB(cx, [128, NBIS + 1], F32, "insteps")
    mid = [TB(cx, [128, 1], F32, "imid%d" % i) for i in range(2)]
    cnt = TB(cx, [128, 1], F32, "icnt")
    dd = TB(cx, [128, 1], F32, "idd")
    scs = [sc, TB(cx, [128, S], F32, "isc2")]
    st = {"n": 0, "nt": 0, "nacc": 0}
    rrb = [TB(cx, [128, 512], BF16, "irrb%d" % k) for k in range(4)]
    pacc = [cx.ps(name="ipacc%d" % k) for k in range(2)]
    pacc_b = [Buf("ipacc%d" % k) for k in range(2)]
    Dh = [TB(cx, [128, 16, 128], BF16, "iDh%d" % k) for k in range(2)]

    def gen_idx(i):
        n = st["n"]
        Si = 1024 * (i + 1)
        qs = slice(i * 128, (i + 1) * 128)
        dh = Dh[i % 2]
        for h in range(16):
            P.op("dve", lambda e, h=h: e.tensor_scalar(out=dh.t[:, h, :], in0=idb.t[:], scalar1=w.t[:, i, h:h + 1], scalar2=None, op0=ALU.mult),
                 reads=[idb.b, w.b], writes=[dh.b])
        units = []
        for kbk in range(Si // 512 if DBG["idx"] else 0):
            for h in range(16):
                units.append((kbk, h, n % 4, (st["nacc"] + kbk) % 2))
                n += 1
        st["nacc"] += Si // 512

        def st_a(un):
            kbk, h, bi, ai = un
            ks = slice(kbk * 512, (kbk + 1) * 512)
            pp, pp_b, r = ps[bi], ps_b[bi], rrb[bi]
            P.op("pe", lambda e: e.matmul(pp[:], qb.t[:, h, qs], kb.t[:, ks], start=True, stop=True),
                 reads=[qb.b, kb.b], writes=[pp_b])
            if h % 4 != 3:
                P.op("act", lambda e: e.activation(out=r.t[:], in_=pp[:], func=AF.Relu), reads=[pp_b], writes=[r.b])
            else:
                P.op("dve", lambda e: e.tensor_scalar(out=r.t[:], in0=pp[:], scalar1=0.0, scalar2=None, op0=ALU.max),
                     reads=[pp_b], writes=[r.b])

        def st_b(un):
            kbk, h, bi, ai = un
            ks = slice(kbk * 512, (kbk + 1) * 512)
            pa, pa_b, r = pacc[ai], pacc_b[ai], rrb[bi]
            P.op("pe", lambda e: e.matmul(pa[:], dh.t[:, h, :], r.t[:], start=(h == 0), stop=(h == 15)),
                 reads=[dh.b, r.b], writes=[pa_b])
            if h == 15:
                P.op("act", lambda e: e.activation(out=scs[i % 2].t[:, ks], in_=pa[:], func=AF.Copy), reads=[pa_b], writes=[scs[i % 2].b])

        LOOK = 3
        for k in range(len(units) + LOOK):
            if k < len(units):
                st_a(units[k])
            if k >= LOOK:
                st_b(units[k - LOOK])
            if k % 4 == 3:
                yield
        st["n"] = n
        yield

    def gen_post(i):
        nt = st["nt"]
        Si = 1024 * (i + 1)
        P.op("dve", lambda e, Si=Si: e.tensor_reduce(out=M.t[:], in_=scs[i % 2].t[:, 0:Si], axis=AX.X, op=ALU.max, apply_absolute_value=True), reads=[scs[i % 2].b], writes=[M.b])
        P.op("dve", lambda e: e.tensor_scalar(out=M.t[:], in0=M.t[:], scalar1=1.001, scalar2=1e-20, op0=ALU.mult, op1=ALU.add), reads=[M.b], writes=[M.b])
        P.op("dve", lambda e, Si=Si: e.tensor_tensor(out=scs[i % 2].t[:, Si - 1024:Si], in0=scs[i % 2].t[:, Si - 1024:Si], in1=am.t[:], op=ALU.add),
             reads=[scs[i % 2].b, am.b], writes=[scs[i % 2].b])
        P.op("dve", lambda e: e.tensor_scalar(out=steps.t[:], in0=p2.t[:], scalar1=M.t[:, 0:1], scalar2=None, op0=ALU.mult), reads=[p2.b, M.b], writes=[steps.b])
        P.op("dve", lambda e: e.tensor_scalar(out=nsteps.t[:], in0=steps.t[:], scalar1=-1.0, scalar2=None, op0=ALU.mult), reads=[steps.b], writes=[nsteps.b])
        P.op("dve", lambda e: e.memset(mid[0].t[:], 0.0), writes=[mid[0].b])
        for k in range(DBG["bis"]):
            m0, m1 = mid[k % 2], mid[(k + 1) % 2]
            P.op("dve", lambda e, Si=Si, m0=m0: e.tensor_scalar(out=jk.t[:, 0:Si], in0=scs[i % 2].t[:, 0:Si], scalar1=m0.t[:, 0:1], scalar2=0.0,
                                                              op0=ALU.is_gt, op1=ALU.add, accum_out=cnt.t[:, 0:1]),
                 reads=[scs[i % 2].b, m0.b], writes=[jk.b, cnt.b])
            P.op("dve", lambda e, k=k: e.tensor_scalar(out=dd.t[:], in0=cnt.t[:], scalar1=float(TOPK), scalar2=steps.t[:, k:k + 1],
                                                       op0=ALU.is_ge, op1=ALU.mult), reads=[cnt.b, steps.b], writes=[dd.b])
            P.op("dve", lambda e, k=k, m0=m0, m1=m1: e.scalar_tensor_tensor(out=m1.t[:], in0=dd.t[:], scalar=nsteps.t[:, k + 1:k + 2], in1=m0.t[:],
                                                                          op0=ALU.add, op1=ALU.add), reads=[dd.b, nsteps.b, m0.b], writes=[m1.b])
            yield
        mf = mid[DBG["bis"] % 2]
        P.op("dve", lambda e, Si=Si, mf=mf: e.tensor_scalar(out=jk.t[:, 0:Si], in0=scs[i % 2].t[:, 0:Si], scalar1=mf.t[:, 0:1], scalar2=-30000.0,
                                                          op0=ALU.is_le, op1=ALU.mult), reads=[scs[i % 2].b, mf.b], writes=[jk.b])
        if negpos is not None:
            if i == 0:
                npos = TB(cx, [128, S], F32, "inpos")
                P.dma("sp", lambda e: e.dma_start(out=npos.t[:], in_=negpos), writes=[npos.b])
                offs = TB(cx, [128, NSLOT], F32, "ioffs")
                for ii in range(NSLOT):
                    P.op("pool", lambda e, ii=ii: e.memset(offs.t[:, ii:ii + 1], -1024.0 * ii), writes=[offs.b])
                dm = TB(cx, [128, NSLOT], F32, "idmin")
                emit_dsa_index._st = (npos, offs, dm)
            npos, offs, dm = emit_dsa_index._st
            P.op("act", lambda e, Si=Si, i=i: e.activation(out=scs[i % 2].t[:, 0:Si], in_=npos.t[:, 0:Si], func=AF.Abs, bias=offs.t[:, i:i + 1], scale=1.0),
                 reads=[npos.b, offs.b, scs[i % 2].b], writes=[scs[i % 2].b])
            P.op("pool", lambda e, Si=Si: e.tensor_tensor(out=scs[i % 2].t[:, 0:Si], in0=jk.t[:, 0:Si], in1=scs[i % 2].t[:, 0:Si], op=ALU.subtract),
                 reads=[jk.b, scs[i % 2].b], writes=[scs[i % 2].b])
            P.op("dve", lambda e, Si=Si, i=i: e.tensor_reduce(out=dm.t[:, i:i + 1], in_=scs[i % 2].t[:, 0:Si], axis=AX.X, op=ALU.max),
                 reads=[scs[i % 2].b], writes=[dm.b])
            if i == DBG["nslot"] - 1:
                P.dma("sp", lambda e: e.dma_start(out=dmin_out, in_=dm.t[:]), reads=[dm.b])
        for g in range(Si // 512 if DBG["tr"] else 0):
            pt, pt_b = pst[nt % 2], pst_b[nt % 2]
            sg = stg[nt % 2]
            nt += 1
            for j in range(4):
                tix = g * 4 + j
                P.op("pe", lambda e, pt=pt, j=j, tix=tix: e.transpose(pt[:, j * 128:(j + 1) * 128], jk.t[:, tix * 128:(tix + 1) * 128], idb.t[:]),
                     reads=[jk.b, idb.b], writes=[pt_b])
            P.op("act", lambda e, pt=pt, sg=sg: e.activation(out=sg.t[:].rearrange("p a b -> p (a b)"), in_=pt[:], func=AF.Copy),
                 reads=[pt_b], writes=[sg.b])
            t0 = slot_off(i) + g * 4
            P.dma("sp", lambda e, sg=sg, t0=t0: e.dma_start(out=selT_out[:, t0:t0 + 4, :], in_=sg.t[:]), reads=[sg.b])
            yield


        st["nt"] = nt
        yield

    ns = DBG["nslot"]
    for _ in gen_idx(0):
        pass
    for i in range(ns):
        gp = gen_post(i)
        gi = gen_idx(i + 1) if i + 1 < ns else iter(())
        n_idx = (4 * 2 * (i + 2)) if i + 1 < ns else 0
        n_post = DBG["bis"] + 2 * (i + 1) + 1
        per = max(1, -(-n_idx // max(1, DBG["bis"])))
        done_i = False
        for _ in gp:
            for _k in range(per):
                try:
                    next(gi)
                except StopIteration:
                    done_i = True
                    break
        for _ in gi:
            pass


def build_C1():
    import contextlib
    nc = bass.Bass("TRN2", target_bir_lowering=False)
    with contextlib.ExitStack() as st:
        cx = Ctx(nc, st)
        kidxT = cx.din("kidxT", [64, SEQ])
        qidxT = cx.din("qidxT", [1024, NSLOT * 128])
        widx = cx.din("widx", [128, NSLOT, 16])
        adm = cx.din("adm", [128, 1024])
        pow2 = cx.din("pow2", [128, NBIS + 1])
        ident = cx.din("ident", [128, 128])
        selT = cx.dout("selT", [128, NT_TOTAL, 128], BF16)
        negpos = cx.din("negpos", [128, SEQ])
        dmin = cx.dout("ndmin", [128, NSLOT])
        emit_dsa_index(cx, kidxT, qidxT, widx, adm, pow2, ident, selT, negpos, dmin)
        cx.finish()
    return nc


B_HEADS = 8
KV_RANK = 256


def dsa_alibi_tables(core):
    slopes = (2.0 ** (-8.0 * (np.arange(8) + 1) / 8)).astype(np.float32)
    s_l = np.arange(128)[:, None, None]
    idx = np.arange(64)[None, None, :]
    rel = s_l + 128 * (idx - 56 - core) - 64
    biascol = (slopes[None, :, None] * np.minimum(rel, 63)).astype(np.float32)
    corr = np.zeros((128, 8, 8, 128), np.float32)
    sl = np.arange(128)[:, None]
    ql = np.arange(128)[None, :]
    corr[:, core, :, :] = -2.0 * slopes[None, :, None] * np.maximum(sl - ql, 0)[:, None, :]
    return biascol, corr


def emit_dsa_attn(cx, kvT, kvg, wkv, qT, selT, biascol, corr, ident, ybT, ndminT=None, qoff=None):
    P = cx.P
    slopes = [2.0 ** (-8.0 * (h + 1) / 8) for h in range(B_HEADS)]
    nd = TB(cx, [1, NSLOT * 128], F32, "dnd")
    qo = TB(cx, [1, 128], F32, "dqo")
    rrow = TB(cx, [1, B_HEADS, NSLOT * 128], BF16, "drrow")
    ones1 = TB(cx, [1, 128], BF16, "dones1")
    P.dma("sp", lambda e: e.dma_start(out=nd.t[:], in_=ndminT), writes=[nd.b])
    P.dma("sp", lambda e: e.dma_start(out=qo.t[:], in_=qoff), writes=[qo.b])
    P.op("pool", lambda e: e.memset(ones1.t[:], 1.0), writes=[ones1.b])
    for i in range(NSLOT):
        P.op("pool", lambda e, i=i: e.tensor_tensor(out=nd.t[:, i * 128:(i + 1) * 128], in0=qo.t[:], in1=nd.t[:, i * 128:(i + 1) * 128], op=ALU.subtract),
             reads=[qo.b, nd.b], writes=[nd.b])
    for h in range(B_HEADS):
        P.op("pool", lambda e, h=h: e.tensor_scalar(out=rrow.t[:, h, :], in0=nd.t[:], scalar1=float(slopes[h]), scalar2=None, op0=ALU.mult),
             reads=[nd.b], writes=[rrow.b])
    S = SEQ
    NKB = S // 512
    kvn = TB(cx, [128, 2, S], BF16, "dkvn")
    Wb = TB(cx, [128, 2, 2048], BF16, "dW")
    P.dma("pool", lambda e: e.dma_start(out=Wb.t[:], in_=wkv.rearrange("(k p) n -> p k n", p=128)), writes=[Wb.b])
    g = TB(cx, [128, 2], F32, "dg")
    P.dma("sp", lambda e: e.dma_start(out=g.t[:], in_=kvg), writes=[g.b])
    bc = TB(cx, [128, B_HEADS, 64], F32, "dbc")
    P.dma("sp", lambda e: e.dma_start(out=bc.t[:], in_=biascol), writes=[bc.b])
    cr = TB(cx, [128, 8, B_HEADS, 128], BF16, "dcorr")
    P.dma("pool", lambda e: e.dma_start(out=cr.t[:], in_=corr), writes=[cr.b])
    idb = TB(cx, [128, 128], BF16, "didb")
    P.dma("pool", lambda e: e.dma_start(out=idb.t[:], in_=ident), writes=[idb.b])
    ones = TB(cx, [128, 128], BF16, "dones")
    P.op("dve", lambda e: e.memset(ones.t[:], 1.0), writes=[ones.b])
    qb = TB(cx, [128, B_HEADS, NSLOT * 128], BF16, "dqb")
    P.dma("pool", lambda e: e.dma_start(out=qb.t[:], in_=qT.rearrange("(h p) t -> p h t", p=128)), writes=[qb.b])
    P.op("act", lambda e: e.activation(out=qb.t[:], in_=qb.t[:], func=AF.Identity, scale=float(HD) ** -0.5), reads=[qb.b], writes=[qb.b])
    kv32 = [TB(cx, [128, 2, 512], F32, "dkv32_%d" % i) for i in range(2)]
    sq = [TB(cx, [128, 512], BF16, "dsq%d" % i) for i in range(2)]
    rt = [TB(cx, [128, 512], F32, "drt%d" % i) for i in range(2)]
    pkv = [cx.ps(name="dpkv%d" % i) for i in range(2)]
    pkv_b = [Buf("dpkv%d" % i) for i in range(2)]
    pst, pst_b = pkv[0], pkv_b[0]
    kvsrc = kvT.rearrange("(k p) t -> p k t", p=128)
    for blk in range(NKB):
        bs = slice(blk * 512, (blk + 1) * 512)
        kv = kv32[blk % 2]
        r = rt[blk % 2]
        P.dma("sp", lambda e, kv=kv, bs=bs: e.dma_start(out=kv.t[:], in_=kvsrc[:, :, bs]), writes=[kv.b])
        for kc in range(2):
            s_ = sq[kc]
            P.op("act", lambda e, kv=kv, kc=kc, s_=s_: e.activation(out=s_.t[:], in_=kv.t[:, kc, :], func=AF.Square), reads=[kv.b], writes=[s_.b])
            P.op("pe", lambda e, kc=kc, s_=s_: e.matmul(pst[:], ones.t[:], s_.t[:], start=(kc == 0), stop=(kc == 1)), reads=[ones.b, s_.b], writes=[pst_b])
        P.op("dve", lambda e, r=r: e.tensor_scalar(out=r.t[:], in0=pst[:], scalar1=1.0 / KV_RANK, scalar2=EPS, op0=ALU.mult, op1=ALU.add), reads=[pst_b], writes=[r.b])
        P.op("act", lambda e, r=r: e.activation(out=r.t[:], in_=r.t[:], func=AF.Sqrt), reads=[r.b], writes=[r.b])
        P.op("dve", lambda e, r=r: e.reciprocal(r.t[:], r.t[:]), reads=[r.b], writes=[r.b])
        for kc in range(2):
            P.op("dve", lambda e, kv=kv, kc=kc, r=r, bs=bs: e.scalar_tensor_tensor(out=kvn.t[:, kc, bs], in0=kv.t[:, kc, :], scalar=g.t[:, kc:kc + 1], in1=r.t[:],
                                                                                   op0=ALU.mult, op1=ALU.mult), reads=[kv.b, g.b, r.b], writes=[kvn.b])
    Kh = TB(cx, [128, S], BF16, "dKh")
    Vh = TB(cx, [128, S // 128, 128], BF16, "dVh")
    pL = [cx.ps(name="dpL%d" % i) for i in range(2)]
    pL_b = [Buf("dpL%d" % i) for i in range(2)]
    pOD = [cx.ps(name="dpOD%d" % i) for i in range(2)]
    pOD_b = [Buf("dpOD%d" % i) for i in range(2)]
    pDD = [cx.ps(name="dpDD%d" % i) for i in range(2)]
    pDD_b = [Buf("dpDD%d" % i) for i in range(2)]
    selb = [TB(cx, [128, 64, 128], BF16, "dsel%d" % i) for i in range(2)]
    eL = [TB(cx, [128, 512], BF16, "deL%d" % i) for i in range(2)]
    pT = [TB(cx, [128, 512], BF16, "dpT%d" % i) for i in range(2)]
    rec = [TB(cx, [128, 128], F32, "drec%d" % i) for i in range(2)]
    ost = [TB(cx, [128, NSLOT * 128], F32, "dost%d" % i) for i in range(2)]
    odst = ybT.rearrange("(h p) t -> p h t", p=128)
    n = 0
    nsel = 0
    ng = 0
    nod = 0
    for h in range(B_HEADS):
        for blk in range(NKB):
            bs = slice(blk * 512, (blk + 1) * 512)
            pp, pp_b = pkv[n % 2], pkv_b[n % 2]
            n += 1
            for kc in range(2):
                P.op("pe", lambda e, pp=pp, kc=kc, h=h, bs=bs: e.matmul(pp[:], Wb.t[:, kc, h * 128:(h + 1) * 128], kvn.t[:, kc, bs], start=(kc == 0), stop=(kc == 1)),
                     reads=[Wb.b, kvn.b], writes=[pp_b])
            P.op("act", lambda e, pp=pp, bs=bs: e.activation(out=Kh.t[:, bs], in_=pp[:], func=AF.Copy), reads=[pp_b], writes=[Kh.b])
        for g4 in range(S // 512):
            pp, pp_b = pkv[n % 2], pkv_b[n % 2]
            n += 1
            for j in range(4):
                tl = g4 * 4 + j
                for kc in range(2):
                    P.op("pe", lambda e, pp=pp, kc=kc, h=h, tl=tl, j=j: e.matmul(
                        pp[:, j * 128:(j + 1) * 128], kvn.t[:, kc, tl * 128:(tl + 1) * 128], Wb.t[:, kc, 1024 + h * 128:1024 + (h + 1) * 128],
                        start=(kc == 0), stop=(kc == 1)), reads=[Wb.b, kvn.b], writes=[pp_b])
            P.op("dve", lambda e, pp=pp, g4=g4: e.tensor_copy(Vh.t[:, g4 * 4:(g4 + 1) * 4, :].rearrange("p a b -> p (a b)"), pp[:]), reads=[pp_b], writes=[Vh.b])
        oh = ost[h % 2]
        items = []
        for i in range(NSLOT):
            ntile = slot_tiles(i)
            ctx = {"i": i, "ntile": ntile, "sb": selb[nsel % 2], "od": pOD[nod % 2], "od_b": pOD_b[nod % 2],
                   "dn": pDD[nod % 2], "dn_b": pDD_b[nod % 2], "rc": rec[nod % 2], "qs": slice(i * 128, (i + 1) * 128)}
            nsel += 1
            nod += 1
            for g4 in range(ntile // 4):
                items.append((ctx, g4, ng % 2))
                ng += 1

        def stage_a(item, h=h):
            ctx, g4, bi = item
            i, ntile, sb_, qs = ctx["i"], ctx["ntile"], ctx["sb"], ctx["qs"]
            pl, pl_b, pt = pL[bi], pL_b[bi], pT[bi]
            if g4 == 0:
                o0 = slot_off(i)
                P.dma("sp", lambda e: e.dma_start(out=sb_.t[:, 0:ntile, :], in_=selT[:, o0:o0 + ntile, :]), writes=[sb_.b])
            for j in range(4):
                tl = g4 * 4 + j
                r = tl - 8 * i
                P.op("pe", lambda e, j=j, tl=tl: e.matmul(
                    pl[:, j * 128:(j + 1) * 128], Kh.t[:, tl * 128:(tl + 1) * 128], qb.t[:, h, qs], start=True, stop=False),
                    reads=[Kh.b, qb.b], writes=[pl_b])
                if r >= 0:
                    P.op("pe", lambda e, j=j, r=r: e.matmul(
                        pl[:, j * 128:(j + 1) * 128], idb.t[:], cr.t[:, r, h, :], start=False, stop=False),
                        reads=[idb.b, cr.b], writes=[pl_b])
                P.op("pe", lambda e, j=j: e.matmul(
                    pl[:, j * 128:(j + 1) * 128], ones1.t[:], rrow.t[:, h, qs], start=False, stop=False),
                    reads=[ones1.b, rrow.b], writes=[pl_b])
                P.op("pe", lambda e, j=j, tl=tl: e.matmul(
                    pl[:, j * 128:(j + 1) * 128], idb.t[:], sb_.t[:, tl, :], start=False, stop=True),
                    reads=[idb.b, sb_.b], writes=[pl_b])
            for j in range(4):
                tl = g4 * 4 + j
                ix = tl - 8 * i + 56
                P.op("act", lambda e, j=j, ix=ix: e.activation(
                    out=pt.t[:, j * 128:(j + 1) * 128], in_=pl[:, j * 128:(j + 1) * 128], func=AF.Exp, bias=bc.t[:, h, ix:ix + 1], scale=1.0),
                    reads=[pl_b, bc.b], writes=[pt.b])

        def stage_b(item, oh=oh):
            ctx, g4, bi = item
            ntile, od, od_b, dn, dn_b, rc, qs = ctx["ntile"], ctx["od"], ctx["od_b"], ctx["dn"], ctx["dn_b"], ctx["rc"], ctx["qs"]
            pt = pT[bi]
            for j in range(4):
                tl = g4 * 4 + j
                P.op("pe", lambda e, j=j, tl=tl: e.matmul(
                    od[:, 0:128], Vh.t[:, tl, :], pt.t[:, j * 128:(j + 1) * 128], start=(tl == 0), stop=(tl == ntile - 1)),
                    reads=[Vh.b, pt.b], writes=[od_b])
            for j in range(4):
                tl = g4 * 4 + j
                P.op("pe", lambda e, j=j, tl=tl: e.matmul(
                    dn[:, 0:128], ones.t[:], pt.t[:, j * 128:(j + 1) * 128], start=(tl == 0), stop=(tl == ntile - 1)),
                    reads=[ones.b, pt.b], writes=[dn_b])
            if g4 == ntile // 4 - 1:
                P.op("dve", lambda e: e.reciprocal(rc.t[:], dn[:, 0:128]), reads=[dn_b], writes=[rc.b])
                P.op("dve", lambda e: e.tensor_tensor(out=oh.t[:, qs], in0=od[:, 0:128], in1=rc.t[:], op=ALU.mult),
                     reads=[od_b, rc.b], writes=[oh.b])

        stage_a(items[0])
        for k in range(1, len(items)):
            stage_a(items[k])
            stage_b(items[k - 1])
        stage_b(items[-1])
        P.dma("sp", lambda e, h=h, oh=oh: e.dma_start(out=odst[:, h, :], in_=oh.t[:]), reads=[oh.b])


def build_C2():
    import contextlib
    nc = bass.Bass("TRN2", target_bir_lowering=False)
    with contextlib.ExitStack() as st:
        cx = Ctx(nc, st)
        kvT = cx.din("kvT", [KV_RANK, SEQ])
        kvg = cx.din("kvg", [128, 2])
        wkv = cx.din("wkv", [KV_RANK, 2048])
        qT = cx.din("qT", [1024, NSLOT * 128])
        selT = cx.din("selT", [128, NT_TOTAL, 128], BF16)
        biascol = cx.din("biascol", [128, B_HEADS, 64])
        corr = cx.din("corr", [128, 8, B_HEADS, 128])
        ident = cx.din("ident", [128, 128])
        ybT = cx.dout("ybT", [1024, NSLOT * 128])
        ndminT = cx.din("ndminT", [1, NSLOT * 128])
        qoff = cx.din("qoff", [1, 128])
        emit_dsa_attn(cx, kvT, kvg, wkv, qT, selT, biascol, corr, ident, ybT, ndminT, qoff)
        cx.finish()
    return nc


def build_D0A1():
    import contextlib
    nc = bass.Bass("TRN2", target_bir_lowering=False)
    with contextlib.ExitStack() as st:
        cx = Ctx(nc, st)
        P = cx.P
        hT = cx.din("hT", [D, TOKC])
        mod0 = cx.din("mod0", [128, 144])
        gT0 = cx.din("gT0", [128, 3, KC])
        yaT = cx.din("yaT", [1024, TOKC])
        ybT = cx.din("ybT", [1024, TOKC])
        w_glu = cx.din("w_glu", [1024, 1024])
        bgT = cx.din("bgT", [128, 8])
        w_out = cx.din("w_out", [D, D])
        wg0 = cx.din("wg0", [D, DFF])
        wu0 = cx.din("wu0", [D, DFF])
        wd0 = cx.din("wd0", [DFF, D])
        mod1 = cx.din("mod1", [128, 144])
        gT1 = cx.din("gT1", [128, 3, KC])
        wg1 = cx.din("wg1", [D, DFF])
        wu1 = cx.din("wu1", [D, DFF])
        wd1 = cx.din("wd1", [DFF, D])
        w_qkv = cx.din("w_qkv", [D, 3 * D])
        hT_out = cx.dout("hT_out", [D, TOKC])
        qkvT = cx.dout("qkvT", [3 * D, TOKC])
        co = Core(cx)
        co.load_h(hT)
        co.load_mod(mod0, gT0)
        co.mixer0_out(yaT, ybT, w_glu, bgT, w_out)
        co.ffn(2, wg0, wu0, wd0)
        co.load_mod(mod1, gT1)
        co.ffn(0, wg1, wu1, wd1)
        co.store_h(hT_out)
        for t0 in range(0, TOKC, TT):
            co.adaln_full(1, t0)
            co.proj(w_qkv, 3 * D, t0, co.out_epilogue(qkvT, t0))
        cx.finish()
    return nc


def build_D1():
    import contextlib
    nc = bass.Bass("TRN2", target_bir_lowering=False)
    with contextlib.ExitStack() as st:
        cx = Ctx(nc, st)
        hT = cx.din("hT", [D, TOKC])
        mod1 = cx.din("mod1", [128, 144])
        gT1 = cx.din("gT1", [128, 3, KC])
        oT = cx.din("oT", [D, TOKC])
        w_out = cx.din("w_out", [D, D])
        wg = cx.din("wg", [D, DFF])
        wu = cx.din("wu", [D, DFF])
        wd = cx.din("wd", [DFF, D])
        gfT = cx.din("gfT", [128, KC])
        outT = cx.dout("outT", [D, TOKC])
        co = Core(cx)
        co.load_h(hT)
        co.load_mod(mod1, gT1)
        co.mixer1_out(oT, w_out)
        co.ffn(2, wg, wu, wd)
        co.final_norm(gfT, outT)
        cx.finish()
    return nc


MODW = 9 * D // NCORES
MODC = MODW // 128


def build_M():
    import contextlib
    nc = bass.Bass("TRN2", target_bir_lowering=False)
    with contextlib.ExitStack() as st:
        cx = Ctx(nc, st)
        P = cx.P
        condT = cx.din("condT", [128, KC])
        aw = cx.din("aw", [2, D, MODW])
        abT = cx.din("abT", [128, 2, MODC])
        modc = cx.dout("modc", [128, 2, MODC])
        ws = Stream(cx)
        c32 = TB(cx, [128, KC], F32, "mc32")
        cb = TB(cx, [128, KC], BF16, "mcb")
        ab = TB(cx, [128, 2, MODC], F32, "mab")
        mo = TB(cx, [128, 2, MODC], F32, "mmo")
        pm = cx.ps(name="mpm")
        pm_b = Buf("mpm")
        P.dma("sp", lambda e: e.dma_start(out=c32.t[:], in_=condT), writes=[c32.b])
        P.dma("sp", lambda e: e.dma_start(out=ab.t[:], in_=abT), writes=[ab.b])
        P.op("act", lambda e: e.activation(out=cb.t[:], in_=c32.t[:], func=AF.Silu), reads=[c32.b], writes=[cb.b])
        for l in range(2):
            src = aw[l].rearrange("(k p) n -> p k n", p=128)
            for nb in range((MODW + 511) // 512):
                ncol = min(512, MODW - nb * 512)
                v, wb = ws.load(src[:, :, nb * 512:nb * 512 + ncol], KC, ncol)
                for j in range(ncol // 128):
                    col = l * MODC + nb * 4 + j
                    for k in range(KC):
                        P.op("pe", lambda e, v=v, j=j, k=k, col=col: e.matmul(
                            pm[:, col:col + 1], v[:, k, j * 128:(j + 1) * 128], cb.t[:, k:k + 1], start=(k == 0), stop=(k == KC - 1)),
                            reads=[wb, cb.b], writes=[pm_b])
        P.op("dve", lambda e: e.tensor_tensor(out=mo.t[:].rearrange("p a b -> p (a b)"), in0=pm[:, 0:2 * MODC],
                                              in1=ab.t[:].rearrange("p a b -> p (a b)"), op=ALU.add), reads=[pm_b, ab.b], writes=[mo.b])
        P.dma("sp", lambda e: e.dma_start(out=modc, in_=mo.t[:]), reads=[mo.b])
        cx.finish()
    return nc


def _fm(v):
    return np.ascontiguousarray(np.asarray(v).reshape(-1, 128).T)


def _gT(norm_g_layer):
    return np.ascontiguousarray(np.asarray(norm_g_layer).reshape(3, KC, 128).transpose(2, 0, 1))


_SAVE = None


def _run(nc, ims):
    return run_bass_kernel_spmd(nc, ims, core_ids=list(range(NCORES))).results


def kernel(**inp):
    inp = {k: np.asarray(v) for k, v in inp.items()}
    f32 = np.float32
    x = inp['x'][0]
    cores = range(NCORES)
    tokc = [slice(c * TOKC, (c + 1) * TOKC) for c in cores]
    condT = _fm(inp['c'][0])
    sv = _SAVE if _SAVE is not None else {}
    if 'M' not in sv:
        ims = [{"condT": condT, "aw": np.ascontiguousarray(inp['ada_w'][:, :, c * MODW:(c + 1) * MODW]),
                "abT": np.ascontiguousarray(inp['ada_b'][:, c * MODW:(c + 1) * MODW].reshape(2, MODC, 128).transpose(2, 0, 1))} for c in cores]
        r = _run(build_M(), ims)
        sv['M'] = [np.ascontiguousarray(np.concatenate([r[c]["modc"][:, l, :] for c in cores], axis=1)) for l in range(2)]
    mod0, mod1 = sv['M']
    if 'A0' not in sv:
        ims = [{"xT": np.ascontiguousarray(x[tokc[c]].T), "mod0": mod0,
                "gT": _gT(inp['norm_g'][0]), "wg": inp['ffn_w_gate'][0, 0], "wu": inp['ffn_w_up'][0, 0], "wd": inp['ffn_w_down'][0, 0],
                "w_in": inp['ab_w_in'][0]} for c in cores]
        r = _run(build_A0(), ims)
        sv['A0'] = {"hT": [r[c]["hT_out"] for c in cores],
                    "pT": np.concatenate([r[c]["pT_out"] for c in cores], axis=1)}
    hT0, pT = sv['A0']["hT"], sv['A0']["pT"]
    if 'S5' not in sv:
        ims = []
        for c in cores:
            im = s5_host_layout(inp, c)
            im["uT"] = np.ascontiguousarray(pT[c * 128:(c + 1) * 128, :])
            ims.append(im)
        r = _run(build_S5(), ims)
        sv['S5'] = np.concatenate([r[c]["yT"] for c in cores], axis=0)
    yaT = sv['S5']
    toks = [np.concatenate([np.arange(128) + 128 * (8 * i + c) for i in range(NSLOT)]) for c in cores]
    ident = np.eye(128, dtype=f32)
    if 'C1' not in sv:
        pow2 = np.tile((0.5 ** np.arange(NBIS + 1)).astype(f32), (128, 1))
        ims = [{"kidxT": np.ascontiguousarray(pT[3328:3392, :]), "qidxT": np.ascontiguousarray(pT[2304:3328, toks[c]]),
                "widx": np.ascontiguousarray(pT[3392:3408, toks[c]].reshape(16, NSLOT, 128).transpose(2, 1, 0)),
                "adm": dsa_adm_mask(c), "pow2": pow2, "ident": ident, "negpos": dsa_negpos(c)} for c in cores]
        r = _run(build_C1(), ims)
        sv['C1'] = [(r[c]["selT"], r[c]["ndmin"]) for c in cores]
    if 'C2' not in sv:
        ims = []
        for c in cores:
            bcol, corr = dsa_alibi_tables(c)
            ims.append({"kvT": np.ascontiguousarray(pT[2048:2304, :]), "kvg": _fm(inp['dsa_kv_norm_g'][0]), "wkv": inp['dsa_w_kv_up'][0],
                        "qT": np.ascontiguousarray(pT[1024:2048, toks[c]]), "selT": sv['C1'][c][0], "biascol": bcol, "corr": corr, "ident": ident,
                        "ndminT": np.ascontiguousarray(sv['C1'][c][1].T.reshape(1, -1)),
                        "qoff": (64 - np.arange(128, dtype=f32)).reshape(1, 128)})
        r = _run(build_C2(), ims)
        ybT = np.zeros((1024, SEQ), f32)
        for c in cores:
            ybT[:, toks[c]] = r[c]["ybT"]
        sv['C2'] = ybT
    ybT = sv['C2']
    if 'D0A1' not in sv:
        ims = [{"hT": hT0[c], "mod0": mod0, "gT0": _gT(inp['norm_g'][0]), "yaT": np.ascontiguousarray(yaT[:, tokc[c]]),
                "ybT": np.ascontiguousarray(ybT[:, tokc[c]]), "w_glu": inp['s5_w_glu'][0], "bgT": _fm(inp['s5_b_glu'][0]),
                "w_out": inp['ab_w_out'][0], "wg0": inp['ffn_w_gate'][0, 1], "wu0": inp['ffn_w_up'][0, 1], "wd0": inp['ffn_w_down'][0, 1],
                "mod1": mod1, "gT1": _gT(inp['norm_g'][1]),
                "wg1": inp['ffn_w_gate'][1, 0], "wu1": inp['ffn_w_up'][1, 0], "wd1": inp['ffn_w_down'][1, 0], "w_qkv": inp['c_w_qkv'][0]}
               for c in cores]
        r = _run(build_D0A1(), ims)
        sv['D0A1'] = {"hT": [r[c]["hT_out"] for c in cores],
                      "qkvT": np.concatenate([r[c]["qkvT"] for c in cores], axis=1)}
    hT1, qkvT = sv['D0A1']["hT"], sv['D0A1']["qkvT"]
    if 'ATT' not in sv:
        biasT = att_bias_table(inp['c_rel_bias'][0])
        kpad = np.concatenate([np.zeros((D, HALO), f32), qkvT[D:2 * D]], axis=1)
        vpad = np.concatenate([np.zeros((D, HALO), f32), qkvT[2 * D:3 * D]], axis=1)
        ims = [{"qT": np.ascontiguousarray(qkvT[0:D, tokc[c]]), "kT": np.ascontiguousarray(kpad[:, c * TOKC:(c + 1) * TOKC + HALO]),
                "v": np.ascontiguousarray(vpad[:, c * TOKC:(c + 1) * TOKC + HALO].T), "biasT": biasT,
                "vones": np.full((128, 128), 0.0 if c == 0 else 1.0, f32)} for c in cores]
        r = _run(build_ATT(), ims)
        sv['ATT'] = [r[c]["oT"] for c in cores]
    oT = sv['ATT']
    ims = [{"hT": hT1[c], "mod1": mod1, "gT1": _gT(inp['norm_g'][1]), "oT": oT[c], "w_out": inp['c_w_out'][0],
            "wg": inp['ffn_w_gate'][1, 1], "wu": inp['ffn_w_up'][1, 1], "wd": inp['ffn_w_down'][1, 1], "gfT": _fm(inp['final_norm_g'])}
           for c in cores]
    r = _run(build_D1(), ims)
    out = np.zeros((1, SEQ, D), f32)
    for c in cores:
        out[0, tokc[c], :] = r[c]["outT"].T
    return out
```
